# Optimizing a Trainium2 kernel written in Bass

```python
import math
import jax, jax.numpy as jnp
from jax import lax
import numpy as np

D_MODEL = 1024
BATCH = 2
SEQ = 16384
DEPTH = 4

CHUNK = 64
MEM_LEN = 256
D_MIX = D_MODEL
SSD_WIDTH = D_MIX // 2
SSD_HEAD_DIM = 64
SSD_HEADS = SSD_WIDTH // SSD_HEAD_DIM
SSD_GROUPS = 2
SSD_STATE = 128
SSD_CONV = 4
SSD_CONV_CH = SSD_WIDTH + 2 * SSD_GROUPS * SSD_STATE
FOX_WIDTH = D_MIX // 4
FOX_HEAD_DIM = 64
FOX_HEADS = FOX_WIDTH // FOX_HEAD_DIM
FOX_BLOCK = 128
POOL_WIDTH = D_MIX - SSD_WIDTH - FOX_WIDTH
POOL_WINDOWS = (2, 4, 8, 16)
POOL_GROUPS = len(POOL_WINDOWS)
POOL_GROUP_DIM = POOL_WIDTH // POOL_GROUPS
IN_SIZES = (SSD_WIDTH, SSD_CONV_CH, SSD_HEADS, FOX_WIDTH, FOX_WIDTH, FOX_WIDTH, FOX_HEADS, POOL_WIDTH)
D_IN = sum(IN_SIZES)
XATTN_HEADS = 4
XATTN_HEAD_DIM = D_MODEL // XATTN_HEADS
D_FF = 7 * D_MODEL // 2
N_EXPERTS = 8
TOP_K = 2
MOE_BLOCK = 256
N_DENSE = (DEPTH + 1) // 2
N_MOE = DEPTH // 2
DN_ALPHA = (2 * DEPTH) ** 0.25
DN_BETA = (8 * DEPTH) ** -0.25
LN_EPS = 1e-5
RMS_EPS = 1e-5

kernel_name = "hybrid_ssd_fox_pool_moe_deepnorm"


def layer_norm(x, g, b):
    xf = x.astype(jnp.float32)
    mu = jnp.mean(xf, axis=-1, keepdims=True)
    var = jnp.mean(jnp.square(xf - mu), axis=-1, keepdims=True)
    return ((xf - mu) * lax.rsqrt(var + LN_EPS) * g + b).astype(x.dtype)


def split_in(h):
    outs, off = [], 0
    for s in IN_SIZES:
        outs.append(h[..., off:off + s])
        off += s
    return outs


def causal_depthwise_conv(u, w, b):
    K, T = w.shape[0], u.shape[1]
    up = jnp.pad(u, ((0, 0), (K - 1, 0), (0, 0)))
    return sum(up[:, k:k + T] * w[k] for k in range(K)) + b


def ssd_mixer(z, xbc, dt_raw, conv_w, conv_b, dt_bias, a_log, d_skip, norm_w):
    Bsz, T, _ = z.shape
    G, R, P, N, L = SSD_GROUPS, SSD_HEADS // SSD_GROUPS, SSD_HEAD_DIM, SSD_STATE, CHUNK
    nc = T // L
    xbc = jax.nn.silu(causal_depthwise_conv(xbc, conv_w, conv_b))
    xs = xbc[..., :SSD_WIDTH]
    bm = xbc[..., SSD_WIDTH:SSD_WIDTH + G * N].reshape(Bsz, nc, L, G, N)
    cm = xbc[..., SSD_WIDTH + G * N:].reshape(Bsz, nc, L, G, N)
    dt = jax.nn.softplus(dt_raw.astype(jnp.float32) + dt_bias)
    a = -jnp.exp(a_log.astype(jnp.float32))
    X = xs.reshape(Bsz, nc, L, G, R, P) * dt.reshape(Bsz, nc, L, G, R)[..., None]
    A = (dt * a).reshape(Bsz, nc, L, G, R).transpose(0, 3, 4, 1, 2)
    a_cs = jnp.cumsum(A, axis=-1)
    seg = a_cs[..., :, None] - a_cs[..., None, :]
    causal = jnp.tril(jnp.ones((L, L), dtype=bool))
    Lmat = jnp.exp(jnp.where(causal, seg, -jnp.inf))
    cb = jnp.einsum('bclgn,bcsgn->bcgls', cm, bm)
    y_diag = jnp.einsum('bcgls,bgrcls,bcsgrp->bclgrp', cb, Lmat, X)
    decay_states = jnp.exp(a_cs[..., -1:] - a_cs)
    states = jnp.einsum('bclgn,bgrcl,bclgrp->bcgrpn', bm, decay_states, X)
    chunk_decay = jnp.exp(a_cs[..., -1])

    def step(h, inp):
        s_c, d_c = inp
        return h * d_c[..., None, None] + s_c, h

    h0 = jnp.zeros((Bsz, G, R, P, N), states.dtype)
    _, prev = lax.scan(step, h0, (jnp.moveaxis(states, 1, 0), jnp.moveaxis(chunk_decay, -1, 0)))
    prev = jnp.moveaxis(prev, 0, 1)
    y_off = jnp.einsum('bclgn,bcgrpn,bgrcl->bclgrp', cm, prev, jnp.exp(a_cs))
    y = (y_diag + y_off).reshape(Bsz, T, SSD_HEADS, P) + xs.reshape(Bsz, T, SSD_HEADS, P) * d_skip[:, None]
    y = y.reshape(Bsz, T, SSD_WIDTH) * jax.nn.silu(z)
    yg = y.reshape(Bsz, T, G, SSD_WIDTH // G).astype(jnp.float32)
    yg = yg * lax.rsqrt(jnp.mean(jnp.square(yg), axis=-1, keepdims=True) + RMS_EPS)
    return (yg.reshape(Bsz, T, SSD_WIDTH) * norm_w).astype(z.dtype)


def fox_mixer(q, k, v, f_logit, f_bias):
    Bsz, T, _ = q.shape
    H, Dh = FOX_HEADS, FOX_HEAD_DIM
    q = q.reshape(Bsz, T, H, Dh).transpose(0, 2, 1, 3)
    k = k.reshape(Bsz, T, H, Dh).transpose(0, 2, 1, 3)
    v = v.reshape(Bsz, T, H, Dh).transpose(0, 2, 1, 3)
    log_f = jax.nn.log_sigmoid(f_logit.astype(jnp.float32) + f_bias)
    c = jnp.cumsum(log_f, axis=1).transpose(0, 2, 1)
    nq = T // FOX_BLOCK
    q_blocks = q.reshape(Bsz, H, nq, FOX_BLOCK, Dh).transpose(2, 0, 1, 3, 4)
    c_blocks = c.reshape(Bsz, H, nq, FOX_BLOCK).transpose(2, 0, 1, 3)
    key_pos = jnp.arange(T)
    scale = Dh ** -0.5

    def block(args):
        qb, cq, i = args
        q_pos = i * FOX_BLOCK + jnp.arange(FOX_BLOCK)
        s = jnp.einsum('bhqd,bhkd->bhqk', qb, k).astype(jnp.float32) * scale
        s = s + cq[..., :, None] - c[:, :, None, :]
        s = jnp.where(key_pos[None, :] <= q_pos[:, None], s, -jnp.inf)
        p = jax.nn.softmax(s, axis=-1)
        return jnp.einsum('bhqk,bhkd->bhqd', p.astype(v.dtype), v)

    out = lax.map(block, (q_blocks, c_blocks, jnp.arange(nq)))
    return out.transpose(1, 0, 3, 2, 4).reshape(Bsz, T, FOX_WIDTH)


def pool_mixer(u, w, b, scale):
    Bsz, T, _ = u.shape
    ug = u.reshape(Bsz, T, POOL_GROUPS, POOL_GROUP_DIM).astype(jnp.float32)
    cs = jnp.cumsum(ug, axis=1)
    means = []
    for g, win in enumerate(POOL_WINDOWS):
        c_g = cs[:, :, g]
        lagged = jnp.pad(c_g, ((0, 0), (win, 0), (0, 0)))[:, :T]
        count = jnp.minimum(jnp.arange(1, T + 1), win).astype(jnp.float32)[None, :, None]
        means.append((c_g - lagged) / count)
    pooled = jnp.stack(means, axis=2) - ug
    y = jnp.einsum('btgc,gcd->btgd', pooled, w) + b
    return (y.reshape(Bsz, T, POOL_WIDTH) * scale).astype(u.dtype)


def cross_attention(x, mem, wq, wk, wv, wo):
    Bsz, T, D = x.shape
    M = mem.shape[1]
    q = (x @ wq).reshape(Bsz, T, XATTN_HEADS, XATTN_HEAD_DIM)
    k = (mem @ wk).reshape(Bsz, M, XATTN_HEADS, XATTN_HEAD_DIM)
    v = (mem @ wv).reshape(Bsz, M, XATTN_HEADS, XATTN_HEAD_DIM)
    s = jnp.einsum('bqhd,bkhd->bhqk', q, k).astype(jnp.float32) * XATTN_HEAD_DIM ** -0.5
    p = jax.nn.softmax(s, axis=-1)
    o = jnp.einsum('bhqk,bkhd->bqhd', p.astype(v.dtype), v).reshape(Bsz, T, D)
    return o @ wo


def swiglu(x, w1, w3, w2):
    return (jax.nn.silu(x @ w1) * (x @ w3)) @ w2


def moe_swiglu(x, router_w, w1, w3, w2):
    Bsz, T, D = x.shape
    xf = x.reshape(-1, D)
    N = xf.shape[0]
    NK = N * TOP_K
    logits = (xf @ router_w).astype(jnp.float32)
    top_val, top_idx = lax.top_k(logits, TOP_K)
    gates = jax.nn.softmax(top_val, axis=-1)
    flat_e = top_idx.reshape(-1)
    flat_tok = jnp.arange(NK) // TOP_K
    flat_g = gates.reshape(-1)
    order = jnp.argsort(flat_e)
    sorted_e = flat_e[order]
    counts = jnp.zeros((N_EXPERTS,), jnp.int32).at[flat_e].add(1)
    padded = (counts + MOE_BLOCK - 1) // MOE_BLOCK * MOE_BLOCK
    start = jnp.cumsum(counts) - counts
    pend = jnp.cumsum(padded)
    pstart = pend - padded
    dest = pstart[sorted_e] + (jnp.arange(NK) - start[sorted_e])
    cap = -(-NK // MOE_BLOCK) * MOE_BLOCK + N_EXPERTS * MOE_BLOCK
    nblk = cap // MOE_BLOCK
    slot_tok = jnp.zeros((cap,), jnp.int32).at[dest].set(flat_tok[order])
    slot_gate = jnp.zeros((cap,), jnp.float32).at[dest].set(flat_g[order])
    block_exp = jnp.minimum(jnp.searchsorted(pend, jnp.arange(nblk) * MOE_BLOCK, side='right'), N_EXPERTS - 1)
    xs = xf[slot_tok].reshape(nblk, MOE_BLOCK, D)

    def run(args):
        xb, e = args
        return swiglu(xb, w1[e], w3[e], w2[e])

    ys = lax.map(run, (xs, block_exp)).reshape(cap, D)
    contrib = (ys * slot_gate[:, None]).astype(x.dtype)
    out = jnp.zeros((N, D), x.dtype).at[slot_tok].add(contrib)
    return out.reshape(Bsz, T, D)


def setup_inputs(seed: int = 0) -> dict:
    key = jax.random.key(seed)
    ks = iter(jax.random.split(key, 40))
    nrm = lambda shape, s: jax.random.normal(next(ks), shape, jnp.float32) * s
    x = nrm((BATCH, SEQ, D_MODEL), 1.0)
    mem = nrm((BATCH, MEM_LEN, D_MODEL), 1.0)
    w_in = nrm((DEPTH, D_MODEL, D_IN), D_MODEL ** -0.5)
    ssm_conv_w = nrm((DEPTH, SSD_CONV, SSD_CONV_CH), SSD_CONV ** -0.5)
    ssm_conv_b = nrm((DEPTH, SSD_CONV_CH), 0.02)
    u = jax.random.uniform(next(ks), (DEPTH, SSD_HEADS), jnp.float32)
    dt0 = jnp.exp(u * (math.log(0.1) - math.log(0.001)) + math.log(0.001))
    ssm_dt_bias = dt0 + jnp.log(-jnp.expm1(-dt0))
    ssm_a_log = jnp.log(jax.random.uniform(next(ks), (DEPTH, SSD_HEADS), jnp.float32, 1.0, 16.0))
    ssm_d = 1.0 + nrm((DEPTH, SSD_HEADS), 0.1)
    ssm_norm_w = 1.0 + nrm((DEPTH, SSD_WIDTH), 0.02)
    fox_f_bias = jax.random.uniform(next(ks), (DEPTH, FOX_HEADS), jnp.float32, 2.0, 7.0)
    pool_w = nrm((DEPTH, POOL_GROUPS, POOL_GROUP_DIM, POOL_GROUP_DIM), POOL_GROUP_DIM ** -0.5)
    pool_b = nrm((DEPTH, POOL_GROUPS, POOL_GROUP_DIM), 0.02)
    pool_scale = 1.0 + nrm((DEPTH, POOL_WIDTH), 0.1)
    w_out = nrm((DEPTH, D_MIX, D_MODEL), D_MIX ** -0.5 * DN_BETA)
    ln1_g = 1.0 + nrm((DEPTH, D_MODEL), 0.02)
    ln1_b = nrm((DEPTH, D_MODEL), 0.02)
    xa_wq = nrm((DEPTH, D_MODEL, D_MODEL), D_MODEL ** -0.5)
    xa_wk = nrm((DEPTH, D_MODEL, D_MODEL), D_MODEL ** -0.5)
    xa_wv = nrm((DEPTH, D_MODEL, D_MODEL), D_MODEL ** -0.5)
    xa_wo = nrm((DEPTH, D_MODEL, D_MODEL), D_MODEL ** -0.5 * DN_BETA)
    ln2_g = 1.0 + nrm((DEPTH, D_MODEL), 0.02)
    ln2_b = nrm((DEPTH, D_MODEL), 0.02)
    ffn_w1 = nrm((N_DENSE, D_MODEL, D_FF), D_MODEL ** -0.5)
    ffn_w3 = nrm((N_DENSE, D_MODEL, D_FF), D_MODEL ** -0.5)
    ffn_w2 = nrm((N_DENSE, D_FF, D_MODEL), D_FF ** -0.5 * DN_BETA)
    router_w = nrm((N_MOE, D_MODEL, N_EXPERTS), D_MODEL ** -0.5)
    moe_w1 = nrm((N_MOE, N_EXPERTS, D_MODEL, D_FF), D_MODEL ** -0.5)
    moe_w3 = nrm((N_MOE, N_EXPERTS, D_MODEL, D_FF), D_MODEL ** -0.5)
    moe_w2 = nrm((N_MOE, N_EXPERTS, D_FF, D_MODEL), D_FF ** -0.5 * DN_BETA)
    ln3_g = 1.0 + nrm((DEPTH, D_MODEL), 0.02)
    ln3_b = nrm((DEPTH, D_MODEL), 0.02)
    return {"x": x, "mem": mem, "w_in": w_in, "ssm_conv_w": ssm_conv_w, "ssm_conv_b": ssm_conv_b,
            "ssm_dt_bias": ssm_dt_bias, "ssm_a_log": ssm_a_log, "ssm_d": ssm_d, "ssm_norm_w": ssm_norm_w,
            "fox_f_bias": fox_f_bias, "pool_w": pool_w, "pool_b": pool_b, "pool_scale": pool_scale,
            "w_out": w_out, "ln1_g": ln1_g, "ln1_b": ln1_b, "xa_wq": xa_wq, "xa_wk": xa_wk,
            "xa_wv": xa_wv, "xa_wo": xa_wo, "ln2_g": ln2_g, "ln2_b": ln2_b, "ffn_w1": ffn_w1,
            "ffn_w3": ffn_w3, "ffn_w2": ffn_w2, "router_w": router_w, "moe_w1": moe_w1,
            "moe_w3": moe_w3, "moe_w2": moe_w2, "ln3_g": ln3_g, "ln3_b": ln3_b}


def reference(x, mem, w_in, ssm_conv_w, ssm_conv_b, ssm_dt_bias, ssm_a_log, ssm_d, ssm_norm_w,
              fox_f_bias, pool_w, pool_b, pool_scale, w_out, ln1_g, ln1_b, xa_wq, xa_wk, xa_wv,
              xa_wo, ln2_g, ln2_b, ffn_w1, ffn_w3, ffn_w2, router_w, moe_w1, moe_w3, moe_w2,
              ln3_g, ln3_b):
    for layer in range(DEPTH):
        h = x @ w_in[layer]
        z, xbc, dt_raw, q, k, v, f_logit, pool_in = split_in(h)
        y_ssd = ssd_mixer(z, xbc, dt_raw, ssm_conv_w[layer], ssm_conv_b[layer], ssm_dt_bias[layer],
                          ssm_a_log[layer], ssm_d[layer], ssm_norm_w[layer])
        y_fox = fox_mixer(q, k, v, f_logit, fox_f_bias[layer])
        y_pool = pool_mixer(pool_in, pool_w[layer], pool_b[layer], pool_scale[layer])
        mix = jnp.concatenate([y_ssd, y_fox, y_pool], axis=-1) @ w_out[layer]
        x = layer_norm(DN_ALPHA * x + mix, ln1_g[layer], ln1_b[layer])
        xa = cross_attention(x, mem, xa_wq[layer], xa_wk[layer], xa_wv[layer], xa_wo[layer])
        x = layer_norm(DN_ALPHA * x + xa, ln2_g[layer], ln2_b[layer])
        j = layer // 2
        if layer % 2 == 0:
            ff = swiglu(x, ffn_w1[j], ffn_w3[j], ffn_w2[j])
        else:
            ff = moe_swiglu(x, router_w[j], moe_w1[j], moe_w3[j], moe_w2[j])
        x = layer_norm(DN_ALPHA * x + ff, ln3_g[layer], ln3_b[layer])
    return x
```

```python
from contextlib import ExitStack
import numpy as np
import concourse.bass as bass
import concourse.mybir as mybir

F32 = mybir.dt.float32
BF16 = mybir.dt.bfloat16
AF = mybir.ActivationFunctionType
ALU = mybir.AluOpType
AX = mybir.AxisListType

ENGS = ["pe", "act", "dve", "pool", "sp"]
NDS = 40


class T:
    __slots__ = ("t", "w", "r", "name")

    def __init__(self, t, name=""):
        self.t = t
        self.w = None
        self.r = {}
        self.name = name

    def __getitem__(self, k):
        return self.t[k]


class Ctx:
    LIMIT = 30000
    NDMAX = 128

    def __init__(self, nc):
        self.nc = nc
        self.es = ExitStack()
        self.eng = {"pe": nc.tensor, "act": nc.scalar, "dve": nc.vector,
                    "pool": nc.gpsimd, "sp": nc.sync}
        nd = self.NDMAX
        self.sems = [self.es.enter_context(nc.semaphore("d%d" % i)) for i in range(NDS)]
        self.mult = [16] * NDS
        self.cnt = [0] * nd
        self.snap = [dict() for _ in range(nd)]
        self.known = {k: np.zeros(nd, np.int64) for k in ENGS}
        self.cur = {}
        self.old = {k: [] for k in ENGS}
        self.edims = {k: set() for k in ENGS}
        for k in ENGS:
            self._new_dim(k)
        self.rr = 0
        self.nwait = 0
        self.ninst = 0

    def _new_dim(self, e):
        d = len(self.sems)
        assert d < self.NDMAX
        self.sems.append(self.es.enter_context(self.nc.semaphore("c_%s_%d" % (e, d))))
        self.mult.append(1)
        if e in self.cur:
            self.old[e].append(self.cur[e])
        self.cur[e] = d
        self.edims[e].add(d)

    def sb(self, name, shape, dt=F32):
        return T(self.es.enter_context(self.nc.sbuf_tensor(name, list(shape), dt)), name)

    def ps(self, name, shape, dt=F32):
        return T(self.es.enter_context(self.nc.psum_tensor(name, list(shape), dt)), name)

    def close(self):
        self.es.close()

    def _wait(self, e, dim, c):
        kn = self.known[e]
        if kn[dim] >= c:
            return
        self.eng[e].wait_ge(self.sems[dim], int(c) * self.mult[dim])
        self.nwait += 1
        s = self.snap[dim].get(c)
        if s is not None:
            np.maximum(kn, s, out=kn)
        kn[dim] = max(kn[dim], c)

    def _deps(self, e, reads, writes, pe_acc=False):
        need = {}
        for t in reads:
            if t.w is not None:
                d, c = t.w
                if need.get(d, 0) < c:
                    need[d] = c
        for t in writes:
            if t.w is not None:
                d, c = t.w
                if not (pe_acc and d in self.edims["pe"]):
                    if need.get(d, 0) < c:
                        need[d] = c
            for d, c in t.r.items():
                if need.get(d, 0) < c:
                    need[d] = c
        for d, c in need.items():
            self._wait(e, d, c)

    def _mark(self, dim, c, reads, writes):
        for t in reads:
            if t.r.get(dim, 0) < c:
                t.r[dim] = c
        for t in writes:
            t.w = (dim, c)
            t.r = {}

    def op(self, e, fn, reads=(), writes=(), pe_acc=False):
        self._deps(e, reads, writes, pe_acc)
        ins = fn()
        if self.cnt[self.cur[e]] >= self.LIMIT:
            self._new_dim(e)
        d = self.cur[e]
        self.cnt[d] += 1
        c = self.cnt[d]
        ins.then_inc(self.sems[d], 1)
        s = self.known[e].copy()
        s[d] = c
        for od in self.old[e]:
            s[od] = self.cnt[od]
        self.snap[d][c] = s
        self._mark(d, c, reads, writes)
        self.ninst += 1
        return ins

    def dma(self, q, out, in_, reads=(), writes=(), **kw):
        self._deps(q, reads, writes)
        kn = self.known[q]
        pick = None
        for k in range(NDS):
            i = (self.rr + k) % NDS
            if kn[i] >= self.cnt[i]:
                pick = i
                break
        if pick is None:
            pick = self.rr % NDS
            self._wait(q, pick, self.cnt[pick])
        self.rr = (pick + 1) % NDS
        d = pick
        ins = self.eng[q].dma_start(out=out, in_=in_, **kw)
        self.cnt[d] += 1
        c = self.cnt[d]
        ins.then_inc(self.sems[d], 16)
        s = kn.copy()
        s[d] = c
        self.snap[d][c] = s
        self._mark(d, c, reads, writes)
        self.ninst += 1
        return ins

    def finish(self, e="sp"):
        for d in range(len(self.sems)):
            if self.cnt[d] > 0:
                self._wait(e, d, self.cnt[d])

    def mm(self, out, lhsT, rhs, start, stop, reads, writes):
        nc = self.nc
        return self.op("pe", lambda: nc.tensor.matmul(out, lhsT, rhs, start=start, stop=stop),
                       reads, writes, pe_acc=not start)

    def tr(self, out, in_, ident, reads, writes):
        nc = self.nc
        return self.op("pe", lambda: nc.tensor.transpose(out, in_, ident), reads, writes)

    def act(self, out, in_, func, reads, writes, **kw):
        nc = self.nc
        return self.op("act", lambda: nc.scalar.activation(out, in_, func, **kw), reads, writes)


D = 1024
DFF = 3584
NFF = DFF // 128
MEM = 256
ALPHA = 8.0 ** 0.25
LN_EPS = 1e-5
RMS_EPS = 1e-5
NEXP = 8


class Pool:
    def __init__(self, c, name, shape, dt, n, ps=False):
        self.ts = [(c.ps if ps else c.sb)("%s%d" % (name, i), shape, dt) for i in range(n)]
        self.i = 0

    def next(self):
        t = self.ts[self.i % len(self.ts)]
        self.i += 1
        return t


def make_ident(c, name, dt):
    nc = c.nc
    t = c.sb(name, [128, 128], dt)
    c.op("pool", lambda: nc.gpsimd.memset(t[:], 1.0), [], [t])
    c.op("pool", lambda: nc.gpsimd.affine_select(t[:], t[:], pattern=[[-1, 128]], compare_op=ALU.is_equal,
                                                 fill=0.0, base=0, channel_multiplier=1), [t], [t])
    return t


def barrier(c):
    for e in ENGS:
        c.finish(e)


def layer_norm_gen(c, P, h, g_bc, b_bc, out):
    nc = c.nc
    st = P["st"].next(); mv = P["mv"].next(); rs = P["rs"].next(); nm = P["nm"].next()
    c.op("dve", lambda: nc.vector.bn_stats(st[:, 0, :], h[:, 0:512]), [h], [st])
    c.op("dve", lambda: nc.vector.bn_stats(st[:, 1, :], h[:, 512:1024]), [h], [st])
    yield
    c.op("dve", lambda: nc.vector.bn_aggr(mv[:], st[:].rearrange("p a b -> p (a b)")), [st], [mv])
    yield
    c.act(rs[:], mv[:, 1:2], AF.Sqrt, [mv, P["eps"]], [rs], bias=P["eps"][:, 0:1], scale=1.0)
    yield
    c.op("dve", lambda: nc.vector.reciprocal(rs[:], rs[:]), [rs], [rs])
    yield
    c.op("dve", lambda: nc.vector.scalar_tensor_tensor(nm[:], mv[:, 0:1], -1.0, rs[:], ALU.mult, ALU.mult), [mv, rs], [nm])
    yield
    tmp = P["lnt"].next()
    c.act(tmp[:], h[:], AF.Identity, [h, rs, nm], [tmp], bias=nm[:, 0:1], scale=rs[:, 0:1])
    yield
    c.op("dve", lambda: nc.vector.tensor_tensor(tmp[:], tmp[:], g_bc[:], ALU.mult), [tmp, g_bc], [tmp])
    yield
    c.op("dve", lambda: nc.vector.tensor_tensor(out[:], tmp[:], b_bc[:], ALU.add), [tmp, b_bc], [out])
    yield


def layer_norm(c, P, h, g_bc, b_bc, out):
    for _ in layer_norm_gen(c, P, h, g_bc, b_bc, out):
        pass


def run_interleaved(gens):
    gens = list(gens)
    while gens:
        for g in list(gens):
            try:
                next(g)
            except StopIteration:
                gens.remove(g)


def load_w_bf(c, dst, src_ap, q="pool"):
    v = src_ap.rearrange("(k p) n -> p k n", p=128)
    for k in range(8):
        c.dma(q, dst[:, k, :], v[:, k, :], writes=[dst])


def build_B(NT, moe, TB2=1024):
    nc = bass.Bass("TRN2", target_bir_lowering=False)
    dt_in = lambda name, shape, dt=F32: nc.dram_tensor(name, list(shape), dt, kind="ExternalInput").ap()
    mix = dt_in("mix", [NT, D]); x = dt_in("x", [NT, D]); mem = dt_in("mem", [MEM, D])
    normw = dt_in("normw", [512]); w_out = dt_in("w_out", [D, D])
    ln_g = [dt_in("ln%d_g" % i, [D]) for i in (1, 2, 3)]
    ln_b = [dt_in("ln%d_b" % i, [D]) for i in (1, 2, 3)]
    wq = dt_in("wq", [D, D]); wk = dt_in("wk", [D, D]); wv = dt_in("wv", [D, D]); wo = dt_in("wo", [D, D])
    if moe:
        router = dt_in("router", [D, NEXP])
        w1 = dt_in("w1", [NEXP, D, DFF]); w3 = dt_in("w3", [NEXP, D, DFF]); w2 = dt_in("w2", [NEXP, DFF, D])
    else:
        w1 = dt_in("w1", [1, D, DFF]); w3 = dt_in("w3", [1, D, DFF]); w2 = dt_in("w2", [1, DFF, D])
    xo = nc.dram_tensor("xo", [NT, D], F32, kind="ExternalOutput").ap()
    xoT = nc.dram_tensor("xoT", [D, NT], BF16, kind="ExternalOutput").ap()
    x2d = nc.dram_tensor("x2d", [NT, D], F32, kind="Internal").ap()
    x2Td = nc.dram_tensor("x2Td", [D, NT], BF16, kind="Internal").ap()
    x2d_T = T(x2d, "x2d"); x2Td_T = T(x2Td, "x2Td")

    c = Ctx(nc)
    NTILE = NT // 128
    NBLK = NT // 512
    nexp = NEXP if moe else 1

    ident_bf = make_ident(c, "ident_bf", BF16)
    ones_bf = c.sb("ones_bf", [128, 128], BF16)
    c.op("pool", lambda: nc.gpsimd.memset(ones_bf[:], 1.0), [], [ones_bf])
    gbc = [None] * 3; bbc = [None] * 3
    def load_ln(sc, i):
        gbc[i] = sc.sb("g%d" % i, [128, D]); bbc[i] = sc.sb("b%d" % i, [128, D])
        c.dma("sp", gbc[i][:], ln_g[i].partition_broadcast(128), writes=[gbc[i]])
        c.dma("sp", bbc[i][:], ln_b[i].partition_broadcast(128), writes=[bbc[i]])
    eps_t = c.sb("eps_t", [128, 1])
    c.op("pool", lambda: nc.gpsimd.memset(eps_t[:], LN_EPS), [], [eps_t])
    gates = c.sb("gates", [128, NTILE, NEXP]) if moe else None
    P = {"st": Pool(c, "st", [128, 2, 6], F32, 3), "mv": Pool(c, "mv", [128, 2], F32, 3),
         "rs": Pool(c, "rs", [128, 1], F32, 3), "nm": Pool(c, "nm", [128, 1], F32, 3),
         "lnt": Pool(c, "lnt", [128, D], F32, 2), "eps": eps_t}

    c1 = Ctx.__new__(Ctx); c1.__dict__.update(c.__dict__); c1.es = ExitStack()
    if True:
        s = c1
        load_ln(s, 0); load_ln(s, 1)
        nw_bc = s.sb("nw_bc", [128, 512])
        c.dma("sp", nw_bc[:], normw.partition_broadcast(128), writes=[nw_bc])
        wout_bf = s.sb("wout_bf", [128, 8, D], BF16); wq_bf = s.sb("wq_bf", [128, 8, D], BF16)
        wo_bf = s.sb("wo_bf", [128, 8, D], BF16)
        kT = s.sb("kT", [128, 8, MEM], BF16)
        vv = s.sb("vv", [128, 2, D], BF16)
        ptp = Pool(s, "ptp", [128, 8, 128], BF16, 2, ps=True)
        pmm = Pool(s, "pmm", [128, 512], F32, 4, ps=True)
        if moe:
            ident_f = make_ident(s, "ident_f", F32)
            ptf = [s.ps("ptf%d" % u, [128, 4, 128], F32) for u in range(2)]
            router_f = s.sb("router_f", [128, 8, NEXP])
            c.dma("sp", router_f[:], router.rearrange("(k p) e -> p k e", p=128), writes=[router_f])
        load_w_bf(c, wq_bf, wk)
        load_w_bf(c, wo_bf, wv)
        load_w_bf(c, wout_bf, w_out)
        memT = s.sb("memT", [128, 8, MEM], BF16)
        mt_p = Pool(s, "mt", [128, D], F32, 2)
        xt_p = Pool(s, "xt", [128, D], F32, 2)
        mb_p = Pool(s, "mb", [128, D], BF16, 2)
        for mc in range(2):
            m_f = mt_p.next(); m_b = mb_p.next()
            c.dma("sp", m_f[:], mem[mc * 128:(mc + 1) * 128, :], writes=[m_f])
            c.op("dve", lambda: nc.vector.tensor_copy(m_b[:], m_f[:]), [m_f], [m_b])
            tp = ptp.next()
            for k in range(8):
                c.tr(tp[:, k, :], m_b[:, k * 128:(k + 1) * 128], ident_bf[:], [m_b, ident_bf], [tp])
            c.op("act", lambda: nc.scalar.copy(memT[:, :, mc * 128:(mc + 1) * 128], tp[:]), [tp], [memT])
        for ch in range(8):
            pk = pmm.next()
            for k in range(8):
                c.mm(pk[:, 0:MEM], wq_bf[:, k, ch * 128:(ch + 1) * 128], memT[:, k, :], k == 0, k == 7, [wq_bf, memT], [pk])
            c.op("act", lambda: nc.scalar.copy(kT[:, ch, :], pk[:, 0:MEM]), [pk], [kT])
        for mc in range(2):
            for half in range(2):
                pv = pmm.next()
                for k in range(8):
                    c.mm(pv[:], memT[:, k, mc * 128:(mc + 1) * 128], wo_bf[:, k, half * 512:(half + 1) * 512], k == 0, k == 7, [wo_bf, memT], [pv])
                c.op("dve", lambda: nc.vector.tensor_copy(vv[:, mc, half * 512:(half + 1) * 512], pv[:]), [pv], [vv])
        load_w_bf(c, wq_bf, wq)
        load_w_bf(c, wo_bf, wo)

        mixn_p = Pool(s, "mixn", [128, D], BF16, 2)
        mixT_p = Pool(s, "mixT", [128, 8, 128], BF16, 2)
        ss_p = Pool(s, "ss", [128, 2], F32, 3)
        junk_p = [s.sb("junk%d" % i, [128, 256]) for i in range(2)]
        h_p = Pool(s, "h", [128, D], F32, 2)
        x1_p = [s.sb("x1_%d" % i, [128, D]) for i in range(4)]
        x1b_p = Pool(s, "x1b", [128, D], BF16, 2)
        x1T = s.sb("x1T", [128, 8, 512], BF16)
        qT = s.sb("qT", [128, 8, 512], BF16)
        pT_p = Pool(s, "pT", [128, 2, 512], BF16, 2)
        rden_p = Pool(s, "rden", [128, 512], F32, 2)
        oT = s.sb("oT", [128, 8, 512], BF16)
        x2_p = Pool(s, "x2", [128, D], F32, 2)
        x2T = s.sb("x2T", [128, 8, 512], BF16)
        if moe:
            x2Tf_p = Pool(s, "x2Tf", [128, 8, 128], F32, 2)
            lg_p = Pool(s, "lg", [128, 8], F32, 2); mx_p = Pool(s, "mx", [128, 8], F32, 2)
            nb_p = Pool(s, "nb", [128, 1], F32, 2); ee_p = Pool(s, "ee", [128, 8], F32, 2)
            mk_p = Pool(s, "mk", [128, 8], F32, 2); dn_p = Pool(s, "dn", [128, 1], F32, 2)

        for blk in range(NBLK):
            def phase1_tile(tt):
                ti = blk * 4 + tt
                r0 = ti * 128
                mt = mt_p.next(); xt = xt_p.next()
                c.dma("sp", mt[:], mix[r0:r0 + 128, :], writes=[mt])
                c.dma("sp", xt[:], x[r0:r0 + 128, :], writes=[xt])
                ss = ss_p.next(); mixn = mixn_p.next()
                yield
                for g in range(2):
                    c.act(junk_p[tt % 2][:], mt[:, g * 256:(g + 1) * 256], AF.Square, [mt], [junk_p[tt % 2], ss], accum_out=ss[:, g:g + 1])
                c.op("act", lambda: nc.scalar.copy(mixn[:, 512:1024], mt[:, 512:1024]), [mt], [mixn])
                yield
                c.act(ss[:], ss[:], AF.Sqrt, [ss], [ss], bias=eps_t[:, 0:1], scale=1.0 / 256)
                yield
                c.op("dve", lambda: nc.vector.reciprocal(ss[:], ss[:]), [ss], [ss])
                yield
                for g in range(2):
                    c.op("dve", lambda g=g: nc.vector.scalar_tensor_tensor(mixn[:, g * 256:(g + 1) * 256], mt[:, g * 256:(g + 1) * 256],
                                                                         ss[:, g:g + 1], nw_bc[:, g * 256:(g + 1) * 256], ALU.mult, ALU.mult),
                         [mt, ss, nw_bc], [mixn])
                yield
                tp = ptp.next()
                for k in range(8):
                    c.tr(tp[:, k, :], mixn[:, k * 128:(k + 1) * 128], ident_bf[:], [mixn, ident_bf], [tp])
                yield
                mixT = mixT_p.next()
                c.op("act", lambda: nc.scalar.copy(mixT[:], tp[:]), [tp], [mixT])
                yield
                h = h_p.next()
                pos = []
                for half in range(2):
                    po = pmm.next()
                    for k in range(8):
                        c.mm(po[:], mixT[:, k, :], wout_bf[:, k, half * 512:(half + 1) * 512], k == 0, k == 7, [mixT, wout_bf], [po])
                    pos.append(po)
                yield
                for half in range(2):
                    po = pos[half]
                    c.op("dve", lambda half=half, po=po: nc.vector.scalar_tensor_tensor(h[:, half * 512:(half + 1) * 512], xt[:, half * 512:(half + 1) * 512],
                                                                                 ALPHA, po[:], ALU.mult, ALU.add), [xt, po], [h])
                yield
                x1 = x1_p[tt]
                yield from layer_norm_gen(c, P, h, gbc[0], bbc[0], x1)
                x1b = x1b_p.next()
                c.op("act", lambda: nc.scalar.copy(x1b[:], x1[:]), [x1], [x1b])
                yield
                tp = ptp.next()
                for k in range(8):
                    c.tr(tp[:, k, :], x1b[:, k * 128:(k + 1) * 128], ident_bf[:], [x1b, ident_bf], [tp])
                yield
                c.op("act", lambda: nc.scalar.copy(x1T[:, :, tt * 128:(tt + 1) * 128], tp[:]), [tp], [x1T])
                yield

            for pair in range(2):
                run_interleaved([phase1_tile(2 * pair), phase1_tile(2 * pair + 1)])
            for ch in range(8):
                pq = pmm.next()
                for k in range(8):
                    c.mm(pq[:], wq_bf[:, k, ch * 128:(ch + 1) * 128], x1T[:, k, :], k == 0, k == 7, [wq_bf, x1T], [pq])
                if ch % 2 == 0:
                    c.op("act", lambda: nc.scalar.copy(qT[:, ch, :], pq[:]), [pq], [qT])
                else:
                    c.op("dve", lambda: nc.vector.tensor_copy(qT[:, ch, :], pq[:]), [pq], [qT])
            for hh in range(4):
                pT = pT_p.next()
                for mc in range(2):
                    psc = pmm.next()
                    for cc in range(2):
                        c.mm(psc[:], kT[:, 2 * hh + cc, mc * 128:(mc + 1) * 128], qT[:, 2 * hh + cc, :], cc == 0, cc == 1, [kT, qT], [psc])
                    c.act(pT[:, mc, :], psc[:], AF.Exp, [psc], [pT], scale=1.0 / 16.0)
                pden = pmm.next()
                for mc in range(2):
                    c.mm(pden[:], ones_bf[:], pT[:, mc, :], mc == 0, mc == 1, [ones_bf, pT], [pden])
                rden = rden_p.next()
                c.op("dve", lambda: nc.vector.reciprocal(rden[:], pden[:]), [pden], [rden])
                for cc in range(2):
                    pov = pmm.next()
                    for mc in range(2):
                        c.mm(pov[:], vv[:, mc, (2 * hh + cc) * 128:(2 * hh + cc + 1) * 128], pT[:, mc, :], mc == 0, mc == 1, [vv, pT], [pov])
                    c.op("dve", lambda cc=cc, pov=pov: nc.vector.tensor_tensor(oT[:, 2 * hh + cc, :], pov[:], rden[:], ALU.mult), [pov, rden], [oT])
            def phase3_tile(tt):
                ti = blk * 4 + tt
                r0 = ti * 128
                x1 = x1_p[tt]
                h = h_p.next()
                pos = []
                for half in range(2):
                    po = pmm.next()
                    for k in range(8):
                        c.mm(po[:], oT[:, k, tt * 128:(tt + 1) * 128], wo_bf[:, k, half * 512:(half + 1) * 512], k == 0, k == 7, [oT, wo_bf], [po])
                    pos.append(po)
                yield
                for half in range(2):
                    po = pos[half]
                    c.op("dve", lambda half=half, po=po: nc.vector.scalar_tensor_tensor(h[:, half * 512:(half + 1) * 512], x1[:, half * 512:(half + 1) * 512],
                                                                                 ALPHA, po[:], ALU.mult, ALU.add), [x1, po], [h])
                yield
                x2 = x2_p.next()
                yield from layer_norm_gen(c, P, h, gbc[1], bbc[1], x2)
                c.dma("sp", x2d[r0:r0 + 128, :], x2[:], reads=[x2], writes=[x2d_T])
                x2b = x1b_p.next()
                c.op("act", lambda: nc.scalar.copy(x2b[:], x2[:]), [x2], [x2b])
                if moe:
                    x2Tf = x2Tf_p.next()
                    for k4 in range(4):
                        c.tr(ptf[tt % 2][:, k4, :], x2[:, k4 * 128:(k4 + 1) * 128], ident_f[:], [x2, ident_f], [ptf[tt % 2]])
                yield
                tp = ptp.next()
                for k in range(8):
                    c.tr(tp[:, k, :], x2b[:, k * 128:(k + 1) * 128], ident_bf[:], [x2b, ident_bf], [tp])
                if moe:
                    c.op("act", lambda: nc.scalar.copy(x2Tf[:, 0:4, :], ptf[tt % 2][:]), [ptf[tt % 2]], [x2Tf])
                yield
                c.op("act", lambda: nc.scalar.copy(x2T[:, :, tt * 128:(tt + 1) * 128], tp[:]), [tp], [x2T])
                if moe:
                    for k4 in range(4):
                        c.tr(ptf[tt % 2][:, k4, :], x2[:, (4 + k4) * 128:(5 + k4) * 128], ident_f[:], [x2, ident_f], [ptf[tt % 2]])
                    yield
                    c.op("act", lambda: nc.scalar.copy(x2Tf[:, 4:8, :], ptf[tt % 2][:]), [ptf[tt % 2]], [x2Tf])
                    yield
                    pl = ptf[tt % 2]
                    for k in range(8):
                        c.mm(pl[:, 0, 0:NEXP], x2Tf[:, k, :], router_f[:, k, :], k == 0, k == 7, [x2Tf, router_f], [pl])
                    yield
                    lg = lg_p.next(); mx = mx_p.next(); nb = nb_p.next(); ee = ee_p.next(); mk = mk_p.next(); dn = dn_p.next()
                    c.op("act", lambda: nc.scalar.copy(lg[:], pl[:, 0, 0:NEXP]), [pl], [lg])
                    yield
                    c.op("dve", lambda: nc.vector.max(mx[:], lg[:]), [lg], [mx])
                    yield
                    c.op("dve", lambda: nc.vector.tensor_scalar(nb[:], mx[:, 0:1], -1.0, None, ALU.mult), [mx], [nb])
                    c.op("dve", lambda: nc.vector.tensor_scalar(mk[:], lg[:], mx[:, 1:2], None, ALU.is_ge), [lg, mx], [mk])
                    yield
                    c.act(ee[:], lg[:], AF.Exp, [lg, nb], [ee], bias=nb[:, 0:1], scale=1.0)
                    yield
                    c.op("dve", lambda: nc.vector.tensor_tensor(mk[:], mk[:], ee[:], ALU.mult), [mk, ee], [mk])
                    yield
                    c.op("dve", lambda: nc.vector.reduce_sum(dn[:], mk[:], axis=AX.X), [mk], [dn])
                    yield
                    c.op("dve", lambda: nc.vector.reciprocal(dn[:], dn[:]), [dn], [dn])
                    yield
                    c.op("dve", lambda: nc.vector.tensor_scalar(gates[:, ti, :], mk[:], dn[:, 0:1], None, ALU.mult), [mk, dn], [gates])
                yield

            for pair in range(2):
                run_interleaved([phase3_tile(2 * pair), phase3_tile(2 * pair + 1)])
            c.dma("sp", x2Td.rearrange("(k p) t -> p k t", p=128)[:, :, blk * 512:(blk + 1) * 512], x2T[:], reads=[x2T], writes=[x2Td_T])
        barrier(c)
        s.es.close()

    TB2 = min(TB2, NT)
    NB2 = NT // TB2
    NT2 = TB2 // 128
    NH2 = TB2 // 512
    s = Ctx.__new__(Ctx); s.__dict__.update(c.__dict__); s.es = ExitStack()
    load_ln(s, 2)
    x2Tb = s.sb("x2Tb", [128, 8, TB2], BF16)
    actT_raw = s.es.enter_context(nc.sbuf_tensor("actT", [128, NFF, TB2], BF16))
    actT = [T(actT_raw[:, f, :], "actT%d" % f) for f in range(NFF)]
    acc_raw = s.es.enter_context(nc.sbuf_tensor("acc", [128, NT2, D], F32))
    acc = [T(acc_raw[:, t, :], "acc%d" % t) for t in range(NT2)]
    w1g_p = Pool(s, "w1g", [128, 8, 256], BF16, 2); w3g_p = Pool(s, "w3g", [128, 8, 256], BF16, 2)
    w2q_p = Pool(s, "w2q", [128, NFF, 256], BF16, 2)
    ph1 = Pool(s, "ph1", [128, 512], F32, 2, ps=True); ph3 = Pool(s, "ph3", [128, 512], F32, 2, ps=True)
    pout = Pool(s, "pout", [128, 512], F32, 2, ps=True)
    ptp = Pool(s, "ptp2", [128, 8, 128], BF16, 2, ps=True)
    sil_p = Pool(s, "sil", [128, 512], F32, 2)
    x2l_p = Pool(s, "x2l", [128, D], F32, 2)
    h_p = Pool(s, "h2", [128, D], F32, 2)
    x3_p = Pool(s, "x3", [128, D], F32, 2)
    x3b_p = Pool(s, "x3b", [128, D], BF16, 2)
    x3T = s.sb("x3T", [128, 8, 512], BF16)
    for b2 in range(NB2):
        t0 = b2 * TB2
        c.dma("sp", x2Tb[:], x2Td.rearrange("(k p) t -> p k t", p=128)[:, :, t0:t0 + TB2], reads=[x2Td_T], writes=[x2Tb])
        for e in range(nexp):
            w1v = w1[e].rearrange("(k p) f -> p k f", p=128)
            w3v = w3[e].rearrange("(k p) f -> p k f", p=128)
            w2v = w2[e].rearrange("(f p) n -> p f n", p=128)
            for ffg in range(NFF // 2):
                w1g = w1g_p.next(); w3g = w3g_p.next()
                c.dma("pool", w1g[:], w1v[:, :, ffg * 256:(ffg + 1) * 256], writes=[w1g])
                c.dma("pool", w3g[:], w3v[:, :, ffg * 256:(ffg + 1) * 256], writes=[w3g])
                for fc in range(2):
                    f = ffg * 2 + fc
                    for tb in range(NH2):
                        p1 = ph1.next(); p3 = ph3.next()
                        for k in range(8):
                            c.mm(p1[:], w1g[:, k, fc * 128:(fc + 1) * 128], x2Tb[:, k, tb * 512:(tb + 1) * 512], k == 0, k == 7, [w1g, x2Tb], [p1])
                        for k in range(8):
                            c.mm(p3[:], w3g[:, k, fc * 128:(fc + 1) * 128], x2Tb[:, k, tb * 512:(tb + 1) * 512], k == 0, k == 7, [w3g, x2Tb], [p3])
                        sl = sil_p.next()
                        c.act(sl[:], p1[:], AF.Silu, [p1], [sl])
                        c.op("dve", lambda f=f, tb=tb, sl=sl, p3=p3: nc.vector.tensor_tensor(actT[f][:, tb * 512:(tb + 1) * 512], sl[:], p3[:], ALU.mult),
                             [sl, p3], [actT[f]])
            for qr in range(4):
                w2q = w2q_p.next()
                for f0 in range(0, NFF, 7):
                    c.dma("pool", w2q[:, f0:f0 + 7, :], w2v[:, f0:f0 + 7, qr * 256:(qr + 1) * 256], writes=[w2q])
                for t in range(NT2):
                    po = pout.next()
                    for f in range(NFF):
                        c.mm(po[:, 0:256], actT[f][:, t * 128:(t + 1) * 128], w2q[:, f, :], f == 0, f == NFF - 1, [actT[f], w2q], [po])
                    dst = acc[t][:, qr * 256:(qr + 1) * 256]
                    if not moe:
                        c.op("act", lambda dst=dst, po=po: nc.scalar.copy(dst, po[:, 0:256]), [po], [acc[t]])
                    else:
                        gt = gates[:, b2 * NT2 + t, e:e + 1]
                        if e == 0:
                            c.op("dve", lambda dst=dst, po=po, gt=gt: nc.vector.tensor_scalar(dst, po[:, 0:256], gt, None, ALU.mult), [po, gates], [acc[t]])
                        else:
                            c.op("dve", lambda dst=dst, po=po, gt=gt: nc.vector.scalar_tensor_tensor(dst, po[:, 0:256], gt, dst, ALU.mult, ALU.add), [po, gates, acc[t]], [acc[t]])
        def tail_tile(t):
            r0 = t0 + t * 128
            x2l = x2l_p.next()
            c.dma("sp", x2l[:], x2d[r0:r0 + 128, :], reads=[x2d_T], writes=[x2l])
            h = h_p.next()
            yield
            c.op("dve", lambda: nc.vector.scalar_tensor_tensor(h[:], x2l[:], ALPHA, acc[t][:], ALU.mult, ALU.add), [x2l, acc[t]], [h])
            yield
            x3 = x3_p.next()
            yield from layer_norm_gen(c, P, h, gbc[2], bbc[2], x3)
            c.dma("sp", xo[r0:r0 + 128, :], x3[:], reads=[x3])
            x3b = x3b_p.next()
            c.op("act", lambda: nc.scalar.copy(x3b[:], x3[:]), [x3], [x3b])
            yield
            tp = ptp.next()
            for k in range(8):
                c.tr(tp[:, k, :], x3b[:, k * 128:(k + 1) * 128], ident_bf[:], [x3b, ident_bf], [tp])
            yield
            tq = t % 4
            c.op("act", lambda: nc.scalar.copy(x3T[:, :, tq * 128:(tq + 1) * 128], tp[:]), [tp], [x3T])
            if tq == 3:
                cb = t0 + (t // 4) * 512
                c.dma("sp", xoT.rearrange("(k p) t -> p k t", p=128)[:, :, cb:cb + 512], x3T[:], reads=[x3T])
            yield

        for pair in range(NT2 // 2):
            run_interleaved([tail_tile(2 * pair), tail_tile(2 * pair + 1)])
    barrier(c)
    s.es.close()
    c.close()
    print("B: instructions", c.ninst, "waits", c.nwait)
    return nc

DO_SSD = True
DO_FOX = True
DO_POOL = True
STOP = 99
class StopBuild(Exception):
    pass
def ckpt(k):
    if k >= STOP:
        raise StopBuild()
TM = 9
DO_QF = True
DO_K = True
DO_PW = True

D = 1024
NFM = 704
NTM = 196
NEG = -30000.0


def consts_A():
    i = np.arange(128)
    same = (i[:, None] // 64) == (i[None, :] // 64)
    tri = ((i[:, None] <= i[None, :]) & same).astype(np.float32)
    blk = same.astype(np.float32)
    umask = ((i[:, None] > i[None, :]) & same).astype(np.float32)
    neg = np.where((i[:, None] <= i[None, :]) & same, 0.0, NEG).astype(np.float32)
    cm = np.stack([(i < 64), (i >= 64)], 1).astype(np.float32)
    cmask = np.concatenate([tri, blk, umask, neg, cm, np.ones((128, 128), np.float32)], 1)
    q = np.arange(512)
    dm = np.stack([np.where((jj * 128 + i[:, None]) <= q[None, :], 0.0, NEG) for jj in range(4)], 1).astype(np.float32)
    sel = np.zeros((6, 8), np.float32)
    sel[0, 0] = 1; sel[1, 1] = 1; sel[2, 2] = 1; sel[3:6, 3] = 1
    sel[3, 4] = -1; sel[4, 5] = -1; sel[5, 6] = -1; sel[0:3, 7] = 1
    return {"cmask": cmask, "dmask": dm, "sel": sel}


def build_A(TT):
    nc = bass.Bass("TRN2", target_bir_lowering=False)
    din = lambda name, shape, dt=F32: nc.dram_tensor(name, list(shape), dt, kind="ExternalInput").ap()
    xT = din("xT", [D, TT], BF16)
    w_fm = din("w_fm", [D, NFM]); w_tm = din("w_tm", [D, NTM])
    conv_w = din("conv_w", [128, 3, 4]); conv_b = din("conv_b", [128, 3])
    pp = din("pp", [8])
    pool_wb = din("pool_wb", [65, 64]); pool_scale = din("pool_scale", [64])
    pool_coef = din("pool_coef", [64, 4]); pool_fix = din("pool_fix", [64, 16])
    cmask_d = din("cmask", [128, 4 * 128 + 2 + 128]); dmask_d = din("dmask", [128, 4, 512]); sel_d = din("sel", [6, 8])
    y = nc.dram_tensor("y", [TT, 256], F32, kind="ExternalOutput").ap()

    c = Ctx(nc)
    NBLK = TT // 512
    NTILE = TT // 128

    ident_bf = make_ident(c, "ident_bf", BF16)
    ident_f = make_ident(c, "ident_f", F32)
    cm = c.sb("cm", [128, 4 * 128 + 2 + 128])
    c.dma("sp", cm[:], cmask_d, writes=[cm])
    TRI = cm[:, 0:128]; BLK = cm[:, 128:256]; UMASK = cm[:, 256:384]; NEGM = cm[:, 384:512]
    CMK = cm[:, 512:514]; ONES = cm[:, 514:642]
    dmask = c.sb("dmask_sb", [128, 4, 512], BF16)
    c.dma("pool", dmask[:], dmask_d, writes=[dmask])
    sel = c.sb("sel_sb", [6, 8])
    c.dma("sp", sel[:], sel_d, writes=[sel])
    wfm = c.sb("wfm", [128, 8, NFM], BF16); wtm = c.sb("wtm", [128, 8, NTM], BF16)
    c.dma("pool", wfm[:], w_fm.rearrange("(k p) n -> p k n", p=128), writes=[wfm])
    c.dma("pool", wtm[:], w_tm.rearrange("(k p) n -> p k n", p=128), writes=[wtm])
    cw = c.sb("cw", [128, 3, 4]); cb = c.sb("cb", [128, 3])
    c.dma("sp", cw[:], conv_w, writes=[cw]); c.dma("sp", cb[:], conv_b, writes=[cb])
    ppb = c.sb("ppb", [128, 8])
    c.dma("sp", ppb[:], pp.partition_broadcast(128), writes=[ppb])
    abc = c.sb("abc", [128, 2])
    c.act(abc[:], ppb[:, 2:4], AF.Exp, [ppb], [abc])
    c.op("dve", lambda: nc.vector.tensor_scalar(abc[:], abc[:], -1.0, None, ALU.mult), [abc], [abc])
    nfb = c.sb("nfb", [128, 1])
    c.op("dve", lambda: nc.vector.tensor_scalar(nfb[:], ppb[:, 6:7], -1.0, None, ALU.mult), [ppb], [nfb])
    dtbias4 = c.sb("dtbias4", [128, 4, 2])
    for tt in range(4):
        c.op("dve", lambda tt=tt: nc.vector.tensor_copy(dtbias4[:, tt, :], ppb[:, 0:2]), [ppb], [dtbias4])
    abc4 = c.sb("abc4", [128, 4, 2])
    for tt in range(4):
        c.op("dve", lambda tt=tt: nc.vector.tensor_copy(abc4[:, tt, :], abc[:]), [abc], [abc4])
    DI = [c.sb("DI%d" % h, [128, 128], BF16) for h in range(2)]
    for h in range(2):
        c.op("dve", lambda h=h: nc.vector.tensor_scalar(DI[h][:], ident_bf[:], ppb[:, 4 + h:5 + h], None, ALU.mult), [ident_bf, ppb], [DI[h]])
    one_t = c.sb("one_t", [128, 1])
    c.op("pool", lambda: nc.gpsimd.memset(one_t[:], 1.0), [], [one_t])
    pwb = c.sb("pwb", [65, 64]); psc = c.sb("psc", [65, 64]); pw_bf = c.sb("pw_bf", [65, 64], BF16)
    c.dma("sp", pwb[:], pool_wb, writes=[pwb])
    c.dma("sp", psc[:], pool_scale.partition_broadcast(65), writes=[psc])
    c.op("dve", lambda: nc.vector.tensor_tensor(pw_bf[:], pwb[:], psc[:], ALU.mult), [pwb, psc], [pw_bf])
    pcoef = c.sb("pcoef", [64, 4]); pfix = c.sb("pfix", [64, 16])
    c.dma("sp", pcoef[:], pool_coef, writes=[pcoef]); c.dma("sp", pfix[:], pool_fix, writes=[pfix])

    try:
      ckpt(1)
    except StopBuild:
      barrier(c); c.close(); return nc
    KT = c.sb("KT", [128, TT], BF16)
    VA = c.sb("VA", [128, NTILE, 66], BF16)
    c.op("pool", lambda: nc.gpsimd.memset(KT[:], 0.0), [], [KT])
    c.op("pool", lambda: nc.gpsimd.memset(VA[:], 1.0), [], [VA])
    state = c.sb("state", [128, 128])
    state_bf = [c.sb("state_bf%d" % i, [128, 128], BF16) for i in range(2)]
    c.op("dve", lambda: nc.vector.memset(state[:], 0.0), [], [state])
    c.op("dve", lambda: nc.vector.memset(state_bf[0][:], 0.0), [], [state_bf[0]])
    ccar = c.sb("ccar", [6, 1])
    c.op("dve", lambda: nc.vector.memset(ccar[:], 0.0), [], [ccar])
    U = [c.sb("U%d" % g, [128, 3 + 512]) for g in range(3)]
    for g in range(3):
        c.op("pool", lambda g=g: nc.gpsimd.memset(U[g][:], 0.0), [], [U[g]])
    PU = c.sb("PU", [64, 16 + 512])
    c.op("pool", lambda: nc.gpsimd.memset(PU[:], 0.0), [], [PU])

    try:
      ckpt(2)
    except StopBuild:
      barrier(c); c.close(); return nc
    xT_p = Pool(c, "xTb", [128, 8, 512], BF16, 2)
    pst = Pool(c, "pst", [128, 512], F32, 2, ps=True)
    ppro = c.ps("ppro", [128, 512], F32)

    class _One:
        def next(self):
            return ppro
    pfm = _One()
    pacc = c.ps("pacc", [128, 512], F32)
    tA = [c.ps("tA%d" % u, [128, 512], F32) for u in range(2)]
    tB = [c.ps("tB%d" % u, [128, 512], F32) for u in range(2)]
    cacc3 = [c.sb("cacc%d" % g, [128, 512]) for g in range(3)]
    zsb_p = Pool(c, "zsb", [128, 4, 128], F32, 2)
    fmT = [Pool(c, "fmT%d" % g, [128, 512], BF16, 2) for g in range(3)]
    QT_p = Pool(c, "QT", [128, 512], BF16, 2)
    for qq in QT_p.ts:
        c.op("pool", lambda qq=qq: nc.gpsimd.memset(qq[:], 0.0), [], [qq])
    f6_p = Pool(c, "f6", [6, 512], F32, 2); lf_p = Pool(c, "lf", [6, 512], F32, 2); cc_p = Pool(c, "cc", [6, 512], F32, 2)
    ones6 = c.sb("ones6", [6, 512])
    c.op("pool", lambda: nc.gpsimd.memset(ones6[:], 1.0), [], [ones6])
    hi_p = Pool(c, "hi", [6, 512], BF16, 2); mid_p = Pool(c, "mid", [6, 512], BF16, 2); lo_p = Pool(c, "lo", [6, 512], BF16, 2)
    r1_p = Pool(c, "r1", [6, 512], F32, 2); r2_p = Pool(c, "r2", [6, 512], F32, 2)
    aq_p = Pool(c, "aq", [6, 512], F32, 2); ak_p = Pool(c, "ak", [6, 512], F32, 2)
    ztmb_p = Pool(c, "ztmb", [128, 4, 128], F32, 2); dtrb_p = Pool(c, "dtrb", [128, 4, 2], F32, 2); dtb_p = Pool(c, "dtb", [128, 4, 2], F32, 2)
    smb_p = Pool(c, "smb", [128, 32], F32, 2); exb_p = Pool(c, "exb", [128, 32], F32, 2)
    Ab_p = Pool(c, "Ab", [128, 4, 2], F32, 2); A4b_p = Pool(c, "A4b", [128, 4, 4], F32, 2)
    dtdb_p = Pool(c, "dtdb", [128, 4, 2], F32, 2)
    xstm_p = Pool(c, "xstm", [128, 128], BF16, 2); btm_p = Pool(c, "btm", [128, 128], BF16, 2)
    X_p = Pool(c, "X", [128, 128], BF16, 2); Xd_p = Pool(c, "Xd", [128, 128], BF16, 2)
    UA_p = Pool(c, "UA", [128, 128], F32, 4); L_p = Pool(c, "L", [128, 128], F32, 4); MT_p = Pool(c, "MT", [128, 128], BF16, 4)
    pysb_p = Pool(c, "pysb", [128, 128], F32, 2); t1_p = Pool(c, "t1", [128, 128], F32, 2)
    zs_p = Pool(c, "zs", [128, 128], F32, 2)
    yo_p = Pool(c, "yo", [128, 256], F32, 8)
    PT_p = Pool(c, "PT", [128, 512], BF16, 3)
    osb_p = Pool(c, "osb", [65, 512], F32, 2); pfs_p = Pool(c, "pfs", [128, 260], F32, 2); rd_p = Pool(c, "rd", [128, 4], F32, 2)
    ps2 = [c.sb("ps%d" % i, [64, 16 + 512]) for i in range(4)]
    pmean = c.sb("pmean", [64, 512]); ptmp = c.sb("ptmp", [64, 512]); paug_p = Pool(c, "paug", [65, 512], BF16, 2)
    for pa in paug_p.ts:
        c.op("pool", lambda pa=pa: nc.gpsimd.memset(pa[:], 1.0), [], [pa])

    xTv = xT.rearrange("(k p) t -> p k t", p=128)
    sbf_i = 0

    BC = {}

    def prologue(blk):
        nonlocal xb_next
        t0 = blk * 512
        if blk == 0:
            xb_next = xT_p.next()
            c.dma("sp", xb_next[:], xTv[:, :, 0:512], writes=[xb_next])
        xb = xb_next
        if blk + 1 < NBLK:
            xb_next = xT_p.next()
            c.dma("sp", xb_next[:], xTv[:, :, t0 + 512:t0 + 1024], writes=[xb_next])
        for g in range(3):
            c.op("pool", lambda g=g: nc.gpsimd.tensor_copy(U[g][:, 0:3], U[g][:, 512:515]), [U[g]], [U[g]])
            pg = pfm.next()
            for k in range(8):
                c.mm(pg[:], wfm[:, k, g * 128:(g + 1) * 128], xb[:, k, :], k == 0, k == 7, [wfm, xb], [pg])
            c.op("act", lambda g=g, pg=pg: nc.scalar.copy(U[g][:, 3:515], pg[:]), [pg], [U[g]])
            ca = cacc3[g]
            c.act(ca[:], U[g][:, 0:512], AF.Identity, [U[g], cw, cb], [ca], bias=cb[:, g:g + 1], scale=cw[:, g, 0:1])
            for kk in range(1, 4):
                c.op("dve", lambda g=g, kk=kk: nc.vector.scalar_tensor_tensor(ca[:], U[g][:, kk:kk + 512], cw[:, g, kk:kk + 1], ca[:], ALU.mult, ALU.add),
                     [U[g], cw, ca], [ca])
            yield
        if DO_QF:
            yield
            QT = QT_p.next()
            pg = pfm.next()
            for k in range(8):
                c.mm(pg[:], wfm[:, k, 384:512], xb[:, k, :], k == 0, k == 7, [wfm, xb], [pg])
            c.op("act", lambda: nc.scalar.copy(QT[64:128, :], pg[64:128, :]), [pg], [QT])
            yield
            f6 = f6_p.next(); lf = lf_p.next(); cc = cc_p.next()
            c.act(f6[:], pg[0:6, :], AF.Exp, [pg, nfb], [f6], bias=nfb[0:6, 0:1], scale=-1.0)
            c.act(lf[:], f6[:], AF.Ln, [f6, one_t], [lf], bias=one_t[0:6, 0:1], scale=1.0)
            c.op("dve", lambda: nc.vector.tensor_tensor_scan(cc[:], ones6[:], lf[:], ccar[:, 0:1], ALU.mult, ALU.subtract), [ones6, lf, ccar], [cc])
            c.op("dve", lambda: nc.vector.tensor_copy(ccar[:], cc[:, 511:512]), [cc], [ccar])
            yield
            hi = hi_p.next(); mid = mid_p.next(); lo = lo_p.next(); r1 = r1_p.next(); r2 = r2_p.next()
            c.op("act", lambda: nc.scalar.copy(hi[:], cc[:]), [cc], [hi])
            c.op("dve", lambda: nc.vector.tensor_tensor(r1[:], cc[:], hi[:], ALU.subtract), [cc, hi], [r1])
            c.op("act", lambda: nc.scalar.copy(mid[:], r1[:]), [r1], [mid])
            c.op("dve", lambda: nc.vector.tensor_tensor(r2[:], r1[:], mid[:], ALU.subtract), [r1, mid], [r2])
            c.op("act", lambda: nc.scalar.copy(lo[:], r2[:]), [r2], [lo])
            yield
            aq = aq_p.next(); ak = ak_p.next()
            for (dst, co, final) in ((aq, 0, QT[0:6, :]), (ak, 4, KT[0:6, t0:t0 + 512])):
                c.op("dve", lambda dst=dst, co=co: nc.vector.tensor_scalar(dst[:], hi[:], sel[:, co:co + 1], sel[:, (3 if co == 0 else 7):(4 if co == 0 else 8)], ALU.mult, ALU.add), [hi, sel], [dst])
                c.op("dve", lambda dst=dst, co=co: nc.vector.scalar_tensor_tensor(dst[:], mid[:], sel[:, co + 1:co + 2], dst[:], ALU.mult, ALU.add), [mid, sel, dst], [dst])
                tgt = QT if co == 0 else KT
                c.op("dve", lambda dst=dst, co=co, final=final: nc.vector.scalar_tensor_tensor(final, lo[:], sel[:, co + 2:co + 3], dst[:], ALU.mult, ALU.add), [lo, sel, dst], [tgt])
        if DO_K:
            yield
            pg = pfm.next()
            for k in range(8):
                c.mm(pg[:], wfm[:, k, 512:640], xb[:, k, :], k == 0, k == 7, [wfm, xb], [pg])
            c.act(KT[64:128, t0:t0 + 512], pg[64:128, :], AF.Identity, [pg], [KT], scale=0.125)
        if DO_PW:
            yield
            c.op("pool", lambda: nc.gpsimd.tensor_copy(PU[:, 0:16], PU[:, 512:528]), [PU], [PU])
            pg = pfm.next()
            for k in range(8):
                c.mm(pg[0:64, :], wfm[:, k, 640:704], xb[:, k, :], k == 0, k == 7, [wfm, xb], [pg])
            c.op("act", lambda: nc.scalar.copy(PU[:, 16:528], pg[0:64, :]), [pg], [PU])
            yield
            srcs = [PU] + ps2
            for lv in range(4):
                sh = 1 << lv
                lo_i = 2 * sh - 1
                src = srcs[lv]; dstt = ps2[lv]
                c.op("pool", lambda src=src, dstt=dstt, sh=sh, lo_i=lo_i: nc.gpsimd.tensor_tensor(dstt[:, lo_i:528], src[:, lo_i:528], src[:, lo_i - sh:528 - sh], ALU.add), [src], [dstt])
            yield
            c.op("dve", lambda: nc.vector.tensor_scalar(pmean[:], ps2[0][:, 16:528], pcoef[:, 0:1], None, ALU.mult), [ps2[0], pcoef], [pmean])
            for lv in range(1, 4):
                c.op("dve", lambda lv=lv: nc.vector.scalar_tensor_tensor(pmean[:], ps2[lv][:, 16:528], pcoef[:, lv:lv + 1], pmean[:], ALU.mult, ALU.add), [ps2[lv], pcoef, pmean], [pmean])
            yield
            if blk == 0:
                c.op("pool", lambda: nc.gpsimd.tensor_tensor(pmean[:, 0:16], pmean[:, 0:16], pfix[:], ALU.mult), [pmean, pfix], [pmean])
            paug = paug_p.next()
            c.op("pool", lambda: nc.gpsimd.tensor_tensor(paug[0:64, :], pmean[:], PU[:, 16:528], ALU.subtract), [pmean, PU], [paug])

        ztmb = ztmb_p.next(); dtrb = dtrb_p.next(); dtb = dtb_p.next()
        for tt in range(4):
            ti = blk * 4 + tt
            cs = slice(tt * 128, (tt + 1) * 128)
            ptm = pfm.next()
            for k in range(8):
                c.mm(ptm[:, 0:NTM], xb[:, k, cs], wtm[:, k, :], k == 0, k == 7, [xb, wtm], [ptm])
            c.op("act", lambda: nc.scalar.copy(ztmb[:, tt, :], ptm[:, 0:128]), [ptm], [ztmb])
            if TM >= 2: c.op("act", lambda: nc.scalar.copy(VA[:, ti, 0:64], ptm[:, 128:192]), [ptm], [VA])
            if TM >= 3: c.op("act", lambda: nc.scalar.copy(dtrb[:, tt, :], ptm[:, 192:194]), [ptm], [dtrb])
            yield
        yield
        fts = []
        for g in range(3):
            ft = fmT[g].next()
            c.act(ft[:], cacc3[g][:], AF.Silu, [cacc3[g]], [ft])
            fts.append(ft)
        xsT, BT, CT = fts
        zsb = zsb_p.next()
        c.act(zsb[:], ztmb[:], AF.Silu, [ztmb], [zsb])
        yield
        if TM >= 3: c.op("dve", lambda: nc.vector.tensor_tensor(dtrb[:], dtrb[:], dtbias4[:], ALU.add), [dtrb, dtbias4], [dtrb])
        if TM >= 4: c.act(dtrb[:], dtrb[:], AF.Exp, [dtrb], [dtrb])
        if TM >= 5: c.act(dtb[:], dtrb[:], AF.Ln, [dtrb, one_t], [dtb], bias=one_t[:, 0:1], scale=1.0)
        Ab = Ab_p.next(); A4b = A4b_p.next()
        c.op("dve", lambda: nc.vector.tensor_tensor(Ab[:], dtb[:], abc4[:], ALU.mult), [dtb, abc4], [Ab])
        for ck in range(2):
            c.op("dve", lambda ck=ck: nc.vector.tensor_scalar(A4b[:, :, ck * 2:ck * 2 + 2], Ab[:], CMK[:, ck:ck + 1], None, ALU.mult), [Ab, cm], [A4b])
        yield
        smb = smb_p.next(); exb = exb_p.next(); dtdb = dtdb_p.next()
        c.mm(ppro[:, 0:8], TRI, Ab[:].rearrange("p a b -> p (a b)"), True, True, [cm, Ab], [ppro])
        c.mm(ppro[:, 8:16], BLK, Ab[:].rearrange("p a b -> p (a b)"), True, True, [cm, Ab], [ppro])
        c.mm(ppro[:, 16:32], ONES, A4b[:].rearrange("p a b -> p (a b)"), True, True, [cm, A4b], [ppro])
        c.op("act", lambda: nc.scalar.copy(smb[:], ppro[:, 0:32]), [ppro], [smb])
        yield
        c.op("dve", lambda: nc.vector.tensor_tensor(smb[:, 8:16], smb[:, 8:16], smb[:, 0:8], ALU.subtract), [smb], [smb])
        yield
        c.act(exb[:], smb[:], AF.Exp, [smb], [exb])
        yield
        c.op("dve", lambda: nc.vector.tensor_tensor(dtdb[:].rearrange("p a b -> p (a b)"), dtb[:].rearrange("p a b -> p (a b)"), exb[:, 8:16], ALU.mult), [dtb, exb], [dtdb])
        BC[blk] = (xsT, BT, CT, QT, paug, ztmb, dtb, Ab, A4b, exb, dtdb, zsb)
        yield

    def tiles_fox(blk):
        t0 = blk * 512
        xsT, BT, CT, QT, paug, ztmb, dtb, Ab, A4b, exb, dtdb, zsb = BC.pop(blk)
        nkt = 4 * blk + 4
        def fox_steps():
            prev = None
            for j in range(nkt + 1):
                cur = None
                if j < nkt:
                    ps_ = pst.next()
                    diag = j >= 4 * blk
                    c.mm(ps_[:], KT[:, j * 128:(j + 1) * 128], QT[:], True, not diag, [KT, QT], [ps_])
                    if diag:
                        c.mm(ps_[:], ident_bf[:], dmask[:, j - 4 * blk, :], False, True, [ident_bf, dmask], [ps_])
                    cur = (j, ps_)
                if prev is not None:
                    pj, pps = prev
                    PT = PT_p.next()
                    c.act(PT[:], pps[:], AF.Exp, [pps], [PT])
                    c.mm(pacc[0:65, :], VA[:, pj, 0:65], PT[:], pj == 0, pj == nkt - 1, [VA, PT], [pacc])
                prev = cur
                yield
        fsteps = fox_steps()
        per_tile = (nkt + 1 + 3) // 4

        yos = []

        def ssd_tile(tt):
            nonlocal sbf_i
            u = tt % 2
            bA = tA[u]; bB = tB[u]
            cs = slice(tt * 128, (tt + 1) * 128)
            ztm = ztmb[:, tt, :]; dt_ = dtb[:, tt, :]
            yo = yo_p.next()
            yos.append(yo)
            A_ = Ab[:, tt, :]
            dtd = dtdb[:, tt, :]
            UAs = []
            for h in range(2):
                UA = UA_p.next()
                c.act(UA[:], UMASK, AF.Identity, [cm, Ab], [UA], scale=A_[:, h:h + 1])
                UAs.append(UA)
            ptb = bA[:].bitcast(BF16)
            c.tr(ptb[:, 0:128], xsT[:, cs], ident_bf[:], [xsT, ident_bf], [bA])
            c.tr(ptb[:, 128:256], BT[:, cs], ident_bf[:], [BT, ident_bf], [bA])
            c.mm(bB[:, 0:128], BT[:, cs], CT[:, cs], True, True, [BT, CT], [bB])
            yield
            xstm = xstm_p.next(); btm = btm_p.next()
            c.op("act", lambda: nc.scalar.copy(xstm[:], ptb[:, 0:128]), [bA], [xstm])
            c.op("act", lambda: nc.scalar.copy(btm[:], ptb[:, 128:256]), [bA], [btm])
            X = X_p.next(); Xd = Xd_p.next()
            for h in range(2):
                hs = slice(h * 64, (h + 1) * 64)
                c.act(X[:, hs], ptb[:, hs], AF.Identity, [bA, dtb], [X], scale=dt_[:, h:h + 1])
            for h in range(2):
                hs = slice(h * 64, (h + 1) * 64)
                c.act(Xd[:, hs], ptb[:, hs], AF.Identity, [bA, dtdb], [Xd], scale=dtd[:, h:h + 1])
            for h in range(2):
                c.mm(bB[:, 128 * (h + 1):128 * (h + 2)], UAs[h][:], TRI, True, False, [UAs[h], cm], [bB])
                c.mm(bB[:, 128 * (h + 1):128 * (h + 2)], ident_f[:], NEGM, False, True, [ident_f, cm], [bB])
            yield
            Ls = []
            for h in range(2):
                L = L_p.next()
                c.act(L[:], bB[:, 128 * (h + 1):128 * (h + 2)], AF.Exp, [bB], [L])
                Ls.append(L)
            yield
            MTs = []
            for h in range(2):
                MT = MT_p.next()
                c.op("dve", lambda h=h, MT=MT: nc.vector.tensor_tensor(MT[:], Ls[h][:], bB[:, 0:128], ALU.mult), [Ls[h], bB], [MT])
                MTs.append(MT)
            yield
            for h in range(2):
                hs = slice(h * 64, (h + 1) * 64)
                c.mm(bA[:, hs], MTs[h][:], X[:, hs], True, False, [MTs[h], X], [bA])
                c.mm(bA[:, hs], DI[h][:], xstm[:, hs], False, True, [DI[h], xstm], [bA])
            yield
            for ck in range(2):
                rs_ = slice(ck * 64, (ck + 1) * 64)
                lcs = slice(tt * 128 + ck * 64, tt * 128 + (ck + 1) * 64)
                sb_cur = state_bf[sbf_i]
                c.mm(bA[:, 256 + ck * 128:256 + (ck + 1) * 128], btm[rs_, :], Xd[rs_, :], True, True, [btm, Xd], [bA])
                c.mm(bA[rs_, 128:256], CT[:, lcs], sb_cur[:], True, True, [CT, sb_cur], [bA])
                for h in range(2):
                    hs = slice(h * 64, (h + 1) * 64)
                    c.op("dve", lambda h=h, hs=hs, ck=ck: nc.vector.scalar_tensor_tensor(state[:, hs], state[:, hs], exb[:, 16 + tt * 4 + ck * 2 + h:17 + tt * 4 + ck * 2 + h],
                                                                                         bA[:, 256 + ck * 128 + h * 64:256 + ck * 128 + (h + 1) * 64], ALU.mult, ALU.add),
                         [state, exb, bA], [state])
                sbf_i = 1 - sbf_i
                nb_ = state_bf[sbf_i]
                c.op("act", lambda nb_=nb_: nc.scalar.copy(nb_[:], state[:]), [state], [nb_])
            yield
            pysb = pysb_p.next(); t1 = t1_p.next()
            c.op("act", lambda: nc.scalar.copy(pysb[:], bA[:, 0:128]), [bA], [pysb])
            c.mm(bB[:, 392:456], paug[0:65, cs], pw_bf[:], True, True, [paug, pw_bf], [bB])
            yield
            for h in range(2):
                hs = slice(h * 64, (h + 1) * 64)
                c.op("dve", lambda h=h, hs=hs: nc.vector.scalar_tensor_tensor(t1[:, hs], bA[:, 128 + h * 64:128 + (h + 1) * 64], exb[:, tt * 2 + h:tt * 2 + h + 1], pysb[:, hs], ALU.mult, ALU.add),
                     [bA, exb, pysb], [t1])
            c.op("act", lambda: nc.scalar.copy(yo[:, 192:256], bB[:, 392:456]), [bB], [yo])
            yield
            c.op("pool", lambda: nc.gpsimd.tensor_tensor(yo[:, 0:128], t1[:], zsb[:, tt, :], ALU.mult), [t1, zsb], [yo])
            yield

        NROUND = 10
        fox_per_round = (nkt + 1 + 2 * NROUND - 1) // (2 * NROUND)
        for pair in range(2):
            gens = [ssd_tile(2 * pair), ssd_tile(2 * pair + 1)]
            while gens:
                for g in list(gens):
                    try:
                        next(g)
                    except StopIteration:
                        gens.remove(g)
                for _ in range(fox_per_round):
                    next(fsteps, None)
                yield
        for _ in fsteps:
            yield
        if DO_FOX:
            osb = osb_p.next(); rd = rd_p.next()
            c.op("act", lambda: nc.scalar.copy(osb[:], pacc[0:65, :]), [pacc], [osb])
            pf = tB[0]
            for tt in range(4):
                c.tr(pf[:, tt * 65:(tt + 1) * 65], osb[:, tt * 128:(tt + 1) * 128], ident_f[0:65, 0:65], [osb, ident_f], [pf])
            pfs = pfs_p.next()
            c.op("act", lambda: nc.scalar.copy(pfs[:], pf[:, 0:260]), [pf], [pfs])
            for tt in range(4):
                c.op("dve", lambda tt=tt: nc.vector.reciprocal(rd[:, tt:tt + 1], pfs[:, tt * 65 + 64:tt * 65 + 65]), [pfs], [rd])
        for tt in range(4):
            yo = yos[tt]
            if DO_FOX: c.op("dve", lambda tt=tt, yo=yo: nc.vector.tensor_scalar(yo[:, 128:192], pfs[:, tt * 65:tt * 65 + 64], rd[:, tt:tt + 1], None, ALU.mult), [pfs, rd], [yo])
            r0 = t0 + tt * 128
            c.dma("sp", y[r0:r0 + 128, :], yo[:], reads=[yo])
        yield

    xb_next = None
    run_interleaved([prologue(0)])
    for blk in range(NBLK):
        gens = [tiles_fox(blk)]
        if blk + 1 < NBLK:
            gens.append(prologue(blk + 1))
        run_interleaved(gens)
    barrier(c)
    c.close()
    print("A: instructions", c.ninst, "waits", c.nwait)
    return nc


def prep_A(w_in, conv_w, conv_b, dt_bias, a_log, d_skip, f_bias, pool_w, pool_b, pool_scale, j):
    g = j // 2
    cz = slice(128 * j, 128 * j + 128)
    cxs = slice(512 + 128 * j, 512 + 128 * j + 128)
    cB = slice(1024 + 128 * g, 1024 + 128 * g + 128)
    cC = slice(1280 + 128 * g, 1280 + 128 * g + 128)
    cdt = slice(1536 + 2 * j, 1536 + 2 * j + 2)
    cq = slice(1544 + 64 * j, 1544 + 64 * j + 64)
    ck = slice(1800 + 64 * j, 1800 + 64 * j + 64)
    cv = slice(2056 + 64 * j, 2056 + 64 * j + 64)
    cf = 2312 + j
    cp = slice(2316 + 64 * j, 2316 + 64 * j + 64)
    w_fm = np.zeros((D, NFM), np.float32)
    w_fm[:, 0:128] = w_in[:, cxs]; w_fm[:, 128:256] = w_in[:, cB]; w_fm[:, 256:384] = w_in[:, cC]
    for r in range(6):
        w_fm[:, 384 + r] = w_in[:, cf]
    w_fm[:, 384 + 64:384 + 128] = w_in[:, cq]
    w_fm[:, 512 + 64:512 + 128] = w_in[:, ck]
    w_fm[:, 640:704] = w_in[:, cp]
    w_tm = np.concatenate([w_in[:, cz], w_in[:, cv], w_in[:, cdt], w_in[:, cf:cf + 1], np.zeros((D, 1), np.float32)], 1).astype(np.float32)
    chans = [np.arange(128 * j, 128 * j + 128), np.arange(512 + 128 * g, 512 + 128 * g + 128), np.arange(768 + 128 * g, 768 + 128 * g + 128)]
    cw = np.stack([conv_w[:, ch].T for ch in chans], 1).astype(np.float32)
    cbb = np.stack([conv_b[ch] for ch in chans], 1).astype(np.float32)
    pp = np.zeros(8, np.float32)
    pp[0:2] = dt_bias[2 * j:2 * j + 2]; pp[2:4] = a_log[2 * j:2 * j + 2]; pp[4:6] = d_skip[2 * j:2 * j + 2]; pp[6] = f_bias[j]
    wb = np.concatenate([pool_w[j], pool_b[j][None, :]], 0).astype(np.float32)
    win = (2, 4, 8, 16)[j]
    coef = np.zeros((64, 4), np.float32); coef[:, j] = 1.0 / win
    fix = np.ones((64, 16), np.float32)
    tpos = np.arange(16)
    fix[:, :] = (win / np.minimum(tpos + 1, win))[None, :]
    return {"w_fm": w_fm, "w_tm": w_tm, "conv_w": np.ascontiguousarray(cw), "conv_b": np.ascontiguousarray(cbb), "pp": pp,
            "pool_wb": wb, "pool_scale": np.ascontiguousarray(pool_scale[64 * j:64 * j + 64]).astype(np.float32),
            "pool_coef": coef, "pool_fix": fix}


def build_P(NT):
    nc = bass.Bass("TRN2", target_bir_lowering=False)
    x = nc.dram_tensor("x", [NT, D], F32, kind="ExternalInput").ap()
    xT = nc.dram_tensor("xT", [D, NT], BF16, kind="ExternalOutput").ap()
    c = Ctx(nc)
    ident_bf = make_ident(c, "ident_bf", BF16)
    xt_p = Pool(c, "xt", [128, D], F32, 3); xb_p = Pool(c, "xb", [128, D], BF16, 2)
    ptp = Pool(c, "ptp", [128, 8, 128], BF16, 2, ps=True)
    xTs_p = Pool(c, "xTs", [128, 8, 512], BF16, 2)
    for blk in range(NT // 512):
        xTs = xTs_p.next()
        for tt in range(4):
            r0 = blk * 512 + tt * 128
            xt = xt_p.next(); xb = xb_p.next()
            c.dma("sp", xt[:], x[r0:r0 + 128, :], writes=[xt])
            c.op("dve", lambda: nc.vector.tensor_copy(xb[:], xt[:]), [xt], [xb])
            tp = ptp.next()
            for k in range(8):
                c.tr(tp[:, k, :], xb[:, k * 128:(k + 1) * 128], ident_bf[:], [xb, ident_bf], [tp])
            c.op("act", lambda: nc.scalar.copy(xTs[:, :, tt * 128:(tt + 1) * 128], tp[:]), [tp], [xTs])
        c.dma("sp", xT.rearrange("(k p) t -> p k t", p=128)[:, :, blk * 512:(blk + 1) * 512], xTs[:], reads=[xTs])
    barrier(c)
    c.close()
    return nc


from concourse.bass_utils import run_bass_kernel_spmd

NCORES = 8
SEQ = 16384
BATCH = 2
NTOK = BATCH * SEQ // NCORES
DEPTH = 4
_CACHE = {}


def _prog(key, fn):
    if key not in _CACHE:
        _CACHE[key] = fn()
    return _CACHE[key]


def _run(nc, in_maps):
    res = run_bass_kernel_spmd(nc, in_maps, core_ids=list(range(NCORES)))
    return res.results


def kernel(**inp):
    f32 = lambda a: np.ascontiguousarray(np.asarray(a, dtype=np.float32))
    x = f32(inp["x"]); mem = f32(inp["mem"])
    xs = x.reshape(NCORES, NTOK, D)
    ncP = _prog("P", lambda: build_P(NTOK))
    resP = _run(ncP, [{"x": xs[c]} for c in range(NCORES)])
    xT_parts = [r["xT"] for r in resP]
    x_cur = [xs[c] for c in range(NCORES)]
    ncA = _prog("A", lambda: build_A(SEQ))
    consts = consts_A()
    for layer in range(DEPTH):
        moe = layer % 2 == 1
        jj = layer // 2
        xT_full = [np.ascontiguousarray(np.concatenate(xT_parts[b * 4:(b + 1) * 4], axis=1)) for b in range(BATCH)]
        in_maps = []
        for c in range(NCORES):
            b, j = divmod(c, 4)
            m = prep_A(f32(inp["w_in"][layer]), f32(inp["ssm_conv_w"][layer]), f32(inp["ssm_conv_b"][layer]),
                       f32(inp["ssm_dt_bias"][layer]), f32(inp["ssm_a_log"][layer]), f32(inp["ssm_d"][layer]),
                       f32(inp["fox_f_bias"][layer]), f32(inp["pool_w"][layer]), f32(inp["pool_b"][layer]),
                       f32(inp["pool_scale"][layer]), j)
            m.update(consts)
            m["xT"] = xT_full[b]
            in_maps.append(m)
        resA = _run(ncA, in_maps)
        mix = np.empty((BATCH, SEQ, D), np.float32)
        for c in range(NCORES):
            b, j = divmod(c, 4)
            y = resA[c]["y"]
            mix[b, :, 128 * j:128 * j + 128] = y[:, 0:128]
            mix[b, :, 512 + 64 * j:512 + 64 * j + 64] = y[:, 128:192]
            mix[b, :, 768 + 64 * j:768 + 64 * j + 64] = y[:, 192:256]
        mixs = mix.reshape(NCORES, NTOK, D)
        ncB = _prog("B%d" % moe, lambda: build_B(NTOK, moe))
        wts = {"normw": f32(inp["ssm_norm_w"][layer]), "w_out": f32(inp["w_out"][layer]),
               "ln1_g": f32(inp["ln1_g"][layer]), "ln1_b": f32(inp["ln1_b"][layer]),
               "ln2_g": f32(inp["ln2_g"][layer]), "ln2_b": f32(inp["ln2_b"][layer]),
               "ln3_g": f32(inp["ln3_g"][layer]), "ln3_b": f32(inp["ln3_b"][layer]),
               "wq": f32(inp["xa_wq"][layer]), "wk": f32(inp["xa_wk"][layer]),
               "wv": f32(inp["xa_wv"][layer]), "wo": f32(inp["xa_wo"][layer])}
        if moe:
            wts.update({"router": f32(inp["router_w"][jj]), "w1": f32(inp["moe_w1"][jj]),
                        "w3": f32(inp["moe_w3"][jj]), "w2": f32(inp["moe_w2"][jj])})
        else:
            wts.update({"w1": f32(inp["ffn_w1"][jj:jj + 1]), "w3": f32(inp["ffn_w3"][jj:jj + 1]),
                        "w2": f32(inp["ffn_w2"][jj:jj + 1])})
        in_maps = []
        for c in range(NCORES):
            m = dict(wts)
            m["mix"] = np.ascontiguousarray(mixs[c]); m["x"] = np.ascontiguousarray(x_cur[c])
            m["mem"] = mem[c // 4]
            in_maps.append(m)
        resB = _run(ncB, in_maps)
        x_cur = [r["xo"] for r in resB]
        xT_parts = [r["xoT"] for r in resB]
    return np.stack(x_cur).reshape(BATCH, SEQ, D).astype(np.float32)
```

```python
from contextlib import ExitStack
import numpy as np
import concourse.bass as bass
import concourse.mybir as mybir

F32 = mybir.dt.float32
BF16 = mybir.dt.bfloat16
AF = mybir.ActivationFunctionType
ALU = mybir.AluOpType
AX = mybir.AxisListType

ENGS = ["pe", "act", "dve", "pool", "sp"]
NDS = 40


class T:
    __slots__ = ("t", "w", "r", "name")

    def __init__(self, t, name=""):
        self.t = t
        self.w = None
        self.r = {}
        self.name = name

    def __getitem__(self, k):
        return self.t[k]


class Ctx:
    LIMIT = 30000
    NDMAX = 128

    def __init__(self, nc):
        self.nc = nc
        self.es = ExitStack()
        self.eng = {"pe": nc.tensor, "act": nc.scalar, "dve": nc.vector,
                    "pool": nc.gpsimd, "sp": nc.sync}
        nd = self.NDMAX
        self.sems = [self.es.enter_context(nc.semaphore("d%d" % i)) for i in range(NDS)]
        self.mult = [16] * NDS
        self.cnt = [0] * nd
        self.snap = [dict() for _ in range(nd)]
        self.known = {k: np.zeros(nd, np.int64) for k in ENGS}
        self.cur = {}
        self.old = {k: [] for k in ENGS}
        self.edims = {k: set() for k in ENGS}
        for k in ENGS:
            self._new_dim(k)
        self.rr = 0
        self.nwait = 0
        self.ninst = 0

    def _new_dim(self, e):
        d = len(self.sems)
        assert d < self.NDMAX
        self.sems.append(self.es.enter_context(self.nc.semaphore("c_%s_%d" % (e, d))))
        self.mult.append(1)
        if e in self.cur:
            self.old[e].append(self.cur[e])
        self.cur[e] = d
        self.edims[e].add(d)

    def sb(self, name, shape, dt=F32):
        return T(self.es.enter_context(self.nc.sbuf_tensor(name, list(shape), dt)), name)

    def ps(self, name, shape, dt=F32):
        return T(self.es.enter_context(self.nc.psum_tensor(name, list(shape), dt)), name)

    def close(self):
        self.es.close()

    def _wait(self, e, dim, c):
        kn = self.known[e]
        if kn[dim] >= c:
            return
        self.eng[e].wait_ge(self.sems[dim], int(c) * self.mult[dim])
        self.nwait += 1
        s = self.snap[dim].get(c)
        if s is not None:
            np.maximum(kn, s, out=kn)
        kn[dim] = max(kn[dim], c)

    def _deps(self, e, reads, writes, pe_acc=False):
        need = {}
        for t in reads:
            if t.w is not None:
                d, c = t.w
                if need.get(d, 0) < c:
                    need[d] = c
        for t in writes:
            if t.w is not None:
                d, c = t.w
                if not (pe_acc and d in self.edims["pe"]):
                    if need.get(d, 0) < c:
                        need[d] = c
            for d, c in t.r.items():
                if need.get(d, 0) < c:
                    need[d] = c
        for d, c in need.items():
            self._wait(e, d, c)

    def _mark(self, dim, c, reads, writes):
        for t in reads:
            if t.r.get(dim, 0) < c:
                t.r[dim] = c
        for t in writes:
            t.w = (dim, c)
            t.r = {}

    def op(self, e, fn, reads=(), writes=(), pe_acc=False):
        self._deps(e, reads, writes, pe_acc)
        ins = fn()
        if self.cnt[self.cur[e]] >= self.LIMIT:
            self._new_dim(e)
        d = self.cur[e]
        self.cnt[d] += 1
        c = self.cnt[d]
        ins.then_inc(self.sems[d], 1)
        s = self.known[e].copy()
        s[d] = c
        for od in self.old[e]:
            s[od] = self.cnt[od]
        self.snap[d][c] = s
        self._mark(d, c, reads, writes)
        self.ninst += 1
        return ins

    def dma(self, q, out, in_, reads=(), writes=(), **kw):
        self._deps(q, reads, writes)
        kn = self.known[q]
        pick = None
        for k in range(NDS):
            i = (self.rr + k) % NDS
            if kn[i] >= self.cnt[i]:
                pick = i
                break
        if pick is None:
            pick = self.rr % NDS
            self._wait(q, pick, self.cnt[pick])
        self.rr = (pick + 1) % NDS
        d = pick
        ins = self.eng[q].dma_start(out=out, in_=in_, **kw)
        self.cnt[d] += 1
        c = self.cnt[d]
        ins.then_inc(self.sems[d], 16)
        s = kn.copy()
        s[d] = c
        self.snap[d][c] = s
        self._mark(d, c, reads, writes)
        self.ninst += 1
        return ins

    def finish(self, e="sp"):
        for d in range(len(self.sems)):
            if self.cnt[d] > 0:
                self._wait(e, d, self.cnt[d])

    def mm(self, out, lhsT, rhs, start, stop, reads, writes):
        nc = self.nc
        return self.op("pe", lambda: nc.tensor.matmul(out, lhsT, rhs, start=start, stop=stop),
                       reads, writes, pe_acc=not start)

    def tr(self, out, in_, ident, reads, writes):
        nc = self.nc
        return self.op("pe", lambda: nc.tensor.transpose(out, in_, ident), reads, writes)

    def act(self, out, in_, func, reads, writes, **kw):
        nc = self.nc
        return self.op("act", lambda: nc.scalar.activation(out, in_, func, **kw), reads, writes)


D = 1024
DFF = 3584
NFF = DFF // 128
MEM = 256
ALPHA = 8.0 ** 0.25
LN_EPS = 1e-5
RMS_EPS = 1e-5
NEXP = 8


class Pool:
    def __init__(self, c, name, shape, dt, n, ps=False):
        self.ts = [(c.ps if ps else c.sb)("%s%d" % (name, i), shape, dt) for i in range(n)]
        self.i = 0

    def next(self):
        t = self.ts[self.i % len(self.ts)]
        self.i += 1
        return t


def make_ident(c, name, dt):
    nc = c.nc
    t = c.sb(name, [128, 128], dt)
    c.op("pool", lambda: nc.gpsimd.memset(t[:], 1.0), [], [t])
    c.op("pool", lambda: nc.gpsimd.affine_select(t[:], t[:], pattern=[[-1, 128]], compare_op=ALU.is_equal,
                                                 fill=0.0, base=0, channel_multiplier=1), [t], [t])
    return t


def barrier(c):
    for e in ENGS:
        c.finish(e)


def layer_norm_gen(c, P, h, g_bc, b_bc, out, out_bf=None):
    nc = c.nc
    st = P["st"].next(); mv = P["mv"].next(); rs = P["rs"].next()
    c.op("dve", lambda: nc.vector.bn_stats(st[:, 0, :], h[:, 0:512]), [h], [st])
    c.op("dve", lambda: nc.vector.bn_stats(st[:, 1, :], h[:, 512:1024]), [h], [st])
    yield
    c.op("dve", lambda: nc.vector.bn_aggr(mv[:], st[:].rearrange("p a b -> p (a b)")), [st], [mv])
    yield
    c.act(rs[:], mv[:, 1:2], AF.Sqrt, [mv, P["eps"]], [rs], bias=P["eps"][:, 0:1], scale=1.0)
    yield
    c.op("dve", lambda: nc.vector.reciprocal(rs[:], rs[:]), [rs], [rs])
    yield
    tmp = P["lnt"].next()
    c.op("dve", lambda: nc.vector.tensor_scalar(tmp[:], h[:], mv[:, 0:1], rs[:, 0:1], ALU.subtract, ALU.mult), [h, mv, rs], [tmp])
    yield
    c.op("dve", lambda: nc.vector.tensor_tensor(tmp[:], tmp[:], g_bc[:], ALU.mult), [tmp, g_bc], [tmp])
    yield
    c.op("dve", lambda: nc.vector.tensor_tensor(out[:], tmp[:], b_bc[:], ALU.add), [tmp, b_bc], [out])
    if out_bf is not None:
        c.op("dve", lambda: nc.vector.tensor_tensor(out_bf[:], tmp[:], b_bc[:], ALU.add), [tmp, b_bc], [out_bf])
    yield


def layer_norm(c, P, h, g_bc, b_bc, out):
    for _ in layer_norm_gen(c, P, h, g_bc, b_bc, out):
        pass


def run_interleaved(gens):
    gens = list(gens)
    while gens:
        for g in list(gens):
            try:
                next(g)
            except StopIteration:
                gens.remove(g)


def load_w_bf(c, dst, src_ap, q="pool"):
    v = src_ap.rearrange("(k p) n -> p k n", p=128)
    for k in range(8):
        c.dma(q, dst[:, k, :], v[:, k, :], writes=[dst])


def build_B(NT, moe, TB2=1024):
    nc = bass.Bass("TRN2", target_bir_lowering=False)
    dt_in = lambda name, shape, dt=F32: nc.dram_tensor(name, list(shape), dt, kind="ExternalInput").ap()
    mix = dt_in("mix", [NT, D]); x = dt_in("x", [NT, D]); mem = dt_in("mem", [MEM, D])
    normw = dt_in("normw", [512]); w_out = dt_in("w_out", [D, D])
    ln_g = [dt_in("ln%d_g" % i, [D]) for i in (1, 2, 3)]
    ln_b = [dt_in("ln%d_b" % i, [D]) for i in (1, 2, 3)]
    wq = dt_in("wq", [D, D]); wk = dt_in("wk", [D, D]); wv = dt_in("wv", [D, D]); wo = dt_in("wo", [D, D])
    if moe:
        router = dt_in("router", [D, NEXP])
        w1 = dt_in("w1", [NEXP, D, DFF]); w3 = dt_in("w3", [NEXP, D, DFF]); w2 = dt_in("w2", [NEXP, DFF, D])
    else:
        w1 = dt_in("w1", [1, D, DFF]); w3 = dt_in("w3", [1, D, DFF]); w2 = dt_in("w2", [1, DFF, D])
    xo = nc.dram_tensor("xo", [NT, D], F32, kind="ExternalOutput").ap()
    xoT = nc.dram_tensor("xoT", [D, NT], BF16, kind="ExternalOutput").ap()
    x2d = nc.dram_tensor("x2d", [NT, D], F32, kind="Internal").ap()
    x2Td = nc.dram_tensor("x2Td", [D, NT], BF16, kind="Internal").ap()
    x2d_T = T(x2d, "x2d"); x2Td_T = T(x2Td, "x2Td")

    c = Ctx(nc)
    NTILE = NT // 128
    NBLK = NT // 512
    nexp = NEXP if moe else 1

    ident_bf = make_ident(c, "ident_bf", BF16)
    ones_bf = c.sb("ones_bf", [128, 128], BF16)
    c.op("pool", lambda: nc.gpsimd.memset(ones_bf[:], 1.0), [], [ones_bf])
    gbc = [None] * 3; bbc = [None] * 3
    def load_ln(sc, i):
        gbc[i] = sc.sb("g%d" % i, [128, D]); bbc[i] = sc.sb("b%d" % i, [128, D])
        c.dma("sp", gbc[i][:], ln_g[i].partition_broadcast(128), writes=[gbc[i]])
        c.dma("sp", bbc[i][:], ln_b[i].partition_broadcast(128), writes=[bbc[i]])
    eps_t = c.sb("eps_t", [128, 1])
    c.op("pool", lambda: nc.gpsimd.memset(eps_t[:], LN_EPS), [], [eps_t])
    gates = c.sb("gates", [128, NTILE, NEXP]) if moe else None
    P = {"st": Pool(c, "st", [128, 2, 6], F32, 3), "mv": Pool(c, "mv", [128, 2], F32, 3),
         "rs": Pool(c, "rs", [128, 1], F32, 3), "nm": Pool(c, "nm", [128, 1], F32, 3),
         "lnt": Pool(c, "lnt", [128, D], F32, 2), "eps": eps_t}

    c1 = Ctx.__new__(Ctx); c1.__dict__.update(c.__dict__); c1.es = ExitStack()
    if True:
        s = c1
        load_ln(s, 0); load_ln(s, 1)
        nw_bc = s.sb("nw_bc", [128, 512])
        c.dma("sp", nw_bc[:], normw.partition_broadcast(128), writes=[nw_bc])
        wout_bf = s.sb("wout_bf", [128, 8, D], BF16); wq_bf = s.sb("wq_bf", [128, 8, D], BF16)
        wo_bf = s.sb("wo_bf", [128, 8, D], BF16)
        kT = s.sb("kT", [128, 8, MEM], BF16)
        vv = s.sb("vv", [128, 2, D], BF16)
        ptp = Pool(s, "ptp", [128, 8, 128], BF16, 2, ps=True)
        pmm = Pool(s, "pmm", [128, 512], F32, 4, ps=True)
        if moe:
            ident_f = make_ident(s, "ident_f", F32)
            ptf = [s.ps("ptf%d" % u, [128, 4, 128], F32) for u in range(2)]
            router_f = s.sb("router_f", [128, 8, NEXP])
            c.dma("sp", router_f[:], router.rearrange("(k p) e -> p k e", p=128), writes=[router_f])
        load_w_bf(c, wq_bf, wk)
        load_w_bf(c, wo_bf, wv)
        load_w_bf(c, wout_bf, w_out)
        memT = s.sb("memT", [128, 8, MEM], BF16)
        mt_p = Pool(s, "mt", [128, D], F32, 2)
        xt_p = Pool(s, "xt", [128, D], F32, 2)
        mb_p = Pool(s, "mb", [128, D], BF16, 2)
        for mc in range(2):
            m_f = mt_p.next(); m_b = mb_p.next()
            c.dma("sp", m_f[:], mem[mc * 128:(mc + 1) * 128, :], writes=[m_f])
            c.op("dve", lambda: nc.vector.tensor_copy(m_b[:], m_f[:]), [m_f], [m_b])
            tp = ptp.next()
            for k in range(8):
                c.tr(tp[:, k, :], m_b[:, k * 128:(k + 1) * 128], ident_bf[:], [m_b, ident_bf], [tp])
            c.op("act", lambda: nc.scalar.copy(memT[:, :, mc * 128:(mc + 1) * 128], tp[:]), [tp], [memT])
        for ch in range(8):
            pk = pmm.next()
            for k in range(8):
                c.mm(pk[:, 0:MEM], wq_bf[:, k, ch * 128:(ch + 1) * 128], memT[:, k, :], k == 0, k == 7, [wq_bf, memT], [pk])
            c.op("act", lambda: nc.scalar.copy(kT[:, ch, :], pk[:, 0:MEM]), [pk], [kT])
        for mc in range(2):
            for half in range(2):
                pv = pmm.next()
                for k in range(8):
                    c.mm(pv[:], memT[:, k, mc * 128:(mc + 1) * 128], wo_bf[:, k, half * 512:(half + 1) * 512], k == 0, k == 7, [wo_bf, memT], [pv])
                c.op("dve", lambda: nc.vector.tensor_copy(vv[:, mc, half * 512:(half + 1) * 512], pv[:]), [pv], [vv])
        load_w_bf(c, wq_bf, wq)
        load_w_bf(c, wo_bf, wo)

        mixn_p = Pool(s, "mixn", [128, D], BF16, 2)
        mixT_p = Pool(s, "mixT", [128, 8, 128], BF16, 2)
        ss_p = Pool(s, "ss", [128, 2], F32, 3)
        junk_p = [s.sb("junk%d" % i, [128, 256]) for i in range(2)]
        h_p = Pool(s, "h", [128, D], F32, 2)
        x1_p = [s.sb("x1_%d" % i, [128, D]) for i in range(4)]
        x1b_p = Pool(s, "x1b", [128, D], BF16, 2)
        x1T = s.sb("x1T", [128, 8, 512], BF16)
        qT = s.sb("qT", [128, 8, 512], BF16)
        pT_p = Pool(s, "pT", [128, 2, 512], BF16, 2)
        rden_p = Pool(s, "rden", [128, 512], F32, 2)
        oT = s.sb("oT", [128, 8, 512], BF16)
        x2_p = Pool(s, "x2", [128, D], F32, 2)
        x2T = s.sb("x2T", [128, 8, 512], BF16)
        if moe:
            x2Tf_p = Pool(s, "x2Tf", [128, 8, 128], F32, 2)
            lg_p = Pool(s, "lg", [128, 8], F32, 2); mx_p = Pool(s, "mx", [128, 8], F32, 2)
            nb_p = Pool(s, "nb", [128, 1], F32, 2); ee_p = Pool(s, "ee", [128, 8], F32, 2)
            mk_p = Pool(s, "mk", [128, 8], F32, 2); dn_p = Pool(s, "dn", [128, 1], F32, 2)

        for blk in range(NBLK):
            def phase1_tile(tt):
                ti = blk * 4 + tt
                r0 = ti * 128
                mt = mt_p.next(); xt = xt_p.next()
                c.dma("sp", mt[:], mix[r0:r0 + 128, :], writes=[mt])
                c.dma("sp", xt[:], x[r0:r0 + 128, :], writes=[xt])
                ss = ss_p.next(); mixn = mixn_p.next()
                yield
                for g in range(2):
                    c.act(junk_p[tt % 2][:], mt[:, g * 256:(g + 1) * 256], AF.Square, [mt], [junk_p[tt % 2], ss], accum_out=ss[:, g:g + 1])
                c.op("act", lambda: nc.scalar.copy(mixn[:, 512:1024], mt[:, 512:1024]), [mt], [mixn])
                yield
                c.act(ss[:], ss[:], AF.Sqrt, [ss], [ss], bias=eps_t[:, 0:1], scale=1.0 / 256)
                yield
                c.op("dve", lambda: nc.vector.reciprocal(ss[:], ss[:]), [ss], [ss])
                yield
                for g in range(2):
                    c.op("dve", lambda g=g: nc.vector.scalar_tensor_tensor(mixn[:, g * 256:(g + 1) * 256], mt[:, g * 256:(g + 1) * 256],
                                                                         ss[:, g:g + 1], nw_bc[:, g * 256:(g + 1) * 256], ALU.mult, ALU.mult),
                         [mt, ss, nw_bc], [mixn])
                yield
                tp = ptp.next()
                for k in range(8):
                    c.tr(tp[:, k, :], mixn[:, k * 128:(k + 1) * 128], ident_bf[:], [mixn, ident_bf], [tp])
                yield
                mixT = mixT_p.next()
                c.op("act", lambda: nc.scalar.copy(mixT[:], tp[:]), [tp], [mixT])
                yield
                h = h_p.next()
                pos = []
                for half in range(2):
                    po = pmm.next()
                    for k in range(8):
                        c.mm(po[:], mixT[:, k, :], wout_bf[:, k, half * 512:(half + 1) * 512], k == 0, k == 7, [mixT, wout_bf], [po])
                    pos.append(po)
                yield
                for half in range(2):
                    po = pos[half]
                    c.op("dve", lambda half=half, po=po: nc.vector.scalar_tensor_tensor(h[:, half * 512:(half + 1) * 512], xt[:, half * 512:(half + 1) * 512],
                                                                                 ALPHA, po[:], ALU.mult, ALU.add), [xt, po], [h])
                yield
                x1 = x1_p[tt]
                x1b = x1b_p.next()
                yield from layer_norm_gen(c, P, h, gbc[0], bbc[0], x1, x1b)
                tp = ptp.next()
                for k in range(8):
                    c.tr(tp[:, k, :], x1b[:, k * 128:(k + 1) * 128], ident_bf[:], [x1b, ident_bf], [tp])
                yield
                c.op("act", lambda: nc.scalar.copy(x1T[:, :, tt * 128:(tt + 1) * 128], tp[:]), [tp], [x1T])
                yield

            for pair in range(2):
                run_interleaved([phase1_tile(2 * pair), phase1_tile(2 * pair + 1)])
            for ch in range(8):
                pq = pmm.next()
                for k in range(8):
                    c.mm(pq[:], wq_bf[:, k, ch * 128:(ch + 1) * 128], x1T[:, k, :], k == 0, k == 7, [wq_bf, x1T], [pq])
                if ch % 2 == 0:
                    c.op("act", lambda: nc.scalar.copy(qT[:, ch, :], pq[:]), [pq], [qT])
                else:
                    c.op("dve", lambda: nc.vector.tensor_copy(qT[:, ch, :], pq[:]), [pq], [qT])
            for hh in range(4):
                pT = pT_p.next()
                for mc in range(2):
                    psc = pmm.next()
                    for cc in range(2):
                        c.mm(psc[:], kT[:, 2 * hh + cc, mc * 128:(mc + 1) * 128], qT[:, 2 * hh + cc, :], cc == 0, cc == 1, [kT, qT], [psc])
                    c.act(pT[:, mc, :], psc[:], AF.Exp, [psc], [pT], scale=1.0 / 16.0)
                pden = pmm.next()
                for mc in range(2):
                    c.mm(pden[:], ones_bf[:], pT[:, mc, :], mc == 0, mc == 1, [ones_bf, pT], [pden])
                rden = rden_p.next()
                c.op("dve", lambda: nc.vector.reciprocal(rden[:], pden[:]), [pden], [rden])
                for cc in range(2):
                    pov = pmm.next()
                    for mc in range(2):
                        c.mm(pov[:], vv[:, mc, (2 * hh + cc) * 128:(2 * hh + cc + 1) * 128], pT[:, mc, :], mc == 0, mc == 1, [vv, pT], [pov])
                    c.op("dve", lambda cc=cc, pov=pov: nc.vector.tensor_tensor(oT[:, 2 * hh + cc, :], pov[:], rden[:], ALU.mult), [pov, rden], [oT])
            def phase3_tile(tt):
                ti = blk * 4 + tt
                r0 = ti * 128
                x1 = x1_p[tt]
                h = h_p.next()
                pos = []
                for half in range(2):
                    po = pmm.next()
                    for k in range(8):
                        c.mm(po[:], oT[:, k, tt * 128:(tt + 1) * 128], wo_bf[:, k, half * 512:(half + 1) * 512], k == 0, k == 7, [oT, wo_bf], [po])
                    pos.append(po)
                yield
                for half in range(2):
                    po = pos[half]
                    c.op("dve", lambda half=half, po=po: nc.vector.scalar_tensor_tensor(h[:, half * 512:(half + 1) * 512], x1[:, half * 512:(half + 1) * 512],
                                                                                 ALPHA, po[:], ALU.mult, ALU.add), [x1, po], [h])
                yield
                x2 = x2_p.next()
                yield from layer_norm_gen(c, P, h, gbc[1], bbc[1], x2)
                c.dma("sp", x2d[r0:r0 + 128, :], x2[:], reads=[x2], writes=[x2d_T])
                x2b = x1b_p.next()
                c.op("act", lambda: nc.scalar.copy(x2b[:], x2[:]), [x2], [x2b])
                if moe:
                    x2Tf = x2Tf_p.next()
                    for k4 in range(4):
                        c.tr(ptf[tt % 2][:, k4, :], x2[:, k4 * 128:(k4 + 1) * 128], ident_f[:], [x2, ident_f], [ptf[tt % 2]])
                yield
                tp = ptp.next()
                for k in range(8):
                    c.tr(tp[:, k, :], x2b[:, k * 128:(k + 1) * 128], ident_bf[:], [x2b, ident_bf], [tp])
                if moe:
                    c.op("act", lambda: nc.scalar.copy(x2Tf[:, 0:4, :], ptf[tt % 2][:]), [ptf[tt % 2]], [x2Tf])
                yield
                c.op("act", lambda: nc.scalar.copy(x2T[:, :, tt * 128:(tt + 1) * 128], tp[:]), [tp], [x2T])
                if moe:
                    for k4 in range(4):
                        c.tr(ptf[tt % 2][:, k4, :], x2[:, (4 + k4) * 128:(5 + k4) * 128], ident_f[:], [x2, ident_f], [ptf[tt % 2]])
                    yield
                    c.op("act", lambda: nc.scalar.copy(x2Tf[:, 4:8, :], ptf[tt % 2][:]), [ptf[tt % 2]], [x2Tf])
                    yield
                    pl = ptf[tt % 2]
                    for k in range(8):
                        c.mm(pl[:, 0, 0:NEXP], x2Tf[:, k, :], router_f[:, k, :], k == 0, k == 7, [x2Tf, router_f], [pl])
                    yield
                    lg = lg_p.next(); mx = mx_p.next(); nb = nb_p.next(); ee = ee_p.next(); mk = mk_p.next(); dn = dn_p.next()
                    c.op("act", lambda: nc.scalar.copy(lg[:], pl[:, 0, 0:NEXP]), [pl], [lg])
                    yield
                    c.op("dve", lambda: nc.vector.max(mx[:], lg[:]), [lg], [mx])
                    yield
                    c.op("dve", lambda: nc.vector.tensor_scalar(nb[:], mx[:, 0:1], -1.0, None, ALU.mult), [mx], [nb])
                    c.op("dve", lambda: nc.vector.tensor_scalar(mk[:], lg[:], mx[:, 1:2], None, ALU.is_ge), [lg, mx], [mk])
                    yield
                    c.act(ee[:], lg[:], AF.Exp, [lg, nb], [ee], bias=nb[:, 0:1], scale=1.0)
                    yield
                    c.op("dve", lambda: nc.vector.tensor_tensor(mk[:], mk[:], ee[:], ALU.mult), [mk, ee], [mk])
                    yield
                    c.op("dve", lambda: nc.vector.reduce_sum(dn[:], mk[:], axis=AX.X), [mk], [dn])
                    yield
                    c.op("dve", lambda: nc.vector.reciprocal(dn[:], dn[:]), [dn], [dn])
                    yield
                    c.op("dve", lambda: nc.vector.tensor_scalar(gates[:, ti, :], mk[:], dn[:, 0:1], None, ALU.mult), [mk, dn], [gates])
                yield

            for pair in range(2):
                run_interleaved([phase3_tile(2 * pair), phase3_tile(2 * pair + 1)])
            c.dma("sp", x2Td.rearrange("(k p) t -> p k t", p=128)[:, :, blk * 512:(blk + 1) * 512], x2T[:], reads=[x2T], writes=[x2Td_T])
        barrier(c)
        s.es.close()

    TB2 = min(TB2, NT)
    NB2 = NT // TB2
    NT2 = TB2 // 128
    NH2 = TB2 // 512
    s = Ctx.__new__(Ctx); s.__dict__.update(c.__dict__); s.es = ExitStack()
    load_ln(s, 2)
    x2Tb = s.sb("x2Tb", [128, 8, TB2], BF16)
    actT_raw = s.es.enter_context(nc.sbuf_tensor("actT", [128, NFF, TB2], BF16))
    actT = [T(actT_raw[:, f, :], "actT%d" % f) for f in range(NFF)]
    acc_raw = s.es.enter_context(nc.sbuf_tensor("acc", [128, NT2, D], F32))
    acc = [T(acc_raw[:, t, :], "acc%d" % t) for t in range(NT2)]
    w1g_p = Pool(s, "w1g", [128, 8, 256], BF16, 2); w3g_p = Pool(s, "w3g", [128, 8, 256], BF16, 2)
    w2q_p = Pool(s, "w2q", [128, NFF, 256], BF16, 2)
    ph1 = Pool(s, "ph1", [128, 512], F32, 2, ps=True); ph3 = Pool(s, "ph3", [128, 512], F32, 2, ps=True)
    pout = Pool(s, "pout", [128, 512], F32, 2, ps=True)
    ptp = Pool(s, "ptp2", [128, 8, 128], BF16, 2, ps=True)
    sil_p = Pool(s, "sil", [128, 512], F32, 2)
    x2l_p = Pool(s, "x2l", [128, D], F32, 2)
    h_p = Pool(s, "h2", [128, D], F32, 2)
    x3_p = Pool(s, "x3", [128, D], F32, 2)
    x3b_p = Pool(s, "x3b", [128, D], BF16, 2)
    x3T = s.sb("x3T", [128, 8, 512], BF16)
    for b2 in range(NB2):
        t0 = b2 * TB2
        c.dma("sp", x2Tb[:], x2Td.rearrange("(k p) t -> p k t", p=128)[:, :, t0:t0 + TB2], reads=[x2Td_T], writes=[x2Tb])
        for e in range(nexp):
            w1v = w1[e].rearrange("(k p) f -> p k f", p=128)
            w3v = w3[e].rearrange("(k p) f -> p k f", p=128)
            w2v = w2[e].rearrange("(f p) n -> p f n", p=128)
            for ffg in range(NFF // 2):
                w1g = w1g_p.next(); w3g = w3g_p.next()
                c.dma("pool", w1g[:], w1v[:, :, ffg * 256:(ffg + 1) * 256], writes=[w1g])
                c.dma("pool", w3g[:], w3v[:, :, ffg * 256:(ffg + 1) * 256], writes=[w3g])
                for fc in range(2):
                    f = ffg * 2 + fc
                    for tb in range(NH2):
                        p1 = ph1.next(); p3 = ph3.next()
                        for k in range(8):
                            c.mm(p1[:], w1g[:, k, fc * 128:(fc + 1) * 128], x2Tb[:, k, tb * 512:(tb + 1) * 512], k == 0, k == 7, [w1g, x2Tb], [p1])
                        for k in range(8):
                            c.mm(p3[:], w3g[:, k, fc * 128:(fc + 1) * 128], x2Tb[:, k, tb * 512:(tb + 1) * 512], k == 0, k == 7, [w3g, x2Tb], [p3])
                        sl = sil_p.next()
                        c.act(sl[:], p1[:], AF.Silu, [p1], [sl])
                        c.op("dve", lambda f=f, tb=tb, sl=sl, p3=p3: nc.vector.tensor_tensor(actT[f][:, tb * 512:(tb + 1) * 512], sl[:], p3[:], ALU.mult),
                             [sl, p3], [actT[f]])
            for qr in range(4):
                w2q = w2q_p.next()
                for f0 in range(0, NFF, 7):
                    c.dma("pool", w2q[:, f0:f0 + 7, :], w2v[:, f0:f0 + 7, qr * 256:(qr + 1) * 256], writes=[w2q])
                for t in range(NT2):
                    po = pout.next()
                    for f in range(NFF):
                        c.mm(po[:, 0:256], actT[f][:, t * 128:(t + 1) * 128], w2q[:, f, :], f == 0, f == NFF - 1, [actT[f], w2q], [po])
                    dst = acc[t][:, qr * 256:(qr + 1) * 256]
                    if not moe:
                        c.op("act", lambda dst=dst, po=po: nc.scalar.copy(dst, po[:, 0:256]), [po], [acc[t]])
                    else:
                        gt = gates[:, b2 * NT2 + t, e:e + 1]
                        if e == 0:
                            c.op("dve", lambda dst=dst, po=po, gt=gt: nc.vector.tensor_scalar(dst, po[:, 0:256], gt, None, ALU.mult), [po, gates], [acc[t]])
                        else:
                            c.op("dve", lambda dst=dst, po=po, gt=gt: nc.vector.scalar_tensor_tensor(dst, po[:, 0:256], gt, dst, ALU.mult, ALU.add), [po, gates, acc[t]], [acc[t]])
        def tail_tile(t):
            r0 = t0 + t * 128
            x2l = x2l_p.next()
            c.dma("sp", x2l[:], x2d[r0:r0 + 128, :], reads=[x2d_T], writes=[x2l])
            h = h_p.next()
            yield
            c.op("dve", lambda: nc.vector.scalar_tensor_tensor(h[:], x2l[:], ALPHA, acc[t][:], ALU.mult, ALU.add), [x2l, acc[t]], [h])
            yield
            x3 = x3_p.next()
            yield from layer_norm_gen(c, P, h, gbc[2], bbc[2], x3)
            c.dma("sp", xo[r0:r0 + 128, :], x3[:], reads=[x3])
            x3b = x3b_p.next()
            c.op("act", lambda: nc.scalar.copy(x3b[:], x3[:]), [x3], [x3b])
            yield
            tp = ptp.next()
            for k in range(8):
                c.tr(tp[:, k, :], x3b[:, k * 128:(k + 1) * 128], ident_bf[:], [x3b, ident_bf], [tp])
            yield
            tq = t % 4
            c.op("act", lambda: nc.scalar.copy(x3T[:, :, tq * 128:(tq + 1) * 128], tp[:]), [tp], [x3T])
            if tq == 3:
                cb = t0 + (t // 4) * 512
                c.dma("sp", xoT.rearrange("(k p) t -> p k t", p=128)[:, :, cb:cb + 512], x3T[:], reads=[x3T])
            yield

        for pair in range(NT2 // 2):
            run_interleaved([tail_tile(2 * pair), tail_tile(2 * pair + 1)])
    barrier(c)
    s.es.close()
    c.close()
    print("B: instructions", c.ninst, "waits", c.nwait)
    return nc

DO_SSD = True
DO_FOX = True
DO_POOL = True
STOP = 99
class StopBuild(Exception):
    pass
def ckpt(k):
    if k >= STOP:
        raise StopBuild()
TM = 9
DO_QF = True
DO_K = True
DO_PW = True

D = 1024
NFM = 704
NTM = 196
NEG = -30000.0


def consts_A():
    i = np.arange(128)
    same = (i[:, None] // 64) == (i[None, :] // 64)
    tri = ((i[:, None] <= i[None, :]) & same).astype(np.float32)
    blk = same.astype(np.float32)
    umask = ((i[:, None] > i[None, :]) & same).astype(np.float32)
    neg = np.where((i[:, None] <= i[None, :]) & same, 0.0, NEG).astype(np.float32)
    cm = np.stack([(i < 64), (i >= 64)], 1).astype(np.float32)
    cmask = np.concatenate([tri, blk, umask, neg, cm, np.ones((128, 128), np.float32)], 1)
    q = np.arange(512)
    dm = np.stack([np.where((jj * 128 + i[:, None]) <= q[None, :], 0.0, NEG) for jj in range(4)], 1).astype(np.float32)
    sel = np.zeros((6, 8), np.float32)
    sel[0, 0] = 1; sel[1, 1] = 1; sel[2, 2] = 1; sel[3:6, 3] = 1
    sel[3, 4] = -1; sel[4, 5] = -1; sel[5, 6] = -1; sel[0:3, 7] = 1
    return {"cmask": cmask, "dmask": dm, "sel": sel}


def build_A(TT):
    nc = bass.Bass("TRN2", target_bir_lowering=False)
    din = lambda name, shape, dt=F32: nc.dram_tensor(name, list(shape), dt, kind="ExternalInput").ap()
    xT = din("xT", [D, TT], BF16)
    w_fm = din("w_fm", [D, NFM]); w_tm = din("w_tm", [D, NTM])
    conv_w = din("conv_w", [128, 3, 4]); conv_b = din("conv_b", [128, 3])
    pp = din("pp", [8])
    pool_wb = din("pool_wb", [65, 64]); pool_scale = din("pool_scale", [64])
    pool_coef = din("pool_coef", [64, 4]); pool_fix = din("pool_fix", [64, 16])
    cmask_d = din("cmask", [128, 4 * 128 + 2 + 128]); dmask_d = din("dmask", [128, 4, 512]); sel_d = din("sel", [6, 8])
    y = nc.dram_tensor("y", [TT, 256], F32, kind="ExternalOutput").ap()

    c = Ctx(nc)
    NBLK = TT // 512
    NTILE = TT // 128

    ident_bf = make_ident(c, "ident_bf", BF16)
    ident_f = make_ident(c, "ident_f", F32)
    cm = c.sb("cm", [128, 4 * 128 + 2 + 128])
    c.dma("sp", cm[:], cmask_d, writes=[cm])
    TRI = cm[:, 0:128]; BLK = cm[:, 128:256]; UMASK = cm[:, 256:384]; NEGM = cm[:, 384:512]
    CMK = cm[:, 512:514]; ONES = cm[:, 514:642]
    dmask = c.sb("dmask_sb", [128, 4, 512], BF16)
    c.dma("pool", dmask[:], dmask_d, writes=[dmask])
    sel = c.sb("sel_sb", [6, 8])
    c.dma("sp", sel[:], sel_d, writes=[sel])
    wfm = c.sb("wfm", [128, 8, NFM], BF16); wtm = c.sb("wtm", [128, 8, NTM], BF16)
    c.dma("pool", wfm[:], w_fm.rearrange("(k p) n -> p k n", p=128), writes=[wfm])
    c.dma("pool", wtm[:], w_tm.rearrange("(k p) n -> p k n", p=128), writes=[wtm])
    cw = c.sb("cw", [128, 3, 4]); cb = c.sb("cb", [128, 3])
    c.dma("sp", cw[:], conv_w, writes=[cw]); c.dma("sp", cb[:], conv_b, writes=[cb])
    ppb = c.sb("ppb", [128, 8])
    c.dma("sp", ppb[:], pp.partition_broadcast(128), writes=[ppb])
    abc = c.sb("abc", [128, 2])
    c.act(abc[:], ppb[:, 2:4], AF.Exp, [ppb], [abc])
    c.op("dve", lambda: nc.vector.tensor_scalar(abc[:], abc[:], -1.0, None, ALU.mult), [abc], [abc])
    nfb = c.sb("nfb", [128, 1])
    c.op("dve", lambda: nc.vector.tensor_scalar(nfb[:], ppb[:, 6:7], -1.0, None, ALU.mult), [ppb], [nfb])
    dtbias4 = c.sb("dtbias4", [128, 4, 2])
    for tt in range(4):
        c.op("dve", lambda tt=tt: nc.vector.tensor_copy(dtbias4[:, tt, :], ppb[:, 0:2]), [ppb], [dtbias4])
    abc4 = c.sb("abc4", [128, 4, 2])
    for tt in range(4):
        c.op("dve", lambda tt=tt: nc.vector.tensor_copy(abc4[:, tt, :], abc[:]), [abc], [abc4])
    DI = [c.sb("DI%d" % h, [128, 128], BF16) for h in range(2)]
    for h in range(2):
        c.op("dve", lambda h=h: nc.vector.tensor_scalar(DI[h][:], ident_bf[:], ppb[:, 4 + h:5 + h], None, ALU.mult), [ident_bf, ppb], [DI[h]])
    one_t = c.sb("one_t", [128, 1])
    c.op("pool", lambda: nc.gpsimd.memset(one_t[:], 1.0), [], [one_t])
    pwb = c.sb("pwb", [65, 64]); psc = c.sb("psc", [65, 64]); pw_bf = c.sb("pw_bf", [65, 64], BF16)
    c.dma("sp", pwb[:], pool_wb, writes=[pwb])
    c.dma("sp", psc[:], pool_scale.partition_broadcast(65), writes=[psc])
    c.op("dve", lambda: nc.vector.tensor_tensor(pw_bf[:], pwb[:], psc[:], ALU.mult), [pwb, psc], [pw_bf])
    pcoef = c.sb("pcoef", [64, 4]); pfix = c.sb("pfix", [64, 16])
    c.dma("sp", pcoef[:], pool_coef, writes=[pcoef]); c.dma("sp", pfix[:], pool_fix, writes=[pfix])

    try:
      ckpt(1)
    except StopBuild:
      barrier(c); c.close(); return nc
    KT = c.sb("KT", [128, TT], BF16)
    VA = c.sb("VA", [128, NTILE, 66], BF16)
    c.op("pool", lambda: nc.gpsimd.memset(KT[:], 0.0), [], [KT])
    c.op("pool", lambda: nc.gpsimd.memset(VA[:], 1.0), [], [VA])
    state = c.sb("state", [128, 128])
    state_bf = [c.sb("state_bf%d" % i, [128, 128], BF16) for i in range(2)]
    c.op("dve", lambda: nc.vector.memset(state[:], 0.0), [], [state])
    c.op("dve", lambda: nc.vector.memset(state_bf[0][:], 0.0), [], [state_bf[0]])
    ccar = c.sb("ccar", [6, 1])
    c.op("dve", lambda: nc.vector.memset(ccar[:], 0.0), [], [ccar])
    U = [c.sb("U%d" % g, [128, 3 + 512]) for g in range(3)]
    for g in range(3):
        c.op("pool", lambda g=g: nc.gpsimd.memset(U[g][:], 0.0), [], [U[g]])
    PU = c.sb("PU", [64, 16 + 512])
    c.op("pool", lambda: nc.gpsimd.memset(PU[:], 0.0), [], [PU])

    try:
      ckpt(2)
    except StopBuild:
      barrier(c); c.close(); return nc
    xT_p = Pool(c, "xTb", [128, 8, 512], BF16, 2)
    pst = Pool(c, "pst", [128, 512], F32, 2, ps=True)
    ppro = c.ps("ppro", [128, 512], F32)

    class _One:
        def next(self):
            return ppro
    pfm = _One()
    pacc = c.ps("pacc", [128, 512], F32)
    tA = [c.ps("tA%d" % u, [128, 512], F32) for u in range(2)]
    tB = [c.ps("tB%d" % u, [128, 512], F32) for u in range(2)]
    cacc3 = [c.sb("cacc%d" % g, [128, 512]) for g in range(3)]
    zsb_p = Pool(c, "zsb", [128, 4, 128], F32, 2)
    fmT = [Pool(c, "fmT%d" % g, [128, 512], BF16, 2) for g in range(3)]
    QT_p = Pool(c, "QT", [128, 512], BF16, 2)
    for qq in QT_p.ts:
        c.op("pool", lambda qq=qq: nc.gpsimd.memset(qq[:], 0.0), [], [qq])
    f6_p = Pool(c, "f6", [6, 512], F32, 2); lf_p = Pool(c, "lf", [6, 512], F32, 2); cc_p = Pool(c, "cc", [6, 512], F32, 2)
    ones6 = c.sb("ones6", [6, 512])
    c.op("pool", lambda: nc.gpsimd.memset(ones6[:], 1.0), [], [ones6])
    hi_p = Pool(c, "hi", [6, 512], BF16, 2); mid_p = Pool(c, "mid", [6, 512], BF16, 2); lo_p = Pool(c, "lo", [6, 512], BF16, 2)
    r1_p = Pool(c, "r1", [6, 512], F32, 2); r2_p = Pool(c, "r2", [6, 512], F32, 2)
    aq_p = Pool(c, "aq", [6, 512], F32, 2); ak_p = Pool(c, "ak", [6, 512], F32, 2)
    ztmb_p = Pool(c, "ztmb", [128, 4, 128], F32, 2); dtrb_p = Pool(c, "dtrb", [128, 4, 2], F32, 2); dtb_p = Pool(c, "dtb", [128, 4, 2], F32, 2)
    smb_p = Pool(c, "smb", [128, 32], F32, 2); exb_p = Pool(c, "exb", [128, 32], F32, 2)
    Ab_p = Pool(c, "Ab", [128, 4, 2], F32, 2); A4b_p = Pool(c, "A4b", [128, 4, 4], F32, 2)
    dtdb_p = Pool(c, "dtdb", [128, 4, 2], F32, 2)
    xstm_p = Pool(c, "xstm", [128, 128], BF16, 2); btm_p = Pool(c, "btm", [128, 128], BF16, 2)
    X_p = Pool(c, "X", [128, 128], BF16, 2); Xd_p = Pool(c, "Xd", [128, 128], BF16, 2)
    UA_p = Pool(c, "UA", [128, 128], F32, 4); L_p = Pool(c, "L", [128, 128], F32, 4); MT_p = Pool(c, "MT", [128, 128], BF16, 4)
    pysb_p = Pool(c, "pysb", [128, 128], F32, 2); t1_p = Pool(c, "t1", [128, 128], F32, 2)
    zs_p = Pool(c, "zs", [128, 128], F32, 2)
    yo_p = Pool(c, "yo", [128, 256], F32, 8)
    PT_p = Pool(c, "PT", [128, 512], BF16, 3)
    osb_p = Pool(c, "osb", [65, 512], F32, 2); pfs_p = Pool(c, "pfs", [128, 260], F32, 2); rd_p = Pool(c, "rd", [128, 4], F32, 2)
    ps2 = [c.sb("ps%d" % i, [64, 16 + 512]) for i in range(4)]
    pmean = c.sb("pmean", [64, 512]); ptmp = c.sb("ptmp", [64, 512]); paug_p = Pool(c, "paug", [65, 512], BF16, 2)
    for pa in paug_p.ts:
        c.op("pool", lambda pa=pa: nc.gpsimd.memset(pa[:], 1.0), [], [pa])

    xTv = xT.rearrange("(k p) t -> p k t", p=128)
    sbf_i = 0

    BC = {}

    def prologue(blk):
        nonlocal xb_next
        t0 = blk * 512
        if blk == 0:
            xb_next = xT_p.next()
            c.dma("sp", xb_next[:], xTv[:, :, 0:512], writes=[xb_next])
        xb = xb_next
        if blk + 1 < NBLK:
            xb_next = xT_p.next()
            c.dma("sp", xb_next[:], xTv[:, :, t0 + 512:t0 + 1024], writes=[xb_next])
        for g in range(3):
            c.op("pool", lambda g=g: nc.gpsimd.tensor_copy(U[g][:, 0:3], U[g][:, 512:515]), [U[g]], [U[g]])
            pg = pfm.next()
            for k in range(8):
                c.mm(pg[:], wfm[:, k, g * 128:(g + 1) * 128], xb[:, k, :], k == 0, k == 7, [wfm, xb], [pg])
            c.op("act", lambda g=g, pg=pg: nc.scalar.copy(U[g][:, 3:515], pg[:]), [pg], [U[g]])
            ca = cacc3[g]
            c.act(ca[:], U[g][:, 0:512], AF.Identity, [U[g], cw, cb], [ca], bias=cb[:, g:g + 1], scale=cw[:, g, 0:1])
            for kk in range(1, 4):
                c.op("dve", lambda g=g, kk=kk: nc.vector.scalar_tensor_tensor(ca[:], U[g][:, kk:kk + 512], cw[:, g, kk:kk + 1], ca[:], ALU.mult, ALU.add),
                     [U[g], cw, ca], [ca])
            yield
        if DO_QF:
            yield
            QT = QT_p.next()
            pg = pfm.next()
            for k in range(8):
                c.mm(pg[:], wfm[:, k, 384:512], xb[:, k, :], k == 0, k == 7, [wfm, xb], [pg])
            c.op("act", lambda: nc.scalar.copy(QT[64:128, :], pg[64:128, :]), [pg], [QT])
            yield
            f6 = f6_p.next(); lf = lf_p.next(); cc = cc_p.next()
            c.act(f6[:], pg[0:6, :], AF.Exp, [pg, nfb], [f6], bias=nfb[0:6, 0:1], scale=-1.0)
            c.act(lf[:], f6[:], AF.Ln, [f6, one_t], [lf], bias=one_t[0:6, 0:1], scale=1.0)
            c.op("dve", lambda: nc.vector.tensor_tensor_scan(cc[:], ones6[:], lf[:], ccar[:, 0:1], ALU.mult, ALU.subtract), [ones6, lf, ccar], [cc])
            c.op("dve", lambda: nc.vector.tensor_copy(ccar[:], cc[:, 511:512]), [cc], [ccar])
            yield
            hi = hi_p.next(); mid = mid_p.next(); lo = lo_p.next(); r1 = r1_p.next(); r2 = r2_p.next()
            c.op("act", lambda: nc.scalar.copy(hi[:], cc[:]), [cc], [hi])
            c.op("dve", lambda: nc.vector.tensor_tensor(r1[:], cc[:], hi[:], ALU.subtract), [cc, hi], [r1])
            c.op("act", lambda: nc.scalar.copy(mid[:], r1[:]), [r1], [mid])
            c.op("dve", lambda: nc.vector.tensor_tensor(r2[:], r1[:], mid[:], ALU.subtract), [r1, mid], [r2])
            c.op("act", lambda: nc.scalar.copy(lo[:], r2[:]), [r2], [lo])
            yield
            aq = aq_p.next(); ak = ak_p.next()
            for (dst, co, final) in ((aq, 0, QT[0:6, :]), (ak, 4, KT[0:6, t0:t0 + 512])):
                c.op("dve", lambda dst=dst, co=co: nc.vector.tensor_scalar(dst[:], hi[:], sel[:, co:co + 1], sel[:, (3 if co == 0 else 7):(4 if co == 0 else 8)], ALU.mult, ALU.add), [hi, sel], [dst])
                c.op("dve", lambda dst=dst, co=co: nc.vector.scalar_tensor_tensor(dst[:], mid[:], sel[:, co + 1:co + 2], dst[:], ALU.mult, ALU.add), [mid, sel, dst], [dst])
                tgt = QT if co == 0 else KT
                c.op("dve", lambda dst=dst, co=co, final=final: nc.vector.scalar_tensor_tensor(final, lo[:], sel[:, co + 2:co + 3], dst[:], ALU.mult, ALU.add), [lo, sel, dst], [tgt])
        if DO_K:
            yield
            pg = pfm.next()
            for k in range(8):
                c.mm(pg[:], wfm[:, k, 512:640], xb[:, k, :], k == 0, k == 7, [wfm, xb], [pg])
            c.act(KT[64:128, t0:t0 + 512], pg[64:128, :], AF.Identity, [pg], [KT], scale=0.125)
        if DO_PW:
            yield
            c.op("pool", lambda: nc.gpsimd.tensor_copy(PU[:, 0:16], PU[:, 512:528]), [PU], [PU])
            pg = pfm.next()
            for k in range(8):
                c.mm(pg[0:64, :], wfm[:, k, 640:704], xb[:, k, :], k == 0, k == 7, [wfm, xb], [pg])
            c.op("act", lambda: nc.scalar.copy(PU[:, 16:528], pg[0:64, :]), [pg], [PU])
            yield
            srcs = [PU] + ps2
            for lv in range(4):
                sh = 1 << lv
                lo_i = 2 * sh - 1
                src = srcs[lv]; dstt = ps2[lv]
                c.op("pool", lambda src=src, dstt=dstt, sh=sh, lo_i=lo_i: nc.gpsimd.tensor_tensor(dstt[:, lo_i:528], src[:, lo_i:528], src[:, lo_i - sh:528 - sh], ALU.add), [src], [dstt])
            yield
            c.op("dve", lambda: nc.vector.tensor_scalar(pmean[:], ps2[0][:, 16:528], pcoef[:, 0:1], None, ALU.mult), [ps2[0], pcoef], [pmean])
            for lv in range(1, 4):
                c.op("dve", lambda lv=lv: nc.vector.scalar_tensor_tensor(pmean[:], ps2[lv][:, 16:528], pcoef[:, lv:lv + 1], pmean[:], ALU.mult, ALU.add), [ps2[lv], pcoef, pmean], [pmean])
            yield
            if blk == 0:
                c.op("pool", lambda: nc.gpsimd.tensor_tensor(pmean[:, 0:16], pmean[:, 0:16], pfix[:], ALU.mult), [pmean, pfix], [pmean])
            paug = paug_p.next()
            c.op("pool", lambda: nc.gpsimd.tensor_tensor(paug[0:64, :], pmean[:], PU[:, 16:528], ALU.subtract), [pmean, PU], [paug])

        ztmb = ztmb_p.next(); dtrb = dtrb_p.next(); dtb = dtb_p.next()
        for tt in range(4):
            ti = blk * 4 + tt
            cs = slice(tt * 128, (tt + 1) * 128)
            ptm = pfm.next()
            for k in range(8):
                c.mm(ptm[:, 0:NTM], xb[:, k, cs], wtm[:, k, :], k == 0, k == 7, [xb, wtm], [ptm])
            c.op("act", lambda: nc.scalar.copy(ztmb[:, tt, :], ptm[:, 0:128]), [ptm], [ztmb])
            if TM >= 2: c.op("act", lambda: nc.scalar.copy(VA[:, ti, 0:64], ptm[:, 128:192]), [ptm], [VA])
            if TM >= 3: c.op("act", lambda: nc.scalar.copy(dtrb[:, tt, :], ptm[:, 192:194]), [ptm], [dtrb])
            yield
        yield
        fts = []
        for g in range(3):
            ft = fmT[g].next()
            c.act(ft[:], cacc3[g][:], AF.Silu, [cacc3[g]], [ft])
            fts.append(ft)
        xsT, BT, CT = fts
        zsb = zsb_p.next()
        c.act(zsb[:], ztmb[:], AF.Silu, [ztmb], [zsb])
        yield
        if TM >= 3: c.op("dve", lambda: nc.vector.tensor_tensor(dtrb[:], dtrb[:], dtbias4[:], ALU.add), [dtrb, dtbias4], [dtrb])
        if TM >= 4: c.act(dtrb[:], dtrb[:], AF.Exp, [dtrb], [dtrb])
        if TM >= 5: c.act(dtb[:], dtrb[:], AF.Ln, [dtrb, one_t], [dtb], bias=one_t[:, 0:1], scale=1.0)
        Ab = Ab_p.next(); A4b = A4b_p.next()
        c.op("dve", lambda: nc.vector.tensor_tensor(Ab[:], dtb[:], abc4[:], ALU.mult), [dtb, abc4], [Ab])
        for ck in range(2):
            c.op("dve", lambda ck=ck: nc.vector.tensor_scalar(A4b[:, :, ck * 2:ck * 2 + 2], Ab[:], CMK[:, ck:ck + 1], None, ALU.mult), [Ab, cm], [A4b])
        yield
        smb = smb_p.next(); exb = exb_p.next(); dtdb = dtdb_p.next()
        c.mm(ppro[:, 0:8], TRI, Ab[:].rearrange("p a b -> p (a b)"), True, True, [cm, Ab], [ppro])
        c.mm(ppro[:, 8:16], BLK, Ab[:].rearrange("p a b -> p (a b)"), True, True, [cm, Ab], [ppro])
        c.mm(ppro[:, 16:32], ONES, A4b[:].rearrange("p a b -> p (a b)"), True, True, [cm, A4b], [ppro])
        c.op("act", lambda: nc.scalar.copy(smb[:], ppro[:, 0:32]), [ppro], [smb])
        yield
        c.op("dve", lambda: nc.vector.tensor_tensor(smb[:, 8:16], smb[:, 8:16], smb[:, 0:8], ALU.subtract), [smb], [smb])
        yield
        c.act(exb[:], smb[:], AF.Exp, [smb], [exb])
        yield
        c.op("dve", lambda: nc.vector.tensor_tensor(dtdb[:].rearrange("p a b -> p (a b)"), dtb[:].rearrange("p a b -> p (a b)"), exb[:, 8:16], ALU.mult), [dtb, exb], [dtdb])
        BC[blk] = (xsT, BT, CT, QT, paug, ztmb, dtb, Ab, A4b, exb, dtdb, zsb)
        yield

    def tiles_fox(blk):
        t0 = blk * 512
        xsT, BT, CT, QT, paug, ztmb, dtb, Ab, A4b, exb, dtdb, zsb = BC.pop(blk)
        nkt = 4 * blk + 4
        def fox_steps():
            prev = None
            for j in range(nkt + 1):
                cur = None
                if j < nkt:
                    ps_ = pst.next()
                    diag = j >= 4 * blk
                    c.mm(ps_[:], KT[:, j * 128:(j + 1) * 128], QT[:], True, not diag, [KT, QT], [ps_])
                    if diag:
                        c.mm(ps_[:], ident_bf[:], dmask[:, j - 4 * blk, :], False, True, [ident_bf, dmask], [ps_])
                    cur = (j, ps_)
                if prev is not None:
                    pj, pps = prev
                    PT = PT_p.next()
                    c.act(PT[:], pps[:], AF.Exp, [pps], [PT])
                    c.mm(pacc[0:65, :], VA[:, pj, 0:65], PT[:], pj == 0, pj == nkt - 1, [VA, PT], [pacc])
                prev = cur
                yield
        fsteps = fox_steps()
        per_tile = (nkt + 1 + 3) // 4

        yos = []

        def ssd_tile(tt):
            nonlocal sbf_i
            u = tt % 2
            bA = tA[u]; bB = tB[u]
            cs = slice(tt * 128, (tt + 1) * 128)
            ztm = ztmb[:, tt, :]; dt_ = dtb[:, tt, :]
            yo = yo_p.next()
            yos.append(yo)
            A_ = Ab[:, tt, :]
            dtd = dtdb[:, tt, :]
            UAs = []
            for h in range(2):
                UA = UA_p.next()
                c.act(UA[:], UMASK, AF.Identity, [cm, Ab], [UA], scale=A_[:, h:h + 1])
                UAs.append(UA)
            ptb = bA[:].bitcast(BF16)
            c.tr(ptb[:, 0:128], xsT[:, cs], ident_bf[:], [xsT, ident_bf], [bA])
            c.tr(ptb[:, 128:256], BT[:, cs], ident_bf[:], [BT, ident_bf], [bA])
            c.mm(bB[:, 0:128], BT[:, cs], CT[:, cs], True, True, [BT, CT], [bB])
            yield
            xstm = xstm_p.next(); btm = btm_p.next()
            c.op("act", lambda: nc.scalar.copy(xstm[:], ptb[:, 0:128]), [bA], [xstm])
            c.op("act", lambda: nc.scalar.copy(btm[:], ptb[:, 128:256]), [bA], [btm])
            X = X_p.next(); Xd = Xd_p.next()
            for h in range(2):
                hs = slice(h * 64, (h + 1) * 64)
                c.act(X[:, hs], ptb[:, hs], AF.Identity, [bA, dtb], [X], scale=dt_[:, h:h + 1])
            for h in range(2):
                hs = slice(h * 64, (h + 1) * 64)
                c.act(Xd[:, hs], ptb[:, hs], AF.Identity, [bA, dtdb], [Xd], scale=dtd[:, h:h + 1])
            for h in range(2):
                c.mm(bB[:, 128 * (h + 1):128 * (h + 2)], UAs[h][:], TRI, True, False, [UAs[h], cm], [bB])
                c.mm(bB[:, 128 * (h + 1):128 * (h + 2)], ident_f[:], NEGM, False, True, [ident_f, cm], [bB])
            yield
            Ls = []
            for h in range(2):
                L = L_p.next()
                c.act(L[:], bB[:, 128 * (h + 1):128 * (h + 2)], AF.Exp, [bB], [L])
                Ls.append(L)
            yield
            MTs = []
            for h in range(2):
                MT = MT_p.next()
                c.op("dve", lambda h=h, MT=MT: nc.vector.tensor_tensor(MT[:], Ls[h][:], bB[:, 0:128], ALU.mult), [Ls[h], bB], [MT])
                MTs.append(MT)
            yield
            for h in range(2):
                hs = slice(h * 64, (h + 1) * 64)
                c.mm(bA[:, hs], MTs[h][:], X[:, hs], True, False, [MTs[h], X], [bA])
                c.mm(bA[:, hs], DI[h][:], xstm[:, hs], False, True, [DI[h], xstm], [bA])
            yield
            for ck in range(2):
                rs_ = slice(ck * 64, (ck + 1) * 64)
                lcs = slice(tt * 128 + ck * 64, tt * 128 + (ck + 1) * 64)
                sb_cur = state_bf[sbf_i]
                c.mm(bA[:, 256 + ck * 128:256 + (ck + 1) * 128], btm[rs_, :], Xd[rs_, :], True, True, [btm, Xd], [bA])
                c.mm(bA[rs_, 128:256], CT[:, lcs], sb_cur[:], True, True, [CT, sb_cur], [bA])
                for h in range(2):
                    hs = slice(h * 64, (h + 1) * 64)
                    c.op("dve", lambda h=h, hs=hs, ck=ck: nc.vector.scalar_tensor_tensor(state[:, hs], state[:, hs], exb[:, 16 + tt * 4 + ck * 2 + h:17 + tt * 4 + ck * 2 + h],
                                                                                         bA[:, 256 + ck * 128 + h * 64:256 + ck * 128 + (h + 1) * 64], ALU.mult, ALU.add),
                         [state, exb, bA], [state])
                sbf_i = 1 - sbf_i
                nb_ = state_bf[sbf_i]
                c.op("act", lambda nb_=nb_: nc.scalar.copy(nb_[:], state[:]), [state], [nb_])
            yield
            pysb = pysb_p.next(); t1 = t1_p.next()
            c.op("act", lambda: nc.scalar.copy(pysb[:], bA[:, 0:128]), [bA], [pysb])
            c.mm(bB[:, 392:456], paug[0:65, cs], pw_bf[:], True, True, [paug, pw_bf], [bB])
            yield
            for h in range(2):
                hs = slice(h * 64, (h + 1) * 64)
                c.op("dve", lambda h=h, hs=hs: nc.vector.scalar_tensor_tensor(t1[:, hs], bA[:, 128 + h * 64:128 + (h + 1) * 64], exb[:, tt * 2 + h:tt * 2 + h + 1], pysb[:, hs], ALU.mult, ALU.add),
                     [bA, exb, pysb], [t1])
            c.op("act", lambda: nc.scalar.copy(yo[:, 192:256], bB[:, 392:456]), [bB], [yo])
            yield
            c.op("pool", lambda: nc.gpsimd.tensor_tensor(yo[:, 0:128], t1[:], zsb[:, tt, :], ALU.mult), [t1, zsb], [yo])
            yield

        NROUND = 10
        fox_per_round = (nkt + 1 + 2 * NROUND - 1) // (2 * NROUND)
        for pair in range(2):
            gens = [ssd_tile(2 * pair), ssd_tile(2 * pair + 1)]
            while gens:
                for g in list(gens):
                    try:
                        next(g)
                    except StopIteration:
                        gens.remove(g)
                for _ in range(fox_per_round):
                    next(fsteps, None)
                yield
        for _ in fsteps:
            yield
        if DO_FOX:
            osb = osb_p.next(); rd = rd_p.next()
            c.op("act", lambda: nc.scalar.copy(osb[:], pacc[0:65, :]), [pacc], [osb])
            pf = tB[0]
            for tt in range(4):
                c.tr(pf[:, tt * 65:(tt + 1) * 65], osb[:, tt * 128:(tt + 1) * 128], ident_f[0:65, 0:65], [osb, ident_f], [pf])
            pfs = pfs_p.next()
            c.op("act", lambda: nc.scalar.copy(pfs[:], pf[:, 0:260]), [pf], [pfs])
            for tt in range(4):
                c.op("dve", lambda tt=tt: nc.vector.reciprocal(rd[:, tt:tt + 1], pfs[:, tt * 65 + 64:tt * 65 + 65]), [pfs], [rd])
        for tt in range(4):
            yo = yos[tt]
            if DO_FOX: c.op("dve", lambda tt=tt, yo=yo: nc.vector.tensor_scalar(yo[:, 128:192], pfs[:, tt * 65:tt * 65 + 64], rd[:, tt:tt + 1], None, ALU.mult), [pfs, rd], [yo])
            r0 = t0 + tt * 128
            c.dma("sp", y[r0:r0 + 128, :], yo[:], reads=[yo])
        yield

    xb_next = None
    run_interleaved([prologue(0)])
    for blk in range(NBLK):
        gens = [tiles_fox(blk)]
        if blk + 1 < NBLK:
            gens.append(prologue(blk + 1))
        run_interleaved(gens)
    barrier(c)
    c.close()
    print("A: instructions", c.ninst, "waits", c.nwait)
    return nc


def prep_A(w_in, conv_w, conv_b, dt_bias, a_log, d_skip, f_bias, pool_w, pool_b, pool_scale, j):
    g = j // 2
    cz = slice(128 * j, 128 * j + 128)
    cxs = slice(512 + 128 * j, 512 + 128 * j + 128)
    cB = slice(1024 + 128 * g, 1024 + 128 * g + 128)
    cC = slice(1280 + 128 * g, 1280 + 128 * g + 128)
    cdt = slice(1536 + 2 * j, 1536 + 2 * j + 2)
    cq = slice(1544 + 64 * j, 1544 + 64 * j + 64)
    ck = slice(1800 + 64 * j, 1800 + 64 * j + 64)
    cv = slice(2056 + 64 * j, 2056 + 64 * j + 64)
    cf = 2312 + j
    cp = slice(2316 + 64 * j, 2316 + 64 * j + 64)
    w_fm = np.zeros((D, NFM), np.float32)
    w_fm[:, 0:128] = w_in[:, cxs]; w_fm[:, 128:256] = w_in[:, cB]; w_fm[:, 256:384] = w_in[:, cC]
    for r in range(6):
        w_fm[:, 384 + r] = w_in[:, cf]
    w_fm[:, 384 + 64:384 + 128] = w_in[:, cq]
    w_fm[:, 512 + 64:512 + 128] = w_in[:, ck]
    w_fm[:, 640:704] = w_in[:, cp]
    w_tm = np.concatenate([w_in[:, cz], w_in[:, cv], w_in[:, cdt], w_in[:, cf:cf + 1], np.zeros((D, 1), np.float32)], 1).astype(np.float32)
    chans = [np.arange(128 * j, 128 * j + 128), np.arange(512 + 128 * g, 512 + 128 * g + 128), np.arange(768 + 128 * g, 768 + 128 * g + 128)]
    cw = np.stack([conv_w[:, ch].T for ch in chans], 1).astype(np.float32)
    cbb = np.stack([conv_b[ch] for ch in chans], 1).astype(np.float32)
    pp = np.zeros(8, np.float32)
    pp[0:2] = dt_bias[2 * j:2 * j + 2]; pp[2:4] = a_log[2 * j:2 * j + 2]; pp[4:6] = d_skip[2 * j:2 * j + 2]; pp[6] = f_bias[j]
    wb = np.concatenate([pool_w[j], pool_b[j][None, :]], 0).astype(np.float32)
    win = (2, 4, 8, 16)[j]
    coef = np.zeros((64, 4), np.float32); coef[:, j] = 1.0 / win
    fix = np.ones((64, 16), np.float32)
    tpos = np.arange(16)
    fix[:, :] = (win / np.minimum(tpos + 1, win))[None, :]
    return {"w_fm": w_fm, "w_tm": w_tm, "conv_w": np.ascontiguousarray(cw), "conv_b": np.ascontiguousarray(cbb), "pp": pp,
            "pool_wb": wb, "pool_scale": np.ascontiguousarray(pool_scale[64 * j:64 * j + 64]).astype(np.float32),
            "pool_coef": coef, "pool_fix": fix}


def build_P(NT):
    nc = bass.Bass("TRN2", target_bir_lowering=False)
    x = nc.dram_tensor("x", [NT, D], F32, kind="ExternalInput").ap()
    xT = nc.dram_tensor("xT", [D, NT], BF16, kind="ExternalOutput").ap()
    c = Ctx(nc)
    ident_bf = make_ident(c, "ident_bf", BF16)
    xt_p = Pool(c, "xt", [128, D], F32, 3); xb_p = Pool(c, "xb", [128, D], BF16, 2)
    ptp = Pool(c, "ptp", [128, 8, 128], BF16, 2, ps=True)
    xTs_p = Pool(c, "xTs", [128, 8, 512], BF16, 2)
    for blk in range(NT // 512):
        xTs = xTs_p.next()
        for tt in range(4):
            r0 = blk * 512 + tt * 128
            xt = xt_p.next(); xb = xb_p.next()
            c.dma("sp", xt[:], x[r0:r0 + 128, :], writes=[xt])
            c.op("dve", lambda: nc.vector.tensor_copy(xb[:], xt[:]), [xt], [xb])
            tp = ptp.next()
            for k in range(8):
                c.tr(tp[:, k, :], xb[:, k * 128:(k + 1) * 128], ident_bf[:], [xb, ident_bf], [tp])
            c.op("act", lambda: nc.scalar.copy(xTs[:, :, tt * 128:(tt + 1) * 128], tp[:]), [tp], [xTs])
        c.dma("sp", xT.rearrange("(k p) t -> p k t", p=128)[:, :, blk * 512:(blk + 1) * 512], xTs[:], reads=[xTs])
    barrier(c)
    c.close()
    return nc


from concourse.bass_utils import run_bass_kernel_spmd

NCORES = 8
SEQ = 16384
BATCH = 2
NTOK = BATCH * SEQ // NCORES
DEPTH = 4
_CACHE = {}


def _prog(key, fn):
    if key not in _CACHE:
        _CACHE[key] = fn()
    return _CACHE[key]


def _run(nc, in_maps):
    res = run_bass_kernel_spmd(nc, in_maps, core_ids=list(range(NCORES)))
    return res.results


def kernel(**inp):
    f32 = lambda a: np.ascontiguousarray(np.asarray(a, dtype=np.float32))
    x = f32(inp["x"]); mem = f32(inp["mem"])
    xs = x.reshape(NCORES, NTOK, D)
    ncP = _prog("P", lambda: build_P(NTOK))
    resP = _run(ncP, [{"x": xs[c]} for c in range(NCORES)])
    xT_parts = [r["xT"] for r in resP]
    x_cur = [xs[c] for c in range(NCORES)]
    ncA = _prog("A", lambda: build_A(SEQ))
    consts = consts_A()
    for layer in range(DEPTH):
        moe = layer % 2 == 1
        jj = layer // 2
        xT_full = [np.ascontiguousarray(np.concatenate(xT_parts[b * 4:(b + 1) * 4], axis=1)) for b in range(BATCH)]
        in_maps = []
        for c in range(NCORES):
            b, j = divmod(c, 4)
            m = prep_A(f32(inp["w_in"][layer]), f32(inp["ssm_conv_w"][layer]), f32(inp["ssm_conv_b"][layer]),
                       f32(inp["ssm_dt_bias"][layer]), f32(inp["ssm_a_log"][layer]), f32(inp["ssm_d"][layer]),
                       f32(inp["fox_f_bias"][layer]), f32(inp["pool_w"][layer]), f32(inp["pool_b"][layer]),
                       f32(inp["pool_scale"][layer]), j)
            m.update(consts)
            m["xT"] = xT_full[b]
            in_maps.append(m)
        resA = _run(ncA, in_maps)
        mix = np.empty((BATCH, SEQ, D), np.float32)
        for c in range(NCORES):
            b, j = divmod(c, 4)
            y = resA[c]["y"]
            mix[b, :, 128 * j:128 * j + 128] = y[:, 0:128]
            mix[b, :, 512 + 64 * j:512 + 64 * j + 64] = y[:, 128:192]
            mix[b, :, 768 + 64 * j:768 + 64 * j + 64] = y[:, 192:256]
        mixs = mix.reshape(NCORES, NTOK, D)
        ncB = _prog("B%d" % moe, lambda: build_B(NTOK, moe))
        wts = {"normw": f32(inp["ssm_norm_w"][layer]), "w_out": f32(inp["w_out"][layer]),
               "ln1_g": f32(inp["ln1_g"][layer]), "ln1_b": f32(inp["ln1_b"][layer]),
               "ln2_g": f32(inp["ln2_g"][layer]), "ln2_b": f32(inp["ln2_b"][layer]),
               "ln3_g": f32(inp["ln3_g"][layer]), "ln3_b": f32(inp["ln3_b"][layer]),
               "wq": f32(inp["xa_wq"][layer]), "wk": f32(inp["xa_wk"][layer]),
               "wv": f32(inp["xa_wv"][layer]), "wo": f32(inp["xa_wo"][layer])}
        if moe:
            wts.update({"router": f32(inp["router_w"][jj]), "w1": f32(inp["moe_w1"][jj]),
                        "w3": f32(inp["moe_w3"][jj]), "w2": f32(inp["moe_w2"][jj])})
        else:
            wts.update({"w1": f32(inp["ffn_w1"][jj:jj + 1]), "w3": f32(inp["ffn_w3"][jj:jj + 1]),
                        "w2": f32(inp["ffn_w2"][jj:jj + 1])})
        in_maps = []
        for c in range(NCORES):
            m = dict(wts)
            m["mix"] = np.ascontiguousarray(mixs[c]); m["x"] = np.ascontiguousarray(x_cur[c])
            m["mem"] = mem[c // 4]
            in_maps.append(m)
        resB = _run(ncB, in_maps)
        x_cur = [r["xo"] for r in resB]
        xT_parts = [r["xoT"] for r in resB]
    return np.stack(x_cur).reshape(BATCH, SEQ, D).astype(np.float32)
```

```python
from contextlib import ExitStack
import numpy as np
import concourse.bass as bass
import concourse.mybir as mybir

F32 = mybir.dt.float32
BF16 = mybir.dt.bfloat16
AF = mybir.ActivationFunctionType
ALU = mybir.AluOpType
AX = mybir.AxisListType

ENGS = ["pe", "act", "dve", "pool", "sp"]
NDS = 40


class T:
    __slots__ = ("t", "w", "r", "name")

    def __init__(self, t, name=""):
        self.t = t
        self.w = None
        self.r = {}
        self.name = name

    def __getitem__(self, k):
        return self.t[k]


class Ctx:
    LIMIT = 30000
    NDMAX = 128

    def __init__(self, nc):
        self.nc = nc
        self.es = ExitStack()
        self.eng = {"pe": nc.tensor, "act": nc.scalar, "dve": nc.vector,
                    "pool": nc.gpsimd, "sp": nc.sync}
        nd = self.NDMAX
        self.sems = [self.es.enter_context(nc.semaphore("d%d" % i)) for i in range(NDS)]
        self.mult = [16] * NDS
        self.cnt = [0] * nd
        self.snap = [dict() for _ in range(nd)]
        self.known = {k: np.zeros(nd, np.int64) for k in ENGS}
        self.cur = {}
        self.old = {k: [] for k in ENGS}
        self.edims = {k: set() for k in ENGS}
        for k in ENGS:
            self._new_dim(k)
        self.rr = 0
        self.nwait = 0
        self.ninst = 0

    def _new_dim(self, e):
        d = len(self.sems)
        assert d < self.NDMAX
        self.sems.append(self.es.enter_context(self.nc.semaphore("c_%s_%d" % (e, d))))
        self.mult.append(1)
        if e in self.cur:
            self.old[e].append(self.cur[e])
        self.cur[e] = d
        self.edims[e].add(d)

    def sb(self, name, shape, dt=F32):
        return T(self.es.enter_context(self.nc.sbuf_tensor(name, list(shape), dt)), name)

    def ps(self, name, shape, dt=F32):
        return T(self.es.enter_context(self.nc.psum_tensor(name, list(shape), dt)), name)

    def close(self):
        self.es.close()

    def _wait(self, e, dim, c):
        kn = self.known[e]
        if kn[dim] >= c:
            return
        self.eng[e].wait_ge(self.sems[dim], int(c) * self.mult[dim])
        self.nwait += 1
        s = self.snap[dim].get(c)
        if s is not None:
            np.maximum(kn, s, out=kn)
        kn[dim] = max(kn[dim], c)

    def _deps(self, e, reads, writes, pe_acc=False):
        need = {}
        for t in reads:
            if t.w is not None:
                d, c = t.w
                if need.get(d, 0) < c:
                    need[d] = c
        for t in writes:
            if t.w is not None:
                d, c = t.w
                if not (pe_acc and d in self.edims["pe"]):
                    if need.get(d, 0) < c:
                        need[d] = c
            for d, c in t.r.items():
                if need.get(d, 0) < c:
                    need[d] = c
        for d, c in need.items():
            self._wait(e, d, c)

    def _mark(self, dim, c, reads, writes):
        for t in reads:
            if t.r.get(dim, 0) < c:
                t.r[dim] = c
        for t in writes:
            t.w = (dim, c)
            t.r = {}

    def op(self, e, fn, reads=(), writes=(), pe_acc=False):
        self._deps(e, reads, writes, pe_acc)
        ins = fn()
        if self.cnt[self.cur[e]] >= self.LIMIT:
            self._new_dim(e)
        d = self.cur[e]
        self.cnt[d] += 1
        c = self.cnt[d]
        ins.then_inc(self.sems[d], 1)
        s = self.known[e].copy()
        s[d] = c
        for od in self.old[e]:
            s[od] = self.cnt[od]
        self.snap[d][c] = s
        self._mark(d, c, reads, writes)
        self.ninst += 1
        return ins

    def dma(self, q, out, in_, reads=(), writes=(), **kw):
        self._deps(q, reads, writes)
        kn = self.known[q]
        pick = None
        for k in range(NDS):
            i = (self.rr + k) % NDS
            if kn[i] >= self.cnt[i]:
                pick = i
                break
        if pick is None:
            pick = self.rr % NDS
            self._wait(q, pick, self.cnt[pick])
        self.rr = (pick + 1) % NDS
        d = pick
        ins = self.eng[q].dma_start(out=out, in_=in_, **kw)
        self.cnt[d] += 1
        c = self.cnt[d]
        ins.then_inc(self.sems[d], 16)
        s = kn.copy()
        s[d] = c
        self.snap[d][c] = s
        self._mark(d, c, reads, writes)
        self.ninst += 1
        return ins

    def finish(self, e="sp"):
        for d in range(len(self.sems)):
            if self.cnt[d] > 0:
                self._wait(e, d, self.cnt[d])

    def mm(self, out, lhsT, rhs, start, stop, reads, writes):
        nc = self.nc
        return self.op("pe", lambda: nc.tensor.matmul(out, lhsT, rhs, start=start, stop=stop),
                       reads, writes, pe_acc=not start)

    def tr(self, out, in_, ident, reads, writes):
        nc = self.nc
        return self.op("pe", lambda: nc.tensor.transpose(out, in_, ident), reads, writes)

    def act(self, out, in_, func, reads, writes, **kw):
        nc = self.nc
        return self.op("act", lambda: nc.scalar.activation(out, in_, func, **kw), reads, writes)


D = 1024
DFF = 3584
NFF = DFF // 128
MEM = 256
ALPHA = 8.0 ** 0.25
LN_EPS = 1e-5
RMS_EPS = 1e-5
NEXP = 8


class Pool:
    def __init__(self, c, name, shape, dt, n, ps=False):
        self.ts = [(c.ps if ps else c.sb)("%s%d" % (name, i), shape, dt) for i in range(n)]
        self.i = 0

    def next(self):
        t = self.ts[self.i % len(self.ts)]
        self.i += 1
        return t


def make_ident(c, name, dt):
    nc = c.nc
    t = c.sb(name, [128, 128], dt)
    c.op("pool", lambda: nc.gpsimd.memset(t[:], 1.0), [], [t])
    c.op("pool", lambda: nc.gpsimd.affine_select(t[:], t[:], pattern=[[-1, 128]], compare_op=ALU.is_equal,
                                                 fill=0.0, base=0, channel_multiplier=1), [t], [t])
    return t


def barrier(c):
    for e in ENGS:
        c.finish(e)


def layer_norm_gen(c, P, h, g_bc, b_bc, out):
    nc = c.nc
    st = P["st"].next(); mv = P["mv"].next(); rs = P["rs"].next(); nm = P["nm"].next()
    c.op("dve", lambda: nc.vector.bn_stats(st[:, 0, :], h[:, 0:512]), [h], [st])
    c.op("dve", lambda: nc.vector.bn_stats(st[:, 1, :], h[:, 512:1024]), [h], [st])
    yield
    c.op("dve", lambda: nc.vector.bn_aggr(mv[:], st[:].rearrange("p a b -> p (a b)")), [st], [mv])
    yield
    c.act(rs[:], mv[:, 1:2], AF.Sqrt, [mv, P["eps"]], [rs], bias=P["eps"][:, 0:1], scale=1.0)
    yield
    c.op("dve", lambda: nc.vector.reciprocal(rs[:], rs[:]), [rs], [rs])
    yield
    c.op("dve", lambda: nc.vector.scalar_tensor_tensor(nm[:], mv[:, 0:1], -1.0, rs[:], ALU.mult, ALU.mult), [mv, rs], [nm])
    yield
    tmp = P["lnt"].next()
    c.act(tmp[:], h[:], AF.Identity, [h, rs, nm], [tmp], bias=nm[:, 0:1], scale=rs[:, 0:1])
    yield
    c.op("dve", lambda: nc.vector.tensor_tensor(tmp[:], tmp[:], g_bc[:], ALU.mult), [tmp, g_bc], [tmp])
    yield
    c.op("dve", lambda: nc.vector.tensor_tensor(out[:], tmp[:], b_bc[:], ALU.add), [tmp, b_bc], [out])
    yield


def layer_norm(c, P, h, g_bc, b_bc, out):
    for _ in layer_norm_gen(c, P, h, g_bc, b_bc, out):
        pass


def run_interleaved(gens):
    gens = list(gens)
    while gens:
        for g in list(gens):
            try:
                next(g)
            except StopIteration:
                gens.remove(g)


def load_w_bf(c, dst, src_ap, q="pool"):
    v = src_ap.rearrange("(k p) n -> p k n", p=128)
    for k in range(8):
        c.dma(q, dst[:, k, :], v[:, k, :], writes=[dst])


def build_B(NT, moe, TB2=1024):
    nc = bass.Bass("TRN2", target_bir_lowering=False)
    dt_in = lambda name, shape, dt=F32: nc.dram_tensor(name, list(shape), dt, kind="ExternalInput").ap()
    mix = dt_in("mix", [NT, D]); x = dt_in("x", [NT, D]); mem = dt_in("mem", [MEM, D])
    normw = dt_in("normw", [512]); w_out = dt_in("w_out", [D, D])
    ln_g = [dt_in("ln%d_g" % i, [D]) for i in (1, 2, 3)]
    ln_b = [dt_in("ln%d_b" % i, [D]) for i in (1, 2, 3)]
    wq = dt_in("wq", [D, D]); wk = dt_in("wk", [D, D]); wv = dt_in("wv", [D, D]); wo = dt_in("wo", [D, D])
    if moe:
        router = dt_in("router", [D, NEXP])
        w1 = dt_in("w1", [NEXP, D, DFF]); w3 = dt_in("w3", [NEXP, D, DFF]); w2 = dt_in("w2", [NEXP, DFF, D])
    else:
        w1 = dt_in("w1", [1, D, DFF]); w3 = dt_in("w3", [1, D, DFF]); w2 = dt_in("w2", [1, DFF, D])
    xo = nc.dram_tensor("xo", [NT, D], F32, kind="ExternalOutput").ap()
    xoT = nc.dram_tensor("xoT", [D, NT], BF16, kind="ExternalOutput").ap()
    x2d = nc.dram_tensor("x2d", [NT, D], F32, kind="Internal").ap()
    x2Td = nc.dram_tensor("x2Td", [D, NT], BF16, kind="Internal").ap()
    x2d_T = T(x2d, "x2d"); x2Td_T = T(x2Td, "x2Td")

    c = Ctx(nc)
    NTILE = NT // 128
    NBLK = NT // 512
    nexp = NEXP if moe else 1

    ident_bf = make_ident(c, "ident_bf", BF16)
    ones_bf = c.sb("ones_bf", [128, 128], BF16)
    c.op("pool", lambda: nc.gpsimd.memset(ones_bf[:], 1.0), [], [ones_bf])
    gbc = [None] * 3; bbc = [None] * 3
    def load_ln(sc, i):
        gbc[i] = sc.sb("g%d" % i, [128, D]); bbc[i] = sc.sb("b%d" % i, [128, D])
        c.dma("sp", gbc[i][:], ln_g[i].partition_broadcast(128), writes=[gbc[i]])
        c.dma("sp", bbc[i][:], ln_b[i].partition_broadcast(128), writes=[bbc[i]])
    eps_t = c.sb("eps_t", [128, 1])
    c.op("pool", lambda: nc.gpsimd.memset(eps_t[:], LN_EPS), [], [eps_t])
    gates = c.sb("gates", [128, NTILE, NEXP]) if moe else None
    P = {"st": Pool(c, "st", [128, 2, 6], F32, 3), "mv": Pool(c, "mv", [128, 2], F32, 3),
         "rs": Pool(c, "rs", [128, 1], F32, 3), "nm": Pool(c, "nm", [128, 1], F32, 3),
         "lnt": Pool(c, "lnt", [128, D], F32, 2), "eps": eps_t}

    c1 = Ctx.__new__(Ctx); c1.__dict__.update(c.__dict__); c1.es = ExitStack()
    if True:
        s = c1
        load_ln(s, 0); load_ln(s, 1)
        nw_bc = s.sb("nw_bc", [128, 512])
        c.dma("sp", nw_bc[:], normw.partition_broadcast(128), writes=[nw_bc])
        wout_bf = s.sb("wout_bf", [128, 8, D], BF16); wq_bf = s.sb("wq_bf", [128, 8, D], BF16)
        wo_bf = s.sb("wo_bf", [128, 8, D], BF16)
        kT = s.sb("kT", [128, 8, MEM], BF16)
        vv = s.sb("vv", [128, 2, D], BF16)
        ptp = Pool(s, "ptp", [128, 8, 128], BF16, 2, ps=True)
        pmm = Pool(s, "pmm", [128, 512], F32, 4, ps=True)
        if moe:
            ident_f = make_ident(s, "ident_f", F32)
            ptf = [s.ps("ptf%d" % u, [128, 4, 128], F32) for u in range(2)]
            router_f = s.sb("router_f", [128, 8, NEXP])
            c.dma("sp", router_f[:], router.rearrange("(k p) e -> p k e", p=128), writes=[router_f])
        load_w_bf(c, wq_bf, wk)
        load_w_bf(c, wo_bf, wv)
        load_w_bf(c, wout_bf, w_out)
        memT = s.sb("memT", [128, 8, MEM], BF16)
        mt_p = Pool(s, "mt", [128, D], F32, 2)
        xt_p = Pool(s, "xt", [128, D], F32, 2)
        mb_p = Pool(s, "mb", [128, D], BF16, 2)
        for mc in range(2):
            m_f = mt_p.next(); m_b = mb_p.next()
            c.dma("sp", m_f[:], mem[mc * 128:(mc + 1) * 128, :], writes=[m_f])
            c.op("dve", lambda: nc.vector.tensor_copy(m_b[:], m_f[:]), [m_f], [m_b])
            tp = ptp.next()
            for k in range(8):
                c.tr(tp[:, k, :], m_b[:, k * 128:(k + 1) * 128], ident_bf[:], [m_b, ident_bf], [tp])
            c.op("act", lambda: nc.scalar.copy(memT[:, :, mc * 128:(mc + 1) * 128], tp[:]), [tp], [memT])
        for ch in range(8):
            pk = pmm.next()
            for k in range(8):
                c.mm(pk[:, 0:MEM], wq_bf[:, k, ch * 128:(ch + 1) * 128], memT[:, k, :], k == 0, k == 7, [wq_bf, memT], [pk])
            c.op("act", lambda: nc.scalar.copy(kT[:, ch, :], pk[:, 0:MEM]), [pk], [kT])
        for mc in range(2):
            for half in range(2):
                pv = pmm.next()
                for k in range(8):
                    c.mm(pv[:], memT[:, k, mc * 128:(mc + 1) * 128], wo_bf[:, k, half * 512:(half + 1) * 512], k == 0, k == 7, [wo_bf, memT], [pv])
                c.op("dve", lambda: nc.vector.tensor_copy(vv[:, mc, half * 512:(half + 1) * 512], pv[:]), [pv], [vv])
        load_w_bf(c, wq_bf, wq)
        load_w_bf(c, wo_bf, wo)

        mixn_p = Pool(s, "mixn", [128, D], BF16, 2)
        mixT_p = Pool(s, "mixT", [128, 8, 128], BF16, 2)
        ss_p = Pool(s, "ss", [128, 2], F32, 3)
        junk_p = [s.sb("junk%d" % i, [128, 256]) for i in range(2)]
        h_p = Pool(s, "h", [128, D], F32, 2)
        x1_p = [s.sb("x1_%d" % i, [128, D]) for i in range(4)]
        x1b_p = Pool(s, "x1b", [128, D], BF16, 2)
        x1T = s.sb("x1T", [128, 8, 512], BF16)
        qT = s.sb("qT", [128, 8, 512], BF16)
        pT_p = Pool(s, "pT", [128, 2, 512], BF16, 2)
        rden_p = Pool(s, "rden", [128, 512], F32, 2)
        oT = s.sb("oT", [128, 8, 512], BF16)
        x2_p = Pool(s, "x2", [128, D], F32, 2)
        x2T = s.sb("x2T", [128, 8, 512], BF16)
        if moe:
            x2Tf_p = Pool(s, "x2Tf", [128, 8, 128], F32, 2)
            lg_p = Pool(s, "lg", [128, 8], F32, 2); mx_p = Pool(s, "mx", [128, 8], F32, 2)
            nb_p = Pool(s, "nb", [128, 1], F32, 2); ee_p = Pool(s, "ee", [128, 8], F32, 2)
            mk_p = Pool(s, "mk", [128, 8], F32, 2); dn_p = Pool(s, "dn", [128, 1], F32, 2)

        for blk in range(NBLK):
            def phase1_tile(tt):
                ti = blk * 4 + tt
                r0 = ti * 128
                mt = mt_p.next(); xt = xt_p.next()
                c.dma("sp", mt[:], mix[r0:r0 + 128, :], writes=[mt])
                c.dma("sp", xt[:], x[r0:r0 + 128, :], writes=[xt])
                ss = ss_p.next(); mixn = mixn_p.next()
                yield
                for g in range(2):
                    c.act(junk_p[tt % 2][:], mt[:, g * 256:(g + 1) * 256], AF.Square, [mt], [junk_p[tt % 2], ss], accum_out=ss[:, g:g + 1])
                c.op("act", lambda: nc.scalar.copy(mixn[:, 512:1024], mt[:, 512:1024]), [mt], [mixn])
                yield
                c.act(ss[:], ss[:], AF.Sqrt, [ss], [ss], bias=eps_t[:, 0:1], scale=1.0 / 256)
                yield
                c.op("dve", lambda: nc.vector.reciprocal(ss[:], ss[:]), [ss], [ss])
                yield
                for g in range(2):
                    c.op("dve", lambda g=g: nc.vector.scalar_tensor_tensor(mixn[:, g * 256:(g + 1) * 256], mt[:, g * 256:(g + 1) * 256],
                                                                         ss[:, g:g + 1], nw_bc[:, g * 256:(g + 1) * 256], ALU.mult, ALU.mult),
                         [mt, ss, nw_bc], [mixn])
                yield
                tp = ptp.next()
                for k in range(8):
                    c.tr(tp[:, k, :], mixn[:, k * 128:(k + 1) * 128], ident_bf[:], [mixn, ident_bf], [tp])
                yield
                mixT = mixT_p.next()
                c.op("act", lambda: nc.scalar.copy(mixT[:], tp[:]), [tp], [mixT])
                yield
                h = h_p.next()
                pos = []
                for half in range(2):
                    po = pmm.next()
                    for k in range(8):
                        c.mm(po[:], mixT[:, k, :], wout_bf[:, k, half * 512:(half + 1) * 512], k == 0, k == 7, [mixT, wout_bf], [po])
                    pos.append(po)
                yield
                for half in range(2):
                    po = pos[half]
                    c.op("dve", lambda half=half, po=po: nc.vector.scalar_tensor_tensor(h[:, half * 512:(half + 1) * 512], xt[:, half * 512:(half + 1) * 512],
                                                                                 ALPHA, po[:], ALU.mult, ALU.add), [xt, po], [h])
                yield
                x1 = x1_p[tt]
                yield from layer_norm_gen(c, P, h, gbc[0], bbc[0], x1)
                x1b = x1b_p.next()
                c.op("act", lambda: nc.scalar.copy(x1b[:], x1[:]), [x1], [x1b])
                yield
                tp = ptp.next()
                for k in range(8):
                    c.tr(tp[:, k, :], x1b[:, k * 128:(k + 1) * 128], ident_bf[:], [x1b, ident_bf], [tp])
                yield
                c.op("act", lambda: nc.scalar.copy(x1T[:, :, tt * 128:(tt + 1) * 128], tp[:]), [tp], [x1T])
                yield

            for pair in range(2):
                run_interleaved([phase1_tile(2 * pair), phase1_tile(2 * pair + 1)])
            for ch in range(8):
                pq = pmm.next()
                for k in range(8):
                    c.mm(pq[:], wq_bf[:, k, ch * 128:(ch + 1) * 128], x1T[:, k, :], k == 0, k == 7, [wq_bf, x1T], [pq])
                if ch % 2 == 0:
                    c.op("act", lambda: nc.scalar.copy(qT[:, ch, :], pq[:]), [pq], [qT])
                else:
                    c.op("dve", lambda: nc.vector.tensor_copy(qT[:, ch, :], pq[:]), [pq], [qT])
            for hh in range(4):
                pT = pT_p.next()
                for mc in range(2):
                    psc = pmm.next()
                    for cc in range(2):
                        c.mm(psc[:], kT[:, 2 * hh + cc, mc * 128:(mc + 1) * 128], qT[:, 2 * hh + cc, :], cc == 0, cc == 1, [kT, qT], [psc])
                    c.act(pT[:, mc, :], psc[:], AF.Exp, [psc], [pT], scale=1.0 / 16.0)
                pden = pmm.next()
                for mc in range(2):
                    c.mm(pden[:], ones_bf[:], pT[:, mc, :], mc == 0, mc == 1, [ones_bf, pT], [pden])
                rden = rden_p.next()
                c.op("dve", lambda: nc.vector.reciprocal(rden[:], pden[:]), [pden], [rden])
                for cc in range(2):
                    pov = pmm.next()
                    for mc in range(2):
                        c.mm(pov[:], vv[:, mc, (2 * hh + cc) * 128:(2 * hh + cc + 1) * 128], pT[:, mc, :], mc == 0, mc == 1, [vv, pT], [pov])
                    c.op("dve", lambda cc=cc, pov=pov: nc.vector.tensor_tensor(oT[:, 2 * hh + cc, :], pov[:], rden[:], ALU.mult), [pov, rden], [oT])
            def phase3_tile(tt):
                ti = blk * 4 + tt
                r0 = ti * 128
                x1 = x1_p[tt]
                h = h_p.next()
                pos = []
                for half in range(2):
                    po = pmm.next()
                    for k in range(8):
                        c.mm(po[:], oT[:, k, tt * 128:(tt + 1) * 128], wo_bf[:, k, half * 512:(half + 1) * 512], k == 0, k == 7, [oT, wo_bf], [po])
                    pos.append(po)
                yield
                for half in range(2):
                    po = pos[half]
                    c.op("dve", lambda half=half, po=po: nc.vector.scalar_tensor_tensor(h[:, half * 512:(half + 1) * 512], x1[:, half * 512:(half + 1) * 512],
                                                                                 ALPHA, po[:], ALU.mult, ALU.add), [x1, po], [h])
                yield
                x2 = x2_p.next()
                yield from layer_norm_gen(c, P, h, gbc[1], bbc[1], x2)
                c.dma("sp", x2d[r0:r0 + 128, :], x2[:], reads=[x2], writes=[x2d_T])
                x2b = x1b_p.next()
                c.op("act", lambda: nc.scalar.copy(x2b[:], x2[:]), [x2], [x2b])
                if moe:
                    x2Tf = x2Tf_p.next()
                    for k4 in range(4):
                        c.tr(ptf[tt % 2][:, k4, :], x2[:, k4 * 128:(k4 + 1) * 128], ident_f[:], [x2, ident_f], [ptf[tt % 2]])
                yield
                tp = ptp.next()
                for k in range(8):
                    c.tr(tp[:, k, :], x2b[:, k * 128:(k + 1) * 128], ident_bf[:], [x2b, ident_bf], [tp])
                if moe:
                    c.op("act", lambda: nc.scalar.copy(x2Tf[:, 0:4, :], ptf[tt % 2][:]), [ptf[tt % 2]], [x2Tf])
                yield
                c.op("act", lambda: nc.scalar.copy(x2T[:, :, tt * 128:(tt + 1) * 128], tp[:]), [tp], [x2T])
                if moe:
                    for k4 in range(4):
                        c.tr(ptf[tt % 2][:, k4, :], x2[:, (4 + k4) * 128:(5 + k4) * 128], ident_f[:], [x2, ident_f], [ptf[tt % 2]])
                    yield
                    c.op("act", lambda: nc.scalar.copy(x2Tf[:, 4:8, :], ptf[tt % 2][:]), [ptf[tt % 2]], [x2Tf])
                    yield
                    pl = ptf[tt % 2]
                    for k in range(8):
                        c.mm(pl[:, 0, 0:NEXP], x2Tf[:, k, :], router_f[:, k, :], k == 0, k == 7, [x2Tf, router_f], [pl])
                    yield
                    lg = lg_p.next(); mx = mx_p.next(); nb = nb_p.next(); ee = ee_p.next(); mk = mk_p.next(); dn = dn_p.next()
                    c.op("act", lambda: nc.scalar.copy(lg[:], pl[:, 0, 0:NEXP]), [pl], [lg])
                    yield
                    c.op("dve", lambda: nc.vector.max(mx[:], lg[:]), [lg], [mx])
                    yield
                    c.op("dve", lambda: nc.vector.tensor_scalar(nb[:], mx[:, 0:1], -1.0, None, ALU.mult), [mx], [nb])
                    c.op("dve", lambda: nc.vector.tensor_scalar(mk[:], lg[:], mx[:, 1:2], None, ALU.is_ge), [lg, mx], [mk])
                    yield
                    c.act(ee[:], lg[:], AF.Exp, [lg, nb], [ee], bias=nb[:, 0:1], scale=1.0)
                    yield
                    c.op("dve", lambda: nc.vector.tensor_tensor(mk[:], mk[:], ee[:], ALU.mult), [mk, ee], [mk])
                    yield
                    c.op("dve", lambda: nc.vector.reduce_sum(dn[:], mk[:], axis=AX.X), [mk], [dn])
                    yield
                    c.op("dve", lambda: nc.vector.reciprocal(dn[:], dn[:]), [dn], [dn])
                    yield
                    c.op("dve", lambda: nc.vector.tensor_scalar(gates[:, ti, :], mk[:], dn[:, 0:1], None, ALU.mult), [mk, dn], [gates])
                yield

            for pair in range(2):
                run_interleaved([phase3_tile(2 * pair), phase3_tile(2 * pair + 1)])
            c.dma("sp", x2Td.rearrange("(k p) t -> p k t", p=128)[:, :, blk * 512:(blk + 1) * 512], x2T[:], reads=[x2T], writes=[x2Td_T])
        barrier(c)
        s.es.close()

    TB2 = min(TB2, NT)
    NB2 = NT // TB2
    NT2 = TB2 // 128
    NH2 = TB2 // 512
    s = Ctx.__new__(Ctx); s.__dict__.update(c.__dict__); s.es = ExitStack()
    load_ln(s, 2)
    x2Tb = s.sb("x2Tb", [128, 8, TB2], BF16)
    actT_raw = s.es.enter_context(nc.sbuf_tensor("actT", [128, NFF, TB2], BF16))
    actT = [T(actT_raw[:, f, :], "actT%d" % f) for f in range(NFF)]
    acc_raw = s.es.enter_context(nc.sbuf_tensor("acc", [128, NT2, D], F32))
    acc = [T(acc_raw[:, t, :], "acc%d" % t) for t in range(NT2)]
    w1g_p = Pool(s, "w1g", [128, 8, 256], BF16, 2); w3g_p = Pool(s, "w3g", [128, 8, 256], BF16, 2)
    w2q_p = Pool(s, "w2q", [128, NFF, 256], BF16, 2)
    ph1 = Pool(s, "ph1", [128, 512], F32, 2, ps=True); ph3 = Pool(s, "ph3", [128, 512], F32, 2, ps=True)
    pout = Pool(s, "pout", [128, 512], F32, 2, ps=True)
    ptp = Pool(s, "ptp2", [128, 8, 128], BF16, 2, ps=True)
    sil_p = Pool(s, "sil", [128, 512], F32, 2)
    x2l_p = Pool(s, "x2l", [128, D], F32, 2)
    h_p = Pool(s, "h2", [128, D], F32, 2)
    x3_p = Pool(s, "x3", [128, D], F32, 2)
    x3b_p = Pool(s, "x3b", [128, D], BF16, 2)
    x3T = s.sb("x3T", [128, 8, 512], BF16)
    def phase_a_gen(e):
        w1v = w1[e].rearrange("(k p) f -> p k f", p=128)
        w3v = w3[e].rearrange("(k p) f -> p k f", p=128)
        for ffg in range(NFF // 2):
            w1g = w1g_p.next(); w3g = w3g_p.next()
            c.dma("pool", w1g[:], w1v[:, :, ffg * 256:(ffg + 1) * 256], writes=[w1g])
            c.dma("pool", w3g[:], w3v[:, :, ffg * 256:(ffg + 1) * 256], writes=[w3g])
            for fc in range(2):
                f = ffg * 2 + fc
                for tb in range(NH2):
                    p1 = ph1.next(); p3 = ph3.next()
                    for k in range(8):
                        c.mm(p1[:], w1g[:, k, fc * 128:(fc + 1) * 128], x2Tb[:, k, tb * 512:(tb + 1) * 512], k == 0, k == 7, [w1g, x2Tb], [p1])
                    for k in range(8):
                        c.mm(p3[:], w3g[:, k, fc * 128:(fc + 1) * 128], x2Tb[:, k, tb * 512:(tb + 1) * 512], k == 0, k == 7, [w3g, x2Tb], [p3])
                    sl = sil_p.next()
                    c.act(sl[:], p1[:], AF.Silu, [p1], [sl])
                    c.op("dve", lambda f=f, tb=tb, sl=sl, p3=p3: nc.vector.tensor_tensor(actT[f][:, tb * 512:(tb + 1) * 512], sl[:], p3[:], ALU.mult),
                         [sl, p3], [actT[f]])
                    yield

    def tail_tile(t0, t):
        r0 = t0 + t * 128
        x2l = x2l_p.next()
        c.dma("sp", x2l[:], x2d[r0:r0 + 128, :], reads=[x2d_T], writes=[x2l])
        h = h_p.next()
        yield
        c.op("dve", lambda: nc.vector.scalar_tensor_tensor(h[:], x2l[:], ALPHA, acc[t][:], ALU.mult, ALU.add), [x2l, acc[t]], [h])
        yield
        x3 = x3_p.next()
        yield from layer_norm_gen(c, P, h, gbc[2], bbc[2], x3)
        c.dma("sp", xo[r0:r0 + 128, :], x3[:], reads=[x3])
        x3b = x3b_p.next()
        c.op("act", lambda: nc.scalar.copy(x3b[:], x3[:]), [x3], [x3b])
        yield
        tp = ptp.next()
        for k in range(8):
            c.tr(tp[:, k, :], x3b[:, k * 128:(k + 1) * 128], ident_bf[:], [x3b, ident_bf], [tp])
        yield
        tq = t % 4
        c.op("act", lambda: nc.scalar.copy(x3T[:, :, tq * 128:(tq + 1) * 128], tp[:]), [tp], [x3T])
        if tq == 3:
            cb = t0 + (t // 4) * 512
            c.dma("sp", xoT.rearrange("(k p) t -> p k t", p=128)[:, :, cb:cb + 512], x3T[:], reads=[x3T])
        yield

    def tail_gen(t0):
        for pair in range(NT2 // 2):
            gens = [tail_tile(t0, 2 * pair), tail_tile(t0, 2 * pair + 1)]
            while gens:
                for g in list(gens):
                    try:
                        next(g)
                    except StopIteration:
                        gens.remove(g)
                yield

    pending = None
    for b2 in range(NB2):
        t0 = b2 * TB2
        c.dma("sp", x2Tb[:], x2Td.rearrange("(k p) t -> p k t", p=128)[:, :, t0:t0 + TB2], reads=[x2Td_T], writes=[x2Tb])
        for e in range(nexp):
            w2v = w2[e].rearrange("(f p) n -> p f n", p=128)
            gens = [phase_a_gen(e)]
            if pending is not None:
                gens.append(pending)
                pending = None
            run_interleaved(gens)
            for qr in range(4):
                w2q = w2q_p.next()
                for f0 in range(0, NFF, 7):
                    c.dma("pool", w2q[:, f0:f0 + 7, :], w2v[:, f0:f0 + 7, qr * 256:(qr + 1) * 256], writes=[w2q])
                for t in range(NT2):
                    po = pout.next()
                    for f in range(NFF):
                        c.mm(po[:, 0:256], actT[f][:, t * 128:(t + 1) * 128], w2q[:, f, :], f == 0, f == NFF - 1, [actT[f], w2q], [po])
                    dst = acc[t][:, qr * 256:(qr + 1) * 256]
                    if not moe:
                        c.op("act", lambda dst=dst, po=po: nc.scalar.copy(dst, po[:, 0:256]), [po], [acc[t]])
                    else:
                        gt = gates[:, b2 * NT2 + t, e:e + 1]
                        if e == 0:
                            c.op("dve", lambda dst=dst, po=po, gt=gt: nc.vector.tensor_scalar(dst, po[:, 0:256], gt, None, ALU.mult), [po, gates], [acc[t]])
                        else:
                            c.op("dve", lambda dst=dst, po=po, gt=gt: nc.vector.scalar_tensor_tensor(dst, po[:, 0:256], gt, dst, ALU.mult, ALU.add), [po, gates, acc[t]], [acc[t]])
        pending = tail_gen(t0)
    run_interleaved([pending])
    barrier(c)
    s.es.close()
    c.close()
    print("B: instructions", c.ninst, "waits", c.nwait)
    return nc

DO_SSD = True
DO_FOX = True
DO_POOL = True
STOP = 99
class StopBuild(Exception):
    pass
def ckpt(k):
    if k >= STOP:
        raise StopBuild()
TM = 9
DO_QF = True
DO_K = True
DO_PW = True

D = 1024
NFM = 704
NTM = 196
NEG = -30000.0


def consts_A():
    i = np.arange(128)
    same = (i[:, None] // 64) == (i[None, :] // 64)
    tri = ((i[:, None] <= i[None, :]) & same).astype(np.float32)
    blk = same.astype(np.float32)
    umask = ((i[:, None] > i[None, :]) & same).astype(np.float32)
    neg = np.where((i[:, None] <= i[None, :]) & same, 0.0, NEG).astype(np.float32)
    cm = np.stack([(i < 64), (i >= 64)], 1).astype(np.float32)
    cmask = np.concatenate([tri, blk, umask, neg, cm, np.ones((128, 128), np.float32)], 1)
    q = np.arange(512)
    dm = np.stack([np.where((jj * 128 + i[:, None]) <= q[None, :], 0.0, NEG) for jj in range(4)], 1).astype(np.float32)
    sel = np.zeros((6, 8), np.float32)
    sel[0, 0] = 1; sel[1, 1] = 1; sel[2, 2] = 1; sel[3:6, 3] = 1
    sel[3, 4] = -1; sel[4, 5] = -1; sel[5, 6] = -1; sel[0:3, 7] = 1
    return {"cmask": cmask, "dmask": dm, "sel": sel}


def build_A(TT):
    nc = bass.Bass("TRN2", target_bir_lowering=False)
    din = lambda name, shape, dt=F32: nc.dram_tensor(name, list(shape), dt, kind="ExternalInput").ap()
    xT = din("xT", [D, TT], BF16)
    w_fm = din("w_fm", [D, NFM]); w_tm = din("w_tm", [D, NTM])
    conv_w = din("conv_w", [128, 3, 4]); conv_b = din("conv_b", [128, 3])
    pp = din("pp", [8])
    pool_wb = din("pool_wb", [65, 64]); pool_scale = din("pool_scale", [64])
    pool_coef = din("pool_coef", [64, 4]); pool_fix = din("pool_fix", [64, 16])
    cmask_d = din("cmask", [128, 4 * 128 + 2 + 128]); dmask_d = din("dmask", [128, 4, 512]); sel_d = din("sel", [6, 8])
    y = nc.dram_tensor("y", [TT, 256], F32, kind="ExternalOutput").ap()

    c = Ctx(nc)
    NBLK = TT // 512
    NTILE = TT // 128

    ident_bf = make_ident(c, "ident_bf", BF16)
    ident_f = make_ident(c, "ident_f", F32)
    cm = c.sb("cm", [128, 4 * 128 + 2 + 128])
    c.dma("sp", cm[:], cmask_d, writes=[cm])
    TRI = cm[:, 0:128]; BLK = cm[:, 128:256]; UMASK = cm[:, 256:384]; NEGM = cm[:, 384:512]
    CMK = cm[:, 512:514]; ONES = cm[:, 514:642]
    dmask = c.sb("dmask_sb", [128, 4, 512], BF16)
    c.dma("pool", dmask[:], dmask_d, writes=[dmask])
    sel = c.sb("sel_sb", [6, 8])
    c.dma("sp", sel[:], sel_d, writes=[sel])
    wfm = c.sb("wfm", [128, 8, NFM], BF16); wtm = c.sb("wtm", [128, 8, NTM], BF16)
    c.dma("pool", wfm[:], w_fm.rearrange("(k p) n -> p k n", p=128), writes=[wfm])
    c.dma("pool", wtm[:], w_tm.rearrange("(k p) n -> p k n", p=128), writes=[wtm])
    cw = c.sb("cw", [128, 3, 4]); cb = c.sb("cb", [128, 3])
    c.dma("sp", cw[:], conv_w, writes=[cw]); c.dma("sp", cb[:], conv_b, writes=[cb])
    ppb = c.sb("ppb", [128, 8])
    c.dma("sp", ppb[:], pp.partition_broadcast(128), writes=[ppb])
    abc = c.sb("abc", [128, 2])
    c.act(abc[:], ppb[:, 2:4], AF.Exp, [ppb], [abc])
    c.op("dve", lambda: nc.vector.tensor_scalar(abc[:], abc[:], -1.0, None, ALU.mult), [abc], [abc])
    nfb = c.sb("nfb", [128, 1])
    c.op("dve", lambda: nc.vector.tensor_scalar(nfb[:], ppb[:, 6:7], -1.0, None, ALU.mult), [ppb], [nfb])
    dtbias4 = c.sb("dtbias4", [128, 4, 2])
    for tt in range(4):
        c.op("dve", lambda tt=tt: nc.vector.tensor_copy(dtbias4[:, tt, :], ppb[:, 0:2]), [ppb], [dtbias4])
    abc4 = c.sb("abc4", [128, 4, 2])
    for tt in range(4):
        c.op("dve", lambda tt=tt: nc.vector.tensor_copy(abc4[:, tt, :], abc[:]), [abc], [abc4])
    DI = [c.sb("DI%d" % h, [128, 128], BF16) for h in range(2)]
    for h in range(2):
        c.op("dve", lambda h=h: nc.vector.tensor_scalar(DI[h][:], ident_bf[:], ppb[:, 4 + h:5 + h], None, ALU.mult), [ident_bf, ppb], [DI[h]])
    one_t = c.sb("one_t", [128, 1])
    c.op("pool", lambda: nc.gpsimd.memset(one_t[:], 1.0), [], [one_t])
    pwb = c.sb("pwb", [65, 64]); psc = c.sb("psc", [65, 64]); pw_bf = c.sb("pw_bf", [65, 64], BF16)
    c.dma("sp", pwb[:], pool_wb, writes=[pwb])
    c.dma("sp", psc[:], pool_scale.partition_broadcast(65), writes=[psc])
    c.op("dve", lambda: nc.vector.tensor_tensor(pw_bf[:], pwb[:], psc[:], ALU.mult), [pwb, psc], [pw_bf])
    pcoef = c.sb("pcoef", [64, 4]); pfix = c.sb("pfix", [64, 16])
    c.dma("sp", pcoef[:], pool_coef, writes=[pcoef]); c.dma("sp", pfix[:], pool_fix, writes=[pfix])

    try:
      ckpt(1)
    except StopBuild:
      barrier(c); c.close(); return nc
    KT = c.sb("KT", [128, TT], BF16)
    VA = c.sb("VA", [128, NTILE, 66], BF16)
    c.op("pool", lambda: nc.gpsimd.memset(KT[:], 0.0), [], [KT])
    c.op("pool", lambda: nc.gpsimd.memset(VA[:], 1.0), [], [VA])
    state = c.sb("state", [128, 128])
    state_bf = [c.sb("state_bf%d" % i, [128, 128], BF16) for i in range(2)]
    c.op("dve", lambda: nc.vector.memset(state[:], 0.0), [], [state])
    c.op("dve", lambda: nc.vector.memset(state_bf[0][:], 0.0), [], [state_bf[0]])
    ccar = c.sb("ccar", [6, 1])
    c.op("dve", lambda: nc.vector.memset(ccar[:], 0.0), [], [ccar])
    U = [c.sb("U%d" % g, [128, 3 + 512]) for g in range(3)]
    for g in range(3):
        c.op("pool", lambda g=g: nc.gpsimd.memset(U[g][:], 0.0), [], [U[g]])
    PU = c.sb("PU", [64, 16 + 512])
    c.op("pool", lambda: nc.gpsimd.memset(PU[:], 0.0), [], [PU])

    try:
      ckpt(2)
    except StopBuild:
      barrier(c); c.close(); return nc
    xT_p = Pool(c, "xTb", [128, 8, 512], BF16, 2)
    pst = Pool(c, "pst", [128, 512], F32, 2, ps=True)
    ppro = c.ps("ppro", [128, 512], F32)

    class _One:
        def next(self):
            return ppro
    pfm = _One()
    pacc = c.ps("pacc", [128, 512], F32)
    tA = [c.ps("tA%d" % u, [128, 512], F32) for u in range(2)]
    tB = [c.ps("tB%d" % u, [128, 512], F32) for u in range(2)]
    cacc3 = [c.sb("cacc%d" % g, [128, 512]) for g in range(3)]
    zsb_p = Pool(c, "zsb", [128, 4, 128], F32, 2)
    fmT = [Pool(c, "fmT%d" % g, [128, 512], BF16, 2) for g in range(3)]
    QT_p = Pool(c, "QT", [128, 512], BF16, 2)
    for qq in QT_p.ts:
        c.op("pool", lambda qq=qq: nc.gpsimd.memset(qq[:], 0.0), [], [qq])
    f6_p = Pool(c, "f6", [6, 512], F32, 2); lf_p = Pool(c, "lf", [6, 512], F32, 2); cc_p = Pool(c, "cc", [6, 512], F32, 2)
    ones6 = c.sb("ones6", [6, 512])
    c.op("pool", lambda: nc.gpsimd.memset(ones6[:], 1.0), [], [ones6])
    hi_p = Pool(c, "hi", [6, 512], BF16, 2); mid_p = Pool(c, "mid", [6, 512], BF16, 2); lo_p = Pool(c, "lo", [6, 512], BF16, 2)
    r1_p = Pool(c, "r1", [6, 512], F32, 2); r2_p = Pool(c, "r2", [6, 512], F32, 2)
    aq_p = Pool(c, "aq", [6, 512], F32, 2); ak_p = Pool(c, "ak", [6, 512], F32, 2)
    ztmb_p = Pool(c, "ztmb", [128, 4, 128], F32, 2); dtrb_p = Pool(c, "dtrb", [128, 4, 2], F32, 2); dtb_p = Pool(c, "dtb", [128, 4, 2], F32, 2)
    smb_p = Pool(c, "smb", [128, 32], F32, 2); exb_p = Pool(c, "exb", [128, 32], F32, 2)
    Ab_p = Pool(c, "Ab", [128, 4, 2], F32, 2); A4b_p = Pool(c, "A4b", [128, 4, 4], F32, 2)
    dtdb_p = Pool(c, "dtdb", [128, 4, 2], F32, 2)
    xstm_p = Pool(c, "xstm", [128, 128], BF16, 2); btm_p = Pool(c, "btm", [128, 128], BF16, 2)
    X_p = Pool(c, "X", [128, 128], BF16, 2); Xd_p = Pool(c, "Xd", [128, 128], BF16, 2)
    UA_p = Pool(c, "UA", [128, 128], F32, 4); L_p = Pool(c, "L", [128, 128], F32, 4); MT_p = Pool(c, "MT", [128, 128], BF16, 4)
    pysb_p = Pool(c, "pysb", [128, 128], F32, 2); t1_p = Pool(c, "t1", [128, 128], F32, 2)
    zs_p = Pool(c, "zs", [128, 128], F32, 2)
    yo_p = Pool(c, "yo", [128, 256], F32, 8)
    PT_p = Pool(c, "PT", [128, 512], BF16, 3)
    osb_p = Pool(c, "osb", [65, 512], F32, 2); pfs_p = Pool(c, "pfs", [128, 260], F32, 2); rd_p = Pool(c, "rd", [128, 4], F32, 2)
    ps2 = [c.sb("ps%d" % i, [64, 16 + 512]) for i in range(4)]
    pmean = c.sb("pmean", [64, 512]); ptmp = c.sb("ptmp", [64, 512]); paug_p = Pool(c, "paug", [65, 512], BF16, 2)
    for pa in paug_p.ts:
        c.op("pool", lambda pa=pa: nc.gpsimd.memset(pa[:], 1.0), [], [pa])

    xTv = xT.rearrange("(k p) t -> p k t", p=128)
    sbf_i = 0

    BC = {}

    def prologue(blk):
        nonlocal xb_next
        t0 = blk * 512
        if blk == 0:
            xb_next = xT_p.next()
            c.dma("sp", xb_next[:], xTv[:, :, 0:512], writes=[xb_next])
        xb = xb_next
        if blk + 1 < NBLK:
            xb_next = xT_p.next()
            c.dma("sp", xb_next[:], xTv[:, :, t0 + 512:t0 + 1024], writes=[xb_next])
        for g in range(3):
            c.op("pool", lambda g=g: nc.gpsimd.tensor_copy(U[g][:, 0:3], U[g][:, 512:515]), [U[g]], [U[g]])
            pg = pfm.next()
            for k in range(8):
                c.mm(pg[:], wfm[:, k, g * 128:(g + 1) * 128], xb[:, k, :], k == 0, k == 7, [wfm, xb], [pg])
            c.op("act", lambda g=g, pg=pg: nc.scalar.copy(U[g][:, 3:515], pg[:]), [pg], [U[g]])
            ca = cacc3[g]
            c.act(ca[:], U[g][:, 0:512], AF.Identity, [U[g], cw, cb], [ca], bias=cb[:, g:g + 1], scale=cw[:, g, 0:1])
            for kk in range(1, 4):
                c.op("dve", lambda g=g, kk=kk: nc.vector.scalar_tensor_tensor(ca[:], U[g][:, kk:kk + 512], cw[:, g, kk:kk + 1], ca[:], ALU.mult, ALU.add),
                     [U[g], cw, ca], [ca])
            yield
        if DO_QF:
            yield
            QT = QT_p.next()
            pg = pfm.next()
            for k in range(8):
                c.mm(pg[:], wfm[:, k, 384:512], xb[:, k, :], k == 0, k == 7, [wfm, xb], [pg])
            c.op("act", lambda: nc.scalar.copy(QT[64:128, :], pg[64:128, :]), [pg], [QT])
            yield
            f6 = f6_p.next(); lf = lf_p.next(); cc = cc_p.next()
            c.act(f6[:], pg[0:6, :], AF.Exp, [pg, nfb], [f6], bias=nfb[0:6, 0:1], scale=-1.0)
            c.act(lf[:], f6[:], AF.Ln, [f6, one_t], [lf], bias=one_t[0:6, 0:1], scale=1.0)
            c.op("dve", lambda: nc.vector.tensor_tensor_scan(cc[:], ones6[:], lf[:], ccar[:, 0:1], ALU.mult, ALU.subtract), [ones6, lf, ccar], [cc])
            c.op("dve", lambda: nc.vector.tensor_copy(ccar[:], cc[:, 511:512]), [cc], [ccar])
            yield
            hi = hi_p.next(); mid = mid_p.next(); lo = lo_p.next(); r1 = r1_p.next(); r2 = r2_p.next()
            c.op("act", lambda: nc.scalar.copy(hi[:], cc[:]), [cc], [hi])
            c.op("dve", lambda: nc.vector.tensor_tensor(r1[:], cc[:], hi[:], ALU.subtract), [cc, hi], [r1])
            c.op("act", lambda: nc.scalar.copy(mid[:], r1[:]), [r1], [mid])
            c.op("dve", lambda: nc.vector.tensor_tensor(r2[:], r1[:], mid[:], ALU.subtract), [r1, mid], [r2])
            c.op("act", lambda: nc.scalar.copy(lo[:], r2[:]), [r2], [lo])
            yield
            aq = aq_p.next(); ak = ak_p.next()
            for (dst, co, final) in ((aq, 0, QT[0:6, :]), (ak, 4, KT[0:6, t0:t0 + 512])):
                c.op("dve", lambda dst=dst, co=co: nc.vector.tensor_scalar(dst[:], hi[:], sel[:, co:co + 1], sel[:, (3 if co == 0 else 7):(4 if co == 0 else 8)], ALU.mult, ALU.add), [hi, sel], [dst])
                c.op("dve", lambda dst=dst, co=co: nc.vector.scalar_tensor_tensor(dst[:], mid[:], sel[:, co + 1:co + 2], dst[:], ALU.mult, ALU.add), [mid, sel, dst], [dst])
                tgt = QT if co == 0 else KT
                c.op("dve", lambda dst=dst, co=co, final=final: nc.vector.scalar_tensor_tensor(final, lo[:], sel[:, co + 2:co + 3], dst[:], ALU.mult, ALU.add), [lo, sel, dst], [tgt])
        if DO_K:
            yield
            pg = pfm.next()
            for k in range(8):
                c.mm(pg[:], wfm[:, k, 512:640], xb[:, k, :], k == 0, k == 7, [wfm, xb], [pg])
            c.act(KT[64:128, t0:t0 + 512], pg[64:128, :], AF.Identity, [pg], [KT], scale=0.125)
        if DO_PW:
            yield
            c.op("pool", lambda: nc.gpsimd.tensor_copy(PU[:, 0:16], PU[:, 512:528]), [PU], [PU])
            pg = pfm.next()
            for k in range(8):
                c.mm(pg[0:64, :], wfm[:, k, 640:704], xb[:, k, :], k == 0, k == 7, [wfm, xb], [pg])
            c.op("act", lambda: nc.scalar.copy(PU[:, 16:528], pg[0:64, :]), [pg], [PU])
            yield
            srcs = [PU] + ps2
            for lv in range(4):
                sh = 1 << lv
                lo_i = 2 * sh - 1
                src = srcs[lv]; dstt = ps2[lv]
                c.op("pool", lambda src=src, dstt=dstt, sh=sh, lo_i=lo_i: nc.gpsimd.tensor_tensor(dstt[:, lo_i:528], src[:, lo_i:528], src[:, lo_i - sh:528 - sh], ALU.add), [src], [dstt])
            yield
            c.op("dve", lambda: nc.vector.tensor_scalar(pmean[:], ps2[0][:, 16:528], pcoef[:, 0:1], None, ALU.mult), [ps2[0], pcoef], [pmean])
            for lv in range(1, 4):
                c.op("dve", lambda lv=lv: nc.vector.scalar_tensor_tensor(pmean[:], ps2[lv][:, 16:528], pcoef[:, lv:lv + 1], pmean[:], ALU.mult, ALU.add), [ps2[lv], pcoef, pmean], [pmean])
            yield
            if blk == 0:
                c.op("pool", lambda: nc.gpsimd.tensor_tensor(pmean[:, 0:16], pmean[:, 0:16], pfix[:], ALU.mult), [pmean, pfix], [pmean])
            paug = paug_p.next()
            c.op("pool", lambda: nc.gpsimd.tensor_tensor(paug[0:64, :], pmean[:], PU[:, 16:528], ALU.subtract), [pmean, PU], [paug])

        ztmb = ztmb_p.next(); dtrb = dtrb_p.next(); dtb = dtb_p.next()
        for tt in range(4):
            ti = blk * 4 + tt
            cs = slice(tt * 128, (tt + 1) * 128)
            ptm = pfm.next()
            for k in range(8):
                c.mm(ptm[:, 0:NTM], xb[:, k, cs], wtm[:, k, :], k == 0, k == 7, [xb, wtm], [ptm])
            c.op("act", lambda: nc.scalar.copy(ztmb[:, tt, :], ptm[:, 0:128]), [ptm], [ztmb])
            if TM >= 2: c.op("act", lambda: nc.scalar.copy(VA[:, ti, 0:64], ptm[:, 128:192]), [ptm], [VA])
            if TM >= 3: c.op("act", lambda: nc.scalar.copy(dtrb[:, tt, :], ptm[:, 192:194]), [ptm], [dtrb])
            yield
        yield
        fts = []
        for g in range(3):
            ft = fmT[g].next()
            c.act(ft[:], cacc3[g][:], AF.Silu, [cacc3[g]], [ft])
            fts.append(ft)
        xsT, BT, CT = fts
        zsb = zsb_p.next()
        c.act(zsb[:], ztmb[:], AF.Silu, [ztmb], [zsb])
        yield
        if TM >= 3: c.op("dve", lambda: nc.vector.tensor_tensor(dtrb[:], dtrb[:], dtbias4[:], ALU.add), [dtrb, dtbias4], [dtrb])
        if TM >= 4: c.act(dtrb[:], dtrb[:], AF.Exp, [dtrb], [dtrb])
        if TM >= 5: c.act(dtb[:], dtrb[:], AF.Ln, [dtrb, one_t], [dtb], bias=one_t[:, 0:1], scale=1.0)
        Ab = Ab_p.next(); A4b = A4b_p.next()
        c.op("dve", lambda: nc.vector.tensor_tensor(Ab[:], dtb[:], abc4[:], ALU.mult), [dtb, abc4], [Ab])
        for ck in range(2):
            c.op("dve", lambda ck=ck: nc.vector.tensor_scalar(A4b[:, :, ck * 2:ck * 2 + 2], Ab[:], CMK[:, ck:ck + 1], None, ALU.mult), [Ab, cm], [A4b])
        yield
        smb = smb_p.next(); exb = exb_p.next(); dtdb = dtdb_p.next()
        c.mm(ppro[:, 0:8], TRI, Ab[:].rearrange("p a b -> p (a b)"), True, True, [cm, Ab], [ppro])
        c.mm(ppro[:, 8:16], BLK, Ab[:].rearrange("p a b -> p (a b)"), True, True, [cm, Ab], [ppro])
        c.mm(ppro[:, 16:32], ONES, A4b[:].rearrange("p a b -> p (a b)"), True, True, [cm, A4b], [ppro])
        c.op("act", lambda: nc.scalar.copy(smb[:], ppro[:, 0:32]), [ppro], [smb])
        yield
        c.op("dve", lambda: nc.vector.tensor_tensor(smb[:, 8:16], smb[:, 8:16], smb[:, 0:8], ALU.subtract), [smb], [smb])
        yield
        c.act(exb[:], smb[:], AF.Exp, [smb], [exb])
        yield
        c.op("dve", lambda: nc.vector.tensor_tensor(dtdb[:].rearrange("p a b -> p (a b)"), dtb[:].rearrange("p a b -> p (a b)"), exb[:, 8:16], ALU.mult), [dtb, exb], [dtdb])
        BC[blk] = (xsT, BT, CT, QT, paug, ztmb, dtb, Ab, A4b, exb, dtdb, zsb)
        yield

    def tiles_fox(blk):
        t0 = blk * 512
        xsT, BT, CT, QT, paug, ztmb, dtb, Ab, A4b, exb, dtdb, zsb = BC.pop(blk)
        nkt = 4 * blk + 4
        def fox_steps():
            prev = None
            for j in range(nkt + 1):
                cur = None
                if j < nkt:
                    ps_ = pst.next()
                    diag = j >= 4 * blk
                    c.mm(ps_[:], KT[:, j * 128:(j + 1) * 128], QT[:], True, not diag, [KT, QT], [ps_])
                    if diag:
                        c.mm(ps_[:], ident_bf[:], dmask[:, j - 4 * blk, :], False, True, [ident_bf, dmask], [ps_])
                    cur = (j, ps_)
                if prev is not None:
                    pj, pps = prev
                    PT = PT_p.next()
                    c.act(PT[:], pps[:], AF.Exp, [pps], [PT])
                    c.mm(pacc[0:65, :], VA[:, pj, 0:65], PT[:], pj == 0, pj == nkt - 1, [VA, PT], [pacc])
                prev = cur
                yield
        fsteps = fox_steps()
        per_tile = (nkt + 1 + 3) // 4

        yos = []

        def ssd_tile(tt):
            nonlocal sbf_i
            u = tt % 2
            bA = tA[u]; bB = tB[u]
            cs = slice(tt * 128, (tt + 1) * 128)
            ztm = ztmb[:, tt, :]; dt_ = dtb[:, tt, :]
            yo = yo_p.next()
            yos.append(yo)
            A_ = Ab[:, tt, :]
            dtd = dtdb[:, tt, :]
            UAs = []
            for h in range(2):
                UA = UA_p.next()
                c.act(UA[:], UMASK, AF.Identity, [cm, Ab], [UA], scale=A_[:, h:h + 1])
                UAs.append(UA)
            ptb = bA[:].bitcast(BF16)
            c.tr(ptb[:, 0:128], xsT[:, cs], ident_bf[:], [xsT, ident_bf], [bA])
            c.tr(ptb[:, 128:256], BT[:, cs], ident_bf[:], [BT, ident_bf], [bA])
            c.mm(bB[:, 0:128], BT[:, cs], CT[:, cs], True, True, [BT, CT], [bB])
            yield
            xstm = xstm_p.next(); btm = btm_p.next()
            c.op("act", lambda: nc.scalar.copy(xstm[:], ptb[:, 0:128]), [bA], [xstm])
            c.op("act", lambda: nc.scalar.copy(btm[:], ptb[:, 128:256]), [bA], [btm])
            X = X_p.next(); Xd = Xd_p.next()
            for h in range(2):
                hs = slice(h * 64, (h + 1) * 64)
                c.act(X[:, hs], ptb[:, hs], AF.Identity, [bA, dtb], [X], scale=dt_[:, h:h + 1])
            for h in range(2):
                hs = slice(h * 64, (h + 1) * 64)
                c.act(Xd[:, hs], ptb[:, hs], AF.Identity, [bA, dtdb], [Xd], scale=dtd[:, h:h + 1])
            for h in range(2):
                c.mm(bB[:, 128 * (h + 1):128 * (h + 2)], UAs[h][:], TRI, True, False, [UAs[h], cm], [bB])
                c.mm(bB[:, 128 * (h + 1):128 * (h + 2)], ident_f[:], NEGM, False, True, [ident_f, cm], [bB])
            yield
            Ls = []
            for h in range(2):
                L = L_p.next()
                c.act(L[:], bB[:, 128 * (h + 1):128 * (h + 2)], AF.Exp, [bB], [L])
                Ls.append(L)
            yield
            MTs = []
            for h in range(2):
                MT = MT_p.next()
                c.op("dve", lambda h=h, MT=MT: nc.vector.tensor_tensor(MT[:], Ls[h][:], bB[:, 0:128], ALU.mult), [Ls[h], bB], [MT])
                MTs.append(MT)
            yield
            for h in range(2):
                hs = slice(h * 64, (h + 1) * 64)
                c.mm(bA[:, hs], MTs[h][:], X[:, hs], True, False, [MTs[h], X], [bA])
                c.mm(bA[:, hs], DI[h][:], xstm[:, hs], False, True, [DI[h], xstm], [bA])
            yield
            for ck in range(2):
                rs_ = slice(ck * 64, (ck + 1) * 64)
                lcs = slice(tt * 128 + ck * 64, tt * 128 + (ck + 1) * 64)
                sb_cur = state_bf[sbf_i]
                c.mm(bA[:, 256 + ck * 128:256 + (ck + 1) * 128], btm[rs_, :], Xd[rs_, :], True, True, [btm, Xd], [bA])
                c.mm(bA[rs_, 128:256], CT[:, lcs], sb_cur[:], True, True, [CT, sb_cur], [bA])
                for h in range(2):
                    hs = slice(h * 64, (h + 1) * 64)
                    c.op("dve", lambda h=h, hs=hs, ck=ck: nc.vector.scalar_tensor_tensor(state[:, hs], state[:, hs], exb[:, 16 + tt * 4 + ck * 2 + h:17 + tt * 4 + ck * 2 + h],
                                                                                         bA[:, 256 + ck * 128 + h * 64:256 + ck * 128 + (h + 1) * 64], ALU.mult, ALU.add),
                         [state, exb, bA], [state])
                sbf_i = 1 - sbf_i
                nb_ = state_bf[sbf_i]
                c.op("act", lambda nb_=nb_: nc.scalar.copy(nb_[:], state[:]), [state], [nb_])
            yield
            pysb = pysb_p.next(); t1 = t1_p.next()
            c.op("act", lambda: nc.scalar.copy(pysb[:], bA[:, 0:128]), [bA], [pysb])
            c.mm(bB[:, 392:456], paug[0:65, cs], pw_bf[:], True, True, [paug, pw_bf], [bB])
            yield
            for h in range(2):
                hs = slice(h * 64, (h + 1) * 64)
                c.op("dve", lambda h=h, hs=hs: nc.vector.scalar_tensor_tensor(t1[:, hs], bA[:, 128 + h * 64:128 + (h + 1) * 64], exb[:, tt * 2 + h:tt * 2 + h + 1], pysb[:, hs], ALU.mult, ALU.add),
                     [bA, exb, pysb], [t1])
            c.op("act", lambda: nc.scalar.copy(yo[:, 192:256], bB[:, 392:456]), [bB], [yo])
            yield
            c.op("pool", lambda: nc.gpsimd.tensor_tensor(yo[:, 0:128], t1[:], zsb[:, tt, :], ALU.mult), [t1, zsb], [yo])
            yield

        NROUND = 10
        fox_per_round = (nkt + 1 + 2 * NROUND - 1) // (2 * NROUND)
        for pair in range(2):
            gens = [ssd_tile(2 * pair), ssd_tile(2 * pair + 1)]
            while gens:
                for g in list(gens):
                    try:
                        next(g)
                    except StopIteration:
                        gens.remove(g)
                for _ in range(fox_per_round):
                    next(fsteps, None)
                yield
        for _ in fsteps:
            yield
        if DO_FOX:
            osb = osb_p.next(); rd = rd_p.next()
            c.op("act", lambda: nc.scalar.copy(osb[:], pacc[0:65, :]), [pacc], [osb])
            pf = tB[0]
            for tt in range(4):
                c.tr(pf[:, tt * 65:(tt + 1) * 65], osb[:, tt * 128:(tt + 1) * 128], ident_f[0:65, 0:65], [osb, ident_f], [pf])
            pfs = pfs_p.next()
            c.op("act", lambda: nc.scalar.copy(pfs[:], pf[:, 0:260]), [pf], [pfs])
            for tt in range(4):
                c.op("dve", lambda tt=tt: nc.vector.reciprocal(rd[:, tt:tt + 1], pfs[:, tt * 65 + 64:tt * 65 + 65]), [pfs], [rd])
        for tt in range(4):
            yo = yos[tt]
            if DO_FOX: c.op("dve", lambda tt=tt, yo=yo: nc.vector.tensor_scalar(yo[:, 128:192], pfs[:, tt * 65:tt * 65 + 64], rd[:, tt:tt + 1], None, ALU.mult), [pfs, rd], [yo])
            r0 = t0 + tt * 128
            c.dma("sp", y[r0:r0 + 128, :], yo[:], reads=[yo])
        yield

    xb_next = None
    run_interleaved([prologue(0)])
    for blk in range(NBLK):
        gens = [tiles_fox(blk)]
        if blk + 1 < NBLK:
            gens.append(prologue(blk + 1))
        run_interleaved(gens)
    barrier(c)
    c.close()
    print("A: instructions", c.ninst, "waits", c.nwait)
    return nc


def prep_A(w_in, conv_w, conv_b, dt_bias, a_log, d_skip, f_bias, pool_w, pool_b, pool_scale, j):
    g = j // 2
    cz = slice(128 * j, 128 * j + 128)
    cxs = slice(512 + 128 * j, 512 + 128 * j + 128)
    cB = slice(1024 + 128 * g, 1024 + 128 * g + 128)
    cC = slice(1280 + 128 * g, 1280 + 128 * g + 128)
    cdt = slice(1536 + 2 * j, 1536 + 2 * j + 2)
    cq = slice(1544 + 64 * j, 1544 + 64 * j + 64)
    ck = slice(1800 + 64 * j, 1800 + 64 * j + 64)
    cv = slice(2056 + 64 * j, 2056 + 64 * j + 64)
    cf = 2312 + j
    cp = slice(2316 + 64 * j, 2316 + 64 * j + 64)
    w_fm = np.zeros((D, NFM), np.float32)
    w_fm[:, 0:128] = w_in[:, cxs]; w_fm[:, 128:256] = w_in[:, cB]; w_fm[:, 256:384] = w_in[:, cC]
    for r in range(6):
        w_fm[:, 384 + r] = w_in[:, cf]
    w_fm[:, 384 + 64:384 + 128] = w_in[:, cq]
    w_fm[:, 512 + 64:512 + 128] = w_in[:, ck]
    w_fm[:, 640:704] = w_in[:, cp]
    w_tm = np.concatenate([w_in[:, cz], w_in[:, cv], w_in[:, cdt], w_in[:, cf:cf + 1], np.zeros((D, 1), np.float32)], 1).astype(np.float32)
    chans = [np.arange(128 * j, 128 * j + 128), np.arange(512 + 128 * g, 512 + 128 * g + 128), np.arange(768 + 128 * g, 768 + 128 * g + 128)]
    cw = np.stack([conv_w[:, ch].T for ch in chans], 1).astype(np.float32)
    cbb = np.stack([conv_b[ch] for ch in chans], 1).astype(np.float32)
    pp = np.zeros(8, np.float32)
    pp[0:2] = dt_bias[2 * j:2 * j + 2]; pp[2:4] = a_log[2 * j:2 * j + 2]; pp[4:6] = d_skip[2 * j:2 * j + 2]; pp[6] = f_bias[j]
    wb = np.concatenate([pool_w[j], pool_b[j][None, :]], 0).astype(np.float32)
    win = (2, 4, 8, 16)[j]
    coef = np.zeros((64, 4), np.float32); coef[:, j] = 1.0 / win
    fix = np.ones((64, 16), np.float32)
    tpos = np.arange(16)
    fix[:, :] = (win / np.minimum(tpos + 1, win))[None, :]
    return {"w_fm": w_fm, "w_tm": w_tm, "conv_w": np.ascontiguousarray(cw), "conv_b": np.ascontiguousarray(cbb), "pp": pp,
            "pool_wb": wb, "pool_scale": np.ascontiguousarray(pool_scale[64 * j:64 * j + 64]).astype(np.float32),
            "pool_coef": coef, "pool_fix": fix}


def build_P(NT):
    nc = bass.Bass("TRN2", target_bir_lowering=False)
    x = nc.dram_tensor("x", [NT, D], F32, kind="ExternalInput").ap()
    xT = nc.dram_tensor("xT", [D, NT], BF16, kind="ExternalOutput").ap()
    c = Ctx(nc)
    ident_bf = make_ident(c, "ident_bf", BF16)
    xt_p = Pool(c, "xt", [128, D], F32, 3); xb_p = Pool(c, "xb", [128, D], BF16, 2)
    ptp = Pool(c, "ptp", [128, 8, 128], BF16, 2, ps=True)
    xTs_p = Pool(c, "xTs", [128, 8, 512], BF16, 2)
    for blk in range(NT // 512):
        xTs = xTs_p.next()
        for tt in range(4):
            r0 = blk * 512 + tt * 128
            xt = xt_p.next(); xb = xb_p.next()
            c.dma("sp", xt[:], x[r0:r0 + 128, :], writes=[xt])
            c.op("dve", lambda: nc.vector.tensor_copy(xb[:], xt[:]), [xt], [xb])
            tp = ptp.next()
            for k in range(8):
                c.tr(tp[:, k, :], xb[:, k * 128:(k + 1) * 128], ident_bf[:], [xb, ident_bf], [tp])
            c.op("act", lambda: nc.scalar.copy(xTs[:, :, tt * 128:(tt + 1) * 128], tp[:]), [tp], [xTs])
        c.dma("sp", xT.rearrange("(k p) t -> p k t", p=128)[:, :, blk * 512:(blk + 1) * 512], xTs[:], reads=[xTs])
    barrier(c)
    c.close()
    return nc


from concourse.bass_utils import run_bass_kernel_spmd

NCORES = 8
SEQ = 16384
BATCH = 2
NTOK = BATCH * SEQ // NCORES
DEPTH = 4
_CACHE = {}


def _prog(key, fn):
    if key not in _CACHE:
        _CACHE[key] = fn()
    return _CACHE[key]


def _run(nc, in_maps):
    res = run_bass_kernel_spmd(nc, in_maps, core_ids=list(range(NCORES)))
    return res.results


def kernel(**inp):
    f32 = lambda a: np.ascontiguousarray(np.asarray(a, dtype=np.float32))
    x = f32(inp["x"]); mem = f32(inp["mem"])
    xs = x.reshape(NCORES, NTOK, D)
    ncP = _prog("P", lambda: build_P(NTOK))
    resP = _run(ncP, [{"x": xs[c]} for c in range(NCORES)])
    xT_parts = [r["xT"] for r in resP]
    x_cur = [xs[c] for c in range(NCORES)]
    ncA = _prog("A", lambda: build_A(SEQ))
    consts = consts_A()
    for layer in range(DEPTH):
        moe = layer % 2 == 1
        jj = layer // 2
        xT_full = [np.ascontiguousarray(np.concatenate(xT_parts[b * 4:(b + 1) * 4], axis=1)) for b in range(BATCH)]
        in_maps = []
        for c in range(NCORES):
            b, j = divmod(c, 4)
            m = prep_A(f32(inp["w_in"][layer]), f32(inp["ssm_conv_w"][layer]), f32(inp["ssm_conv_b"][layer]),
                       f32(inp["ssm_dt_bias"][layer]), f32(inp["ssm_a_log"][layer]), f32(inp["ssm_d"][layer]),
                       f32(inp["fox_f_bias"][layer]), f32(inp["pool_w"][layer]), f32(inp["pool_b"][layer]),
                       f32(inp["pool_scale"][layer]), j)
            m.update(consts)
            m["xT"] = xT_full[b]
            in_maps.append(m)
        resA = _run(ncA, in_maps)
        mix = np.empty((BATCH, SEQ, D), np.float32)
        for c in range(NCORES):
            b, j = divmod(c, 4)
            y = resA[c]["y"]
            mix[b, :, 128 * j:128 * j + 128] = y[:, 0:128]
            mix[b, :, 512 + 64 * j:512 + 64 * j + 64] = y[:, 128:192]
            mix[b, :, 768 + 64 * j:768 + 64 * j + 64] = y[:, 192:256]
        mixs = mix.reshape(NCORES, NTOK, D)
        ncB = _prog("B%d" % moe, lambda: build_B(NTOK, moe))
        wts = {"normw": f32(inp["ssm_norm_w"][layer]), "w_out": f32(inp["w_out"][layer]),
               "ln1_g": f32(inp["ln1_g"][layer]), "ln1_b": f32(inp["ln1_b"][layer]),
               "ln2_g": f32(inp["ln2_g"][layer]), "ln2_b": f32(inp["ln2_b"][layer]),
               "ln3_g": f32(inp["ln3_g"][layer]), "ln3_b": f32(inp["ln3_b"][layer]),
               "wq": f32(inp["xa_wq"][layer]), "wk": f32(inp["xa_wk"][layer]),
               "wv": f32(inp["xa_wv"][layer]), "wo": f32(inp["xa_wo"][layer])}
        if moe:
            wts.update({"router": f32(inp["router_w"][jj]), "w1": f32(inp["moe_w1"][jj]),
                        "w3": f32(inp["moe_w3"][jj]), "w2": f32(inp["moe_w2"][jj])})
        else:
            wts.update({"w1": f32(inp["ffn_w1"][jj:jj + 1]), "w3": f32(inp["ffn_w3"][jj:jj + 1]),
                        "w2": f32(inp["ffn_w2"][jj:jj + 1])})
        in_maps = []
        for c in range(NCORES):
            m = dict(wts)
            m["mix"] = np.ascontiguousarray(mixs[c]); m["x"] = np.ascontiguousarray(x_cur[c])
            m["mem"] = mem[c // 4]
            in_maps.append(m)
        resB = _run(ncB, in_maps)
        x_cur = [r["xo"] for r in resB]
        xT_parts = [r["xoT"] for r in resB]
    return np.stack(x_cur).reshape(BATCH, SEQ, D).astype(np.float32)
```

```python
from contextlib import ExitStack
import numpy as np
import concourse.bass as bass
import concourse.mybir as mybir

F32 = mybir.dt.float32
BF16 = mybir.dt.bfloat16
AF = mybir.ActivationFunctionType
ALU = mybir.AluOpType
AX = mybir.AxisListType

ENGS = ["pe", "act", "dve", "pool", "sp"]
NDS = 40


class T:
    __slots__ = ("t", "w", "r", "name")

    def __init__(self, t, name=""):
        self.t = t
        self.w = None
        self.r = {}
        self.name = name

    def __getitem__(self, k):
        return self.t[k]


class Ctx:
    LIMIT = 30000
    NDMAX = 128

    def __init__(self, nc):
        self.nc = nc
        self.es = ExitStack()
        self.eng = {"pe": nc.tensor, "act": nc.scalar, "dve": nc.vector,
                    "pool": nc.gpsimd, "sp": nc.sync}
        nd = self.NDMAX
        self.sems = [self.es.enter_context(nc.semaphore("d%d" % i)) for i in range(NDS)]
        self.mult = [16] * NDS
        self.cnt = [0] * nd
        self.snap = [dict() for _ in range(nd)]
        self.known = {k: np.zeros(nd, np.int64) for k in ENGS}
        self.cur = {}
        self.old = {k: [] for k in ENGS}
        self.edims = {k: set() for k in ENGS}
        for k in ENGS:
            self._new_dim(k)
        self.rr = 0
        self.rrq = {}
        self.nwait = 0
        self.ninst = 0

    def _new_dim(self, e):
        d = len(self.sems)
        assert d < self.NDMAX
        self.sems.append(self.es.enter_context(self.nc.semaphore("c_%s_%d" % (e, d))))
        self.mult.append(1)
        if e in self.cur:
            self.old[e].append(self.cur[e])
        self.cur[e] = d
        self.edims[e].add(d)

    def sb(self, name, shape, dt=F32):
        return T(self.es.enter_context(self.nc.sbuf_tensor(name, list(shape), dt)), name)

    def ps(self, name, shape, dt=F32):
        return T(self.es.enter_context(self.nc.psum_tensor(name, list(shape), dt)), name)

    def close(self):
        self.es.close()

    def _wait(self, e, dim, c):
        kn = self.known[e]
        if kn[dim] >= c:
            return
        self.eng[e].wait_ge(self.sems[dim], int(c) * self.mult[dim])
        self.nwait += 1
        s = self.snap[dim].get(c)
        if s is not None:
            np.maximum(kn, s, out=kn)
        kn[dim] = max(kn[dim], c)

    def _deps(self, e, reads, writes, pe_acc=False):
        need = {}
        for t in reads:
            if t.w is not None:
                d, c = t.w
                if need.get(d, 0) < c:
                    need[d] = c
        for t in writes:
            if t.w is not None:
                d, c = t.w
                if not (pe_acc and d in self.edims["pe"]):
                    if need.get(d, 0) < c:
                        need[d] = c
            for d, c in t.r.items():
                if need.get(d, 0) < c:
                    need[d] = c
        for d, c in need.items():
            self._wait(e, d, c)

    def _mark(self, dim, c, reads, writes):
        for t in reads:
            if t.r.get(dim, 0) < c:
                t.r[dim] = c
        for t in writes:
            t.w = (dim, c)
            t.r = {}

    def op(self, e, fn, reads=(), writes=(), pe_acc=False):
        self._deps(e, reads, writes, pe_acc)
        ins = fn()
        if self.cnt[self.cur[e]] >= self.LIMIT:
            self._new_dim(e)
        d = self.cur[e]
        self.cnt[d] += 1
        c = self.cnt[d]
        ins.then_inc(self.sems[d], 1)
        s = self.known[e].copy()
        s[d] = c
        for od in self.old[e]:
            s[od] = self.cnt[od]
        self.snap[d][c] = s
        self._mark(d, c, reads, writes)
        self.ninst += 1
        return ins

    def dma(self, q, out, in_, reads=(), writes=(), **kw):
        self._deps(q, reads, writes)
        kn = self.known[q]
        half = NDS // 2
        base = half if q == "pool" else 0
        rr = self.rrq.get(q, 0)
        pick = None
        for k in range(half):
            i = base + (rr + k) % half
            if kn[i] >= self.cnt[i]:
                pick = i
                break
        if pick is None:
            pick = base + rr % half
            self._wait(q, pick, self.cnt[pick])
        self.rrq[q] = (pick - base + 1) % half
        d = pick
        ins = self.eng[q].dma_start(out=out, in_=in_, **kw)
        self.cnt[d] += 1
        c = self.cnt[d]
        ins.then_inc(self.sems[d], 16)
        s = kn.copy()
        s[d] = c
        self.snap[d][c] = s
        self._mark(d, c, reads, writes)
        self.ninst += 1
        return ins

    def finish(self, e="sp"):
        for d in range(len(self.sems)):
            if self.cnt[d] > 0:
                self._wait(e, d, self.cnt[d])

    def mm(self, out, lhsT, rhs, start, stop, reads, writes):
        nc = self.nc
        return self.op("pe", lambda: nc.tensor.matmul(out, lhsT, rhs, start=start, stop=stop),
                       reads, writes, pe_acc=not start)

    def tr(self, out, in_, ident, reads, writes):
        nc = self.nc
        return self.op("pe", lambda: nc.tensor.transpose(out, in_, ident), reads, writes)

    def act(self, out, in_, func, reads, writes, **kw):
        nc = self.nc
        return self.op("act", lambda: nc.scalar.activation(out, in_, func, **kw), reads, writes)


D = 1024
DFF = 3584
NFF = DFF // 128
MEM = 256
ALPHA = 8.0 ** 0.25
LN_EPS = 1e-5
RMS_EPS = 1e-5
NEXP = 8


class Pool:
    def __init__(self, c, name, shape, dt, n, ps=False):
        self.ts = [(c.ps if ps else c.sb)("%s%d" % (name, i), shape, dt) for i in range(n)]
        self.i = 0

    def next(self):
        t = self.ts[self.i % len(self.ts)]
        self.i += 1
        return t


def make_ident(c, name, dt):
    nc = c.nc
    t = c.sb(name, [128, 128], dt)
    c.op("pool", lambda: nc.gpsimd.memset(t[:], 1.0), [], [t])
    c.op("pool", lambda: nc.gpsimd.affine_select(t[:], t[:], pattern=[[-1, 128]], compare_op=ALU.is_equal,
                                                 fill=0.0, base=0, channel_multiplier=1), [t], [t])
    return t


def barrier(c):
    for e in ENGS:
        c.finish(e)


def layer_norm_gen(c, P, h, g_bc, b_bc, out):
    nc = c.nc
    st = P["st"].next(); mv = P["mv"].next(); rs = P["rs"].next(); nm = P["nm"].next()
    c.op("dve", lambda: nc.vector.bn_stats(st[:, 0, :], h[:, 0:512]), [h], [st])
    c.op("dve", lambda: nc.vector.bn_stats(st[:, 1, :], h[:, 512:1024]), [h], [st])
    yield
    c.op("dve", lambda: nc.vector.bn_aggr(mv[:], st[:].rearrange("p a b -> p (a b)")), [st], [mv])
    yield
    c.act(rs[:], mv[:, 1:2], AF.Sqrt, [mv, P["eps"]], [rs], bias=P["eps"][:, 0:1], scale=1.0)
    yield
    c.op("dve", lambda: nc.vector.reciprocal(rs[:], rs[:]), [rs], [rs])
    yield
    c.op("dve", lambda: nc.vector.scalar_tensor_tensor(nm[:], mv[:, 0:1], -1.0, rs[:], ALU.mult, ALU.mult), [mv, rs], [nm])
    yield
    tmp = P["lnt"].next()
    c.act(tmp[:], h[:], AF.Identity, [h, rs, nm], [tmp], bias=nm[:, 0:1], scale=rs[:, 0:1])
    yield
    c.op("dve", lambda: nc.vector.tensor_tensor(tmp[:], tmp[:], g_bc[:], ALU.mult), [tmp, g_bc], [tmp])
    yield
    c.op("dve", lambda: nc.vector.tensor_tensor(out[:], tmp[:], b_bc[:], ALU.add), [tmp, b_bc], [out])
    yield


def layer_norm(c, P, h, g_bc, b_bc, out):
    for _ in layer_norm_gen(c, P, h, g_bc, b_bc, out):
        pass


def run_interleaved(gens):
    gens = list(gens)
    while gens:
        for g in list(gens):
            try:
                next(g)
            except StopIteration:
                gens.remove(g)


def load_w_bf(c, dst, src_ap, q="pool"):
    v = src_ap.rearrange("(k p) n -> p k n", p=128)
    for k in range(8):
        c.dma(q, dst[:, k, :], v[:, k, :], writes=[dst])


def build_B(NT, moe, TB2=1024):
    nc = bass.Bass("TRN2", target_bir_lowering=False)
    dt_in = lambda name, shape, dt=F32: nc.dram_tensor(name, list(shape), dt, kind="ExternalInput").ap()
    mix = dt_in("mix", [NT, D]); x = dt_in("x", [NT, D]); mem = dt_in("mem", [MEM, D])
    normw = dt_in("normw", [512]); w_out = dt_in("w_out", [D, D])
    ln_g = [dt_in("ln%d_g" % i, [D]) for i in (1, 2, 3)]
    ln_b = [dt_in("ln%d_b" % i, [D]) for i in (1, 2, 3)]
    wq = dt_in("wq", [D, D]); wk = dt_in("wk", [D, D]); wv = dt_in("wv", [D, D]); wo = dt_in("wo", [D, D])
    if moe:
        router = dt_in("router", [D, NEXP])
        w1 = dt_in("w1", [NEXP, D, DFF]); w3 = dt_in("w3", [NEXP, D, DFF]); w2 = dt_in("w2", [NEXP, DFF, D])
    else:
        w1 = dt_in("w1", [1, D, DFF]); w3 = dt_in("w3", [1, D, DFF]); w2 = dt_in("w2", [1, DFF, D])
    xo = nc.dram_tensor("xo", [NT, D], F32, kind="ExternalOutput").ap()
    xoT = nc.dram_tensor("xoT", [D, NT], BF16, kind="ExternalOutput").ap()
    x2d = nc.dram_tensor("x2d", [NT, D], F32, kind="Internal").ap()
    x2Td = nc.dram_tensor("x2Td", [D, NT], BF16, kind="Internal").ap()
    x2d_T = T(x2d, "x2d"); x2Td_T = T(x2Td, "x2Td")

    c = Ctx(nc)
    NTILE = NT // 128
    NBLK = NT // 512
    nexp = NEXP if moe else 1

    ident_bf = make_ident(c, "ident_bf", BF16)
    ones_bf = c.sb("ones_bf", [128, 128], BF16)
    c.op("pool", lambda: nc.gpsimd.memset(ones_bf[:], 1.0), [], [ones_bf])
    gbc = [None] * 3; bbc = [None] * 3
    def load_ln(sc, i):
        gbc[i] = sc.sb("g%d" % i, [128, D]); bbc[i] = sc.sb("b%d" % i, [128, D])
        c.dma("sp", gbc[i][:], ln_g[i].partition_broadcast(128), writes=[gbc[i]])
        c.dma("sp", bbc[i][:], ln_b[i].partition_broadcast(128), writes=[bbc[i]])
    eps_t = c.sb("eps_t", [128, 1])
    c.op("pool", lambda: nc.gpsimd.memset(eps_t[:], LN_EPS), [], [eps_t])
    gates = c.sb("gates", [128, NTILE, NEXP]) if moe else None
    P = {"st": Pool(c, "st", [128, 2, 6], F32, 3), "mv": Pool(c, "mv", [128, 2], F32, 3),
         "rs": Pool(c, "rs", [128, 1], F32, 3), "nm": Pool(c, "nm", [128, 1], F32, 3),
         "lnt": Pool(c, "lnt", [128, D], F32, 2), "eps": eps_t}

    c1 = Ctx.__new__(Ctx); c1.__dict__.update(c.__dict__); c1.es = ExitStack()
    if True:
        s = c1
        load_ln(s, 0); load_ln(s, 1)
        nw_bc = s.sb("nw_bc", [128, 512])
        c.dma("sp", nw_bc[:], normw.partition_broadcast(128), writes=[nw_bc])
        wout_bf = s.sb("wout_bf", [128, 8, D], BF16); wq_bf = s.sb("wq_bf", [128, 8, D], BF16)
        wo_bf = s.sb("wo_bf", [128, 8, D], BF16)
        kT = s.sb("kT", [128, 8, MEM], BF16)
        vv = s.sb("vv", [128, 2, D], BF16)
        ptp = Pool(s, "ptp", [128, 8, 128], BF16, 2, ps=True)
        pmm = Pool(s, "pmm", [128, 512], F32, 4, ps=True)
        if moe:
            ident_f = make_ident(s, "ident_f", F32)
            ptf = [s.ps("ptf%d" % u, [128, 4, 128], F32) for u in range(2)]
            router_f = s.sb("router_f", [128, 8, NEXP])
            c.dma("sp", router_f[:], router.rearrange("(k p) e -> p k e", p=128), writes=[router_f])
        load_w_bf(c, wq_bf, wk)
        load_w_bf(c, wo_bf, wv)
        load_w_bf(c, wout_bf, w_out)
        memT = s.sb("memT", [128, 8, MEM], BF16)
        mt_p = Pool(s, "mt", [128, D], F32, 2)
        xt_p = Pool(s, "xt", [128, D], F32, 2)
        mb_p = Pool(s, "mb", [128, D], BF16, 2)
        for mc in range(2):
            m_f = mt_p.next(); m_b = mb_p.next()
            c.dma("sp", m_f[:], mem[mc * 128:(mc + 1) * 128, :], writes=[m_f])
            c.op("dve", lambda: nc.vector.tensor_copy(m_b[:], m_f[:]), [m_f], [m_b])
            tp = ptp.next()
            for k in range(8):
                c.tr(tp[:, k, :], m_b[:, k * 128:(k + 1) * 128], ident_bf[:], [m_b, ident_bf], [tp])
            c.op("act", lambda: nc.scalar.copy(memT[:, :, mc * 128:(mc + 1) * 128], tp[:]), [tp], [memT])
        for ch in range(8):
            pk = pmm.next()
            for k in range(8):
                c.mm(pk[:, 0:MEM], wq_bf[:, k, ch * 128:(ch + 1) * 128], memT[:, k, :], k == 0, k == 7, [wq_bf, memT], [pk])
            c.op("act", lambda: nc.scalar.copy(kT[:, ch, :], pk[:, 0:MEM]), [pk], [kT])
        for mc in range(2):
            for half in range(2):
                pv = pmm.next()
                for k in range(8):
                    c.mm(pv[:], memT[:, k, mc * 128:(mc + 1) * 128], wo_bf[:, k, half * 512:(half + 1) * 512], k == 0, k == 7, [wo_bf, memT], [pv])
                c.op("dve", lambda: nc.vector.tensor_copy(vv[:, mc, half * 512:(half + 1) * 512], pv[:]), [pv], [vv])
        load_w_bf(c, wq_bf, wq)
        load_w_bf(c, wo_bf, wo)

        mixn_p = Pool(s, "mixn", [128, D], BF16, 2)
        mixT_p = Pool(s, "mixT", [128, 8, 128], BF16, 2)
        ss_p = Pool(s, "ss", [128, 2], F32, 3)
        junk_p = [s.sb("junk%d" % i, [128, 256]) for i in range(2)]
        h_p = Pool(s, "h", [128, D], F32, 2)
        x1_p = [s.sb("x1_%d" % i, [128, D]) for i in range(4)]
        x1b_p = Pool(s, "x1b", [128, D], BF16, 2)
        x1T = s.sb("x1T", [128, 8, 512], BF16)
        qT = s.sb("qT", [128, 8, 512], BF16)
        pT_p = Pool(s, "pT", [128, 2, 512], BF16, 2)
        rden_p = Pool(s, "rden", [128, 512], F32, 2)
        oT = s.sb("oT", [128, 8, 512], BF16)
        x2_p = Pool(s, "x2", [128, D], F32, 2)
        x2T = s.sb("x2T", [128, 8, 512], BF16)
        if moe:
            x2Tf_p = Pool(s, "x2Tf", [128, 8, 128], F32, 2)
            lg_p = Pool(s, "lg", [128, 8], F32, 2); mx_p = Pool(s, "mx", [128, 8], F32, 2)
            nb_p = Pool(s, "nb", [128, 1], F32, 2); ee_p = Pool(s, "ee", [128, 8], F32, 2)
            mk_p = Pool(s, "mk", [128, 8], F32, 2); dn_p = Pool(s, "dn", [128, 1], F32, 2)

        for blk in range(NBLK):
            def phase1_tile(tt):
                ti = blk * 4 + tt
                r0 = ti * 128
                mt = mt_p.next(); xt = xt_p.next()
                c.dma("sp", mt[:], mix[r0:r0 + 128, :], writes=[mt])
                c.dma("sp", xt[:], x[r0:r0 + 128, :], writes=[xt])
                ss = ss_p.next(); mixn = mixn_p.next()
                yield
                for g in range(2):
                    c.act(junk_p[tt % 2][:], mt[:, g * 256:(g + 1) * 256], AF.Square, [mt], [junk_p[tt % 2], ss], accum_out=ss[:, g:g + 1])
                c.op("act", lambda: nc.scalar.copy(mixn[:, 512:1024], mt[:, 512:1024]), [mt], [mixn])
                yield
                c.act(ss[:], ss[:], AF.Sqrt, [ss], [ss], bias=eps_t[:, 0:1], scale=1.0 / 256)
                yield
                c.op("dve", lambda: nc.vector.reciprocal(ss[:], ss[:]), [ss], [ss])
                yield
                for g in range(2):
                    c.op("dve", lambda g=g: nc.vector.scalar_tensor_tensor(mixn[:, g * 256:(g + 1) * 256], mt[:, g * 256:(g + 1) * 256],
                                                                         ss[:, g:g + 1], nw_bc[:, g * 256:(g + 1) * 256], ALU.mult, ALU.mult),
                         [mt, ss, nw_bc], [mixn])
                yield
                tp = ptp.next()
                for k in range(8):
                    c.tr(tp[:, k, :], mixn[:, k * 128:(k + 1) * 128], ident_bf[:], [mixn, ident_bf], [tp])
                yield
                mixT = mixT_p.next()
                c.op("act", lambda: nc.scalar.copy(mixT[:], tp[:]), [tp], [mixT])
                yield
                h = h_p.next()
                pos = []
                for half in range(2):
                    po = pmm.next()
                    for k in range(8):
                        c.mm(po[:], mixT[:, k, :], wout_bf[:, k, half * 512:(half + 1) * 512], k == 0, k == 7, [mixT, wout_bf], [po])
                    pos.append(po)
                yield
                for half in range(2):
                    po = pos[half]
                    c.op("dve", lambda half=half, po=po: nc.vector.scalar_tensor_tensor(h[:, half * 512:(half + 1) * 512], xt[:, half * 512:(half + 1) * 512],
                                                                                 ALPHA, po[:], ALU.mult, ALU.add), [xt, po], [h])
                yield
                x1 = x1_p[tt]
                yield from layer_norm_gen(c, P, h, gbc[0], bbc[0], x1)
                x1b = x1b_p.next()
                c.op("act", lambda: nc.scalar.copy(x1b[:], x1[:]), [x1], [x1b])
                yield
                tp = ptp.next()
                for k in range(8):
                    c.tr(tp[:, k, :], x1b[:, k * 128:(k + 1) * 128], ident_bf[:], [x1b, ident_bf], [tp])
                yield
                c.op("act", lambda: nc.scalar.copy(x1T[:, :, tt * 128:(tt + 1) * 128], tp[:]), [tp], [x1T])
                yield

            for pair in range(2):
                run_interleaved([phase1_tile(2 * pair), phase1_tile(2 * pair + 1)])
            for ch in range(8):
                pq = pmm.next()
                for k in range(8):
                    c.mm(pq[:], wq_bf[:, k, ch * 128:(ch + 1) * 128], x1T[:, k, :], k == 0, k == 7, [wq_bf, x1T], [pq])
                if ch % 2 == 0:
                    c.op("act", lambda: nc.scalar.copy(qT[:, ch, :], pq[:]), [pq], [qT])
                else:
                    c.op("dve", lambda: nc.vector.tensor_copy(qT[:, ch, :], pq[:]), [pq], [qT])
            for hh in range(4):
                pT = pT_p.next()
                for mc in range(2):
                    psc = pmm.next()
                    for cc in range(2):
                        c.mm(psc[:], kT[:, 2 * hh + cc, mc * 128:(mc + 1) * 128], qT[:, 2 * hh + cc, :], cc == 0, cc == 1, [kT, qT], [psc])
                    c.act(pT[:, mc, :], psc[:], AF.Exp, [psc], [pT], scale=1.0 / 16.0)
                pden = pmm.next()
                for mc in range(2):
                    c.mm(pden[:], ones_bf[:], pT[:, mc, :], mc == 0, mc == 1, [ones_bf, pT], [pden])
                rden = rden_p.next()
                c.op("dve", lambda: nc.vector.reciprocal(rden[:], pden[:]), [pden], [rden])
                for cc in range(2):
                    pov = pmm.next()
                    for mc in range(2):
                        c.mm(pov[:], vv[:, mc, (2 * hh + cc) * 128:(2 * hh + cc + 1) * 128], pT[:, mc, :], mc == 0, mc == 1, [vv, pT], [pov])
                    c.op("dve", lambda cc=cc, pov=pov: nc.vector.tensor_tensor(oT[:, 2 * hh + cc, :], pov[:], rden[:], ALU.mult), [pov, rden], [oT])
            def phase3_tile(tt):
                ti = blk * 4 + tt
                r0 = ti * 128
                x1 = x1_p[tt]
                h = h_p.next()
                pos = []
                for half in range(2):
                    po = pmm.next()
                    for k in range(8):
                        c.mm(po[:], oT[:, k, tt * 128:(tt + 1) * 128], wo_bf[:, k, half * 512:(half + 1) * 512], k == 0, k == 7, [oT, wo_bf], [po])
                    pos.append(po)
                yield
                for half in range(2):
                    po = pos[half]
                    c.op("dve", lambda half=half, po=po: nc.vector.scalar_tensor_tensor(h[:, half * 512:(half + 1) * 512], x1[:, half * 512:(half + 1) * 512],
                                                                                 ALPHA, po[:], ALU.mult, ALU.add), [x1, po], [h])
                yield
                x2 = x2_p.next()
                yield from layer_norm_gen(c, P, h, gbc[1], bbc[1], x2)
                c.dma("sp", x2d[r0:r0 + 128, :], x2[:], reads=[x2], writes=[x2d_T])
                x2b = x1b_p.next()
                c.op("act", lambda: nc.scalar.copy(x2b[:], x2[:]), [x2], [x2b])
                if moe:
                    x2Tf = x2Tf_p.next()
                    for k4 in range(4):
                        c.tr(ptf[tt % 2][:, k4, :], x2[:, k4 * 128:(k4 + 1) * 128], ident_f[:], [x2, ident_f], [ptf[tt % 2]])
                yield
                tp = ptp.next()
                for k in range(8):
                    c.tr(tp[:, k, :], x2b[:, k * 128:(k + 1) * 128], ident_bf[:], [x2b, ident_bf], [tp])
                if moe:
                    c.op("act", lambda: nc.scalar.copy(x2Tf[:, 0:4, :], ptf[tt % 2][:]), [ptf[tt % 2]], [x2Tf])
                yield
                c.op("act", lambda: nc.scalar.copy(x2T[:, :, tt * 128:(tt + 1) * 128], tp[:]), [tp], [x2T])
                if moe:
                    for k4 in range(4):
                        c.tr(ptf[tt % 2][:, k4, :], x2[:, (4 + k4) * 128:(5 + k4) * 128], ident_f[:], [x2, ident_f], [ptf[tt % 2]])
                    yield
                    c.op("act", lambda: nc.scalar.copy(x2Tf[:, 4:8, :], ptf[tt % 2][:]), [ptf[tt % 2]], [x2Tf])
                    yield
                    pl = ptf[tt % 2]
                    for k in range(8):
                        c.mm(pl[:, 0, 0:NEXP], x2Tf[:, k, :], router_f[:, k, :], k == 0, k == 7, [x2Tf, router_f], [pl])
                    yield
                    lg = lg_p.next(); mx = mx_p.next(); nb = nb_p.next(); ee = ee_p.next(); mk = mk_p.next(); dn = dn_p.next()
                    c.op("act", lambda: nc.scalar.copy(lg[:], pl[:, 0, 0:NEXP]), [pl], [lg])
                    yield
                    c.op("dve", lambda: nc.vector.max(mx[:], lg[:]), [lg], [mx])
                    yield
                    c.op("dve", lambda: nc.vector.tensor_scalar(nb[:], mx[:, 0:1], -1.0, None, ALU.mult), [mx], [nb])
                    c.op("dve", lambda: nc.vector.tensor_scalar(mk[:], lg[:], mx[:, 1:2], None, ALU.is_ge), [lg, mx], [mk])
                    yield
                    c.act(ee[:], lg[:], AF.Exp, [lg, nb], [ee], bias=nb[:, 0:1], scale=1.0)
                    yield
                    c.op("dve", lambda: nc.vector.tensor_tensor(mk[:], mk[:], ee[:], ALU.mult), [mk, ee], [mk])
                    yield
                    c.op("dve", lambda: nc.vector.reduce_sum(dn[:], mk[:], axis=AX.X), [mk], [dn])
                    yield
                    c.op("dve", lambda: nc.vector.reciprocal(dn[:], dn[:]), [dn], [dn])
                    yield
                    c.op("dve", lambda: nc.vector.tensor_scalar(gates[:, ti, :], mk[:], dn[:, 0:1], None, ALU.mult), [mk, dn], [gates])
                yield

            for pair in range(2):
                run_interleaved([phase3_tile(2 * pair), phase3_tile(2 * pair + 1)])
            c.dma("sp", x2Td.rearrange("(k p) t -> p k t", p=128)[:, :, blk * 512:(blk + 1) * 512], x2T[:], reads=[x2T], writes=[x2Td_T])
        barrier(c)
        s.es.close()

    TB2 = min(TB2, NT)
    NB2 = NT // TB2
    NT2 = TB2 // 128
    NH2 = TB2 // 512
    s = Ctx.__new__(Ctx); s.__dict__.update(c.__dict__); s.es = ExitStack()
    load_ln(s, 2)
    x2Tb = s.sb("x2Tb", [128, 8, TB2], BF16)
    actT_raw = s.es.enter_context(nc.sbuf_tensor("actT", [128, NFF, TB2], BF16))
    actT = [T(actT_raw[:, f, :], "actT%d" % f) for f in range(NFF)]
    acc_raw = s.es.enter_context(nc.sbuf_tensor("acc", [128, NT2, D], F32))
    acc = [T(acc_raw[:, t, :], "acc%d" % t) for t in range(NT2)]
    w1g_p = Pool(s, "w1g", [128, 8, 256], BF16, 2); w3g_p = Pool(s, "w3g", [128, 8, 256], BF16, 2)
    w2q_p = Pool(s, "w2q", [128, NFF, 256], BF16, 2)
    ph1 = Pool(s, "ph1", [128, 512], F32, 2, ps=True); ph3 = Pool(s, "ph3", [128, 512], F32, 2, ps=True)
    pout = Pool(s, "pout", [128, 512], F32, 2, ps=True)
    ptp = Pool(s, "ptp2", [128, 8, 128], BF16, 2, ps=True)
    sil_p = Pool(s, "sil", [128, 512], F32, 2)
    x2l_p = Pool(s, "x2l", [128, D], F32, 2)
    h_p = Pool(s, "h2", [128, D], F32, 2)
    x3_p = Pool(s, "x3", [128, D], F32, 2)
    x3b_p = Pool(s, "x3b", [128, D], BF16, 2)
    x3T = s.sb("x3T", [128, 8, 512], BF16)
    for b2 in range(NB2):
        t0 = b2 * TB2
        c.dma("sp", x2Tb[:], x2Td.rearrange("(k p) t -> p k t", p=128)[:, :, t0:t0 + TB2], reads=[x2Td_T], writes=[x2Tb])
        for e in range(nexp):
            w1v = w1[e].rearrange("(k p) f -> p k f", p=128)
            w3v = w3[e].rearrange("(k p) f -> p k f", p=128)
            w2v = w2[e].rearrange("(f p) n -> p f n", p=128)
            for ffg in range(NFF // 2):
                w1g = w1g_p.next(); w3g = w3g_p.next()
                c.dma("pool", w1g[:], w1v[:, :, ffg * 256:(ffg + 1) * 256], writes=[w1g])
                c.dma("pool", w3g[:], w3v[:, :, ffg * 256:(ffg + 1) * 256], writes=[w3g])
                for fc in range(2):
                    f = ffg * 2 + fc
                    for tb in range(NH2):
                        p1 = ph1.next(); p3 = ph3.next()
                        for k in range(8):
                            c.mm(p1[:], w1g[:, k, fc * 128:(fc + 1) * 128], x2Tb[:, k, tb * 512:(tb + 1) * 512], k == 0, k == 7, [w1g, x2Tb], [p1])
                        for k in range(8):
                            c.mm(p3[:], w3g[:, k, fc * 128:(fc + 1) * 128], x2Tb[:, k, tb * 512:(tb + 1) * 512], k == 0, k == 7, [w3g, x2Tb], [p3])
                        sl = sil_p.next()
                        c.act(sl[:], p1[:], AF.Silu, [p1], [sl])
                        c.op("dve", lambda f=f, tb=tb, sl=sl, p3=p3: nc.vector.tensor_tensor(actT[f][:, tb * 512:(tb + 1) * 512], sl[:], p3[:], ALU.mult),
                             [sl, p3], [actT[f]])
            for qr in range(4):
                w2q = w2q_p.next()
                for f0 in range(0, NFF, 7):
                    c.dma("pool", w2q[:, f0:f0 + 7, :], w2v[:, f0:f0 + 7, qr * 256:(qr + 1) * 256], writes=[w2q])
                for t in range(NT2):
                    po = pout.next()
                    for f in range(NFF):
                        c.mm(po[:, 0:256], actT[f][:, t * 128:(t + 1) * 128], w2q[:, f, :], f == 0, f == NFF - 1, [actT[f], w2q], [po])
                    dst = acc[t][:, qr * 256:(qr + 1) * 256]
                    if not moe:
                        c.op("act", lambda dst=dst, po=po: nc.scalar.copy(dst, po[:, 0:256]), [po], [acc[t]])
                    else:
                        gt = gates[:, b2 * NT2 + t, e:e + 1]
                        if e == 0:
                            c.op("dve", lambda dst=dst, po=po, gt=gt: nc.vector.tensor_scalar(dst, po[:, 0:256], gt, None, ALU.mult), [po, gates], [acc[t]])
                        else:
                            c.op("dve", lambda dst=dst, po=po, gt=gt: nc.vector.scalar_tensor_tensor(dst, po[:, 0:256], gt, dst, ALU.mult, ALU.add), [po, gates, acc[t]], [acc[t]])
        def tail_tile(t):
            r0 = t0 + t * 128
            x2l = x2l_p.next()
            c.dma("sp", x2l[:], x2d[r0:r0 + 128, :], reads=[x2d_T], writes=[x2l])
            h = h_p.next()
            yield
            c.op("dve", lambda: nc.vector.scalar_tensor_tensor(h[:], x2l[:], ALPHA, acc[t][:], ALU.mult, ALU.add), [x2l, acc[t]], [h])
            yield
            x3 = x3_p.next()
            yield from layer_norm_gen(c, P, h, gbc[2], bbc[2], x3)
            c.dma("sp", xo[r0:r0 + 128, :], x3[:], reads=[x3])
            x3b = x3b_p.next()
            c.op("act", lambda: nc.scalar.copy(x3b[:], x3[:]), [x3], [x3b])
            yield
            tp = ptp.next()
            for k in range(8):
                c.tr(tp[:, k, :], x3b[:, k * 128:(k + 1) * 128], ident_bf[:], [x3b, ident_bf], [tp])
            yield
            tq = t % 4
            c.op("act", lambda: nc.scalar.copy(x3T[:, :, tq * 128:(tq + 1) * 128], tp[:]), [tp], [x3T])
            if tq == 3:
                cb = t0 + (t // 4) * 512
                c.dma("sp", xoT.rearrange("(k p) t -> p k t", p=128)[:, :, cb:cb + 512], x3T[:], reads=[x3T])
            yield

        for pair in range(NT2 // 2):
            run_interleaved([tail_tile(2 * pair), tail_tile(2 * pair + 1)])
    barrier(c)
    s.es.close()
    c.close()
    print("B: instructions", c.ninst, "waits", c.nwait)
    return nc

DO_SSD = True
DO_FOX = True
DO_POOL = True
STOP = 99
class StopBuild(Exception):
    pass
def ckpt(k):
    if k >= STOP:
        raise StopBuild()
TM = 9
DO_QF = True
DO_K = True
DO_PW = True

D = 1024
NFM = 704
NTM = 196
NEG = -30000.0


def consts_A():
    i = np.arange(128)
    same = (i[:, None] // 64) == (i[None, :] // 64)
    tri = ((i[:, None] <= i[None, :]) & same).astype(np.float32)
    blk = same.astype(np.float32)
    umask = ((i[:, None] > i[None, :]) & same).astype(np.float32)
    neg = np.where((i[:, None] <= i[None, :]) & same, 0.0, NEG).astype(np.float32)
    cm = np.stack([(i < 64), (i >= 64)], 1).astype(np.float32)
    cmask = np.concatenate([tri, blk, umask, neg, cm, np.ones((128, 128), np.float32)], 1)
    q = np.arange(512)
    dm = np.stack([np.where((jj * 128 + i[:, None]) <= q[None, :], 0.0, NEG) for jj in range(4)], 1).astype(np.float32)
    sel = np.zeros((6, 8), np.float32)
    sel[0, 0] = 1; sel[1, 1] = 1; sel[2, 2] = 1; sel[3:6, 3] = 1
    sel[3, 4] = -1; sel[4, 5] = -1; sel[5, 6] = -1; sel[0:3, 7] = 1
    return {"cmask": cmask, "dmask": dm, "sel": sel}


def build_A(TT):
    nc = bass.Bass("TRN2", target_bir_lowering=False)
    din = lambda name, shape, dt=F32: nc.dram_tensor(name, list(shape), dt, kind="ExternalInput").ap()
    xT = din("xT", [D, TT], BF16)
    w_fm = din("w_fm", [D, NFM]); w_tm = din("w_tm", [D, NTM])
    conv_w = din("conv_w", [128, 3, 4]); conv_b = din("conv_b", [128, 3])
    pp = din("pp", [8])
    pool_wb = din("pool_wb", [65, 64]); pool_scale = din("pool_scale", [64])
    pool_coef = din("pool_coef", [64, 4]); pool_fix = din("pool_fix", [64, 16])
    cmask_d = din("cmask", [128, 4 * 128 + 2 + 128]); dmask_d = din("dmask", [128, 4, 512]); sel_d = din("sel", [6, 8])
    y = nc.dram_tensor("y", [TT, 256], F32, kind="ExternalOutput").ap()

    c = Ctx(nc)
    NBLK = TT // 512
    NTILE = TT // 128

    ident_bf = make_ident(c, "ident_bf", BF16)
    ident_f = make_ident(c, "ident_f", F32)
    cm = c.sb("cm", [128, 4 * 128 + 2 + 128])
    c.dma("sp", cm[:], cmask_d, writes=[cm])
    TRI = cm[:, 0:128]; BLK = cm[:, 128:256]; UMASK = cm[:, 256:384]; NEGM = cm[:, 384:512]
    CMK = cm[:, 512:514]; ONES = cm[:, 514:642]
    dmask = c.sb("dmask_sb", [128, 4, 512], BF16)
    c.dma("pool", dmask[:], dmask_d, writes=[dmask])
    sel = c.sb("sel_sb", [6, 8])
    c.dma("sp", sel[:], sel_d, writes=[sel])
    wfm = c.sb("wfm", [128, 8, NFM], BF16); wtm = c.sb("wtm", [128, 8, NTM], BF16)
    c.dma("pool", wfm[:], w_fm.rearrange("(k p) n -> p k n", p=128), writes=[wfm])
    c.dma("pool", wtm[:], w_tm.rearrange("(k p) n -> p k n", p=128), writes=[wtm])
    cw = c.sb("cw", [128, 3, 4]); cb = c.sb("cb", [128, 3])
    c.dma("sp", cw[:], conv_w, writes=[cw]); c.dma("sp", cb[:], conv_b, writes=[cb])
    ppb = c.sb("ppb", [128, 8])
    c.dma("sp", ppb[:], pp.partition_broadcast(128), writes=[ppb])
    abc = c.sb("abc", [128, 2])
    c.act(abc[:], ppb[:, 2:4], AF.Exp, [ppb], [abc])
    c.op("dve", lambda: nc.vector.tensor_scalar(abc[:], abc[:], -1.0, None, ALU.mult), [abc], [abc])
    nfb = c.sb("nfb", [128, 1])
    c.op("dve", lambda: nc.vector.tensor_scalar(nfb[:], ppb[:, 6:7], -1.0, None, ALU.mult), [ppb], [nfb])
    dtbias4 = c.sb("dtbias4", [128, 4, 2])
    for tt in range(4):
        c.op("dve", lambda tt=tt: nc.vector.tensor_copy(dtbias4[:, tt, :], ppb[:, 0:2]), [ppb], [dtbias4])
    abc4 = c.sb("abc4", [128, 4, 2])
    for tt in range(4):
        c.op("dve", lambda tt=tt: nc.vector.tensor_copy(abc4[:, tt, :], abc[:]), [abc], [abc4])
    DI = [c.sb("DI%d" % h, [128, 128], BF16) for h in range(2)]
    for h in range(2):
        c.op("dve", lambda h=h: nc.vector.tensor_scalar(DI[h][:], ident_bf[:], ppb[:, 4 + h:5 + h], None, ALU.mult), [ident_bf, ppb], [DI[h]])
    one_t = c.sb("one_t", [128, 1])
    c.op("pool", lambda: nc.gpsimd.memset(one_t[:], 1.0), [], [one_t])
    pwb = c.sb("pwb", [65, 64]); psc = c.sb("psc", [65, 64]); pw_bf = c.sb("pw_bf", [65, 64], BF16)
    c.dma("sp", pwb[:], pool_wb, writes=[pwb])
    c.dma("sp", psc[:], pool_scale.partition_broadcast(65), writes=[psc])
    c.op("dve", lambda: nc.vector.tensor_tensor(pw_bf[:], pwb[:], psc[:], ALU.mult), [pwb, psc], [pw_bf])
    pcoef = c.sb("pcoef", [64, 4]); pfix = c.sb("pfix", [64, 16])
    c.dma("sp", pcoef[:], pool_coef, writes=[pcoef]); c.dma("sp", pfix[:], pool_fix, writes=[pfix])

    try:
      ckpt(1)
    except StopBuild:
      barrier(c); c.close(); return nc
    KT = c.sb("KT", [128, TT], BF16)
    VA = c.sb("VA", [128, NTILE, 66], BF16)
    c.op("pool", lambda: nc.gpsimd.memset(KT[:], 0.0), [], [KT])
    c.op("pool", lambda: nc.gpsimd.memset(VA[:], 1.0), [], [VA])
    state = c.sb("state", [128, 128])
    state_bf = [c.sb("state_bf%d" % i, [128, 128], BF16) for i in range(2)]
    c.op("dve", lambda: nc.vector.memset(state[:], 0.0), [], [state])
    c.op("dve", lambda: nc.vector.memset(state_bf[0][:], 0.0), [], [state_bf[0]])
    ccar = c.sb("ccar", [6, 1])
    c.op("dve", lambda: nc.vector.memset(ccar[:], 0.0), [], [ccar])
    U = [c.sb("U%d" % g, [128, 3 + 512]) for g in range(3)]
    for g in range(3):
        c.op("pool", lambda g=g: nc.gpsimd.memset(U[g][:], 0.0), [], [U[g]])
    PU = c.sb("PU", [64, 16 + 512])
    c.op("pool", lambda: nc.gpsimd.memset(PU[:], 0.0), [], [PU])

    try:
      ckpt(2)
    except StopBuild:
      barrier(c); c.close(); return nc
    xT_p = Pool(c, "xTb", [128, 8, 512], BF16, 2)
    pst = Pool(c, "pst", [128, 512], F32, 2, ps=True)
    ppro = c.ps("ppro", [128, 512], F32)

    class _One:
        def next(self):
            return ppro
    pfm = _One()
    pacc = c.ps("pacc", [128, 512], F32)
    tA = [c.ps("tA%d" % u, [128, 512], F32) for u in range(2)]
    tB = [c.ps("tB%d" % u, [128, 512], F32) for u in range(2)]
    cacc3 = [c.sb("cacc%d" % g, [128, 512]) for g in range(3)]
    zsb_p = Pool(c, "zsb", [128, 4, 128], F32, 2)
    fmT = [Pool(c, "fmT%d" % g, [128, 512], BF16, 2) for g in range(3)]
    QT_p = Pool(c, "QT", [128, 512], BF16, 2)
    for qq in QT_p.ts:
        c.op("pool", lambda qq=qq: nc.gpsimd.memset(qq[:], 0.0), [], [qq])
    f6_p = Pool(c, "f6", [6, 512], F32, 2); lf_p = Pool(c, "lf", [6, 512], F32, 2); cc_p = Pool(c, "cc", [6, 512], F32, 2)
    ones6 = c.sb("ones6", [6, 512])
    c.op("pool", lambda: nc.gpsimd.memset(ones6[:], 1.0), [], [ones6])
    hi_p = Pool(c, "hi", [6, 512], BF16, 2); mid_p = Pool(c, "mid", [6, 512], BF16, 2); lo_p = Pool(c, "lo", [6, 512], BF16, 2)
    r1_p = Pool(c, "r1", [6, 512], F32, 2); r2_p = Pool(c, "r2", [6, 512], F32, 2)
    aq_p = Pool(c, "aq", [6, 512], F32, 2); ak_p = Pool(c, "ak", [6, 512], F32, 2)
    ztmb_p = Pool(c, "ztmb", [128, 4, 128], F32, 2); dtrb_p = Pool(c, "dtrb", [128, 4, 2], F32, 2); dtb_p = Pool(c, "dtb", [128, 4, 2], F32, 2)
    smb_p = Pool(c, "smb", [128, 32], F32, 2); exb_p = Pool(c, "exb", [128, 32], F32, 2)
    Ab_p = Pool(c, "Ab", [128, 4, 2], F32, 2); A4b_p = Pool(c, "A4b", [128, 4, 4], F32, 2)
    dtdb_p = Pool(c, "dtdb", [128, 4, 2], F32, 2)
    xstm_p = Pool(c, "xstm", [128, 128], BF16, 2); btm_p = Pool(c, "btm", [128, 128], BF16, 2)
    X_p = Pool(c, "X", [128, 128], BF16, 2); Xd_p = Pool(c, "Xd", [128, 128], BF16, 2)
    UA_p = Pool(c, "UA", [128, 128], F32, 4); L_p = Pool(c, "L", [128, 128], F32, 4); MT_p = Pool(c, "MT", [128, 128], BF16, 4)
    pysb_p = Pool(c, "pysb", [128, 128], F32, 2); t1_p = Pool(c, "t1", [128, 128], F32, 2)
    zs_p = Pool(c, "zs", [128, 128], F32, 2)
    yo_p = Pool(c, "yo", [128, 256], F32, 8)
    PT_p = Pool(c, "PT", [128, 512], BF16, 3)
    osb_p = Pool(c, "osb", [65, 512], F32, 2); pfs_p = Pool(c, "pfs", [128, 260], F32, 2); rd_p = Pool(c, "rd", [128, 4], F32, 2)
    ps2 = [c.sb("ps%d" % i, [64, 16 + 512]) for i in range(4)]
    pmean = c.sb("pmean", [64, 512]); ptmp = c.sb("ptmp", [64, 512]); paug_p = Pool(c, "paug", [65, 512], BF16, 2)
    for pa in paug_p.ts:
        c.op("pool", lambda pa=pa: nc.gpsimd.memset(pa[:], 1.0), [], [pa])

    xTv = xT.rearrange("(k p) t -> p k t", p=128)
    sbf_i = 0

    BC = {}

    def prologue(blk):
        nonlocal xb_next
        t0 = blk * 512
        if blk == 0:
            xb_next = xT_p.next()
            c.dma("sp", xb_next[:], xTv[:, :, 0:512], writes=[xb_next])
        xb = xb_next
        if blk + 1 < NBLK:
            xb_next = xT_p.next()
            c.dma("sp", xb_next[:], xTv[:, :, t0 + 512:t0 + 1024], writes=[xb_next])
        for g in range(3):
            c.op("pool", lambda g=g: nc.gpsimd.tensor_copy(U[g][:, 0:3], U[g][:, 512:515]), [U[g]], [U[g]])
            pg = pfm.next()
            for k in range(8):
                c.mm(pg[:], wfm[:, k, g * 128:(g + 1) * 128], xb[:, k, :], k == 0, k == 7, [wfm, xb], [pg])
            c.op("act", lambda g=g, pg=pg: nc.scalar.copy(U[g][:, 3:515], pg[:]), [pg], [U[g]])
            ca = cacc3[g]
            c.act(ca[:], U[g][:, 0:512], AF.Identity, [U[g], cw, cb], [ca], bias=cb[:, g:g + 1], scale=cw[:, g, 0:1])
            for kk in range(1, 4):
                c.op("dve", lambda g=g, kk=kk: nc.vector.scalar_tensor_tensor(ca[:], U[g][:, kk:kk + 512], cw[:, g, kk:kk + 1], ca[:], ALU.mult, ALU.add),
                     [U[g], cw, ca], [ca])
            yield
        if DO_QF:
            yield
            QT = QT_p.next()
            pg = pfm.next()
            for k in range(8):
                c.mm(pg[:], wfm[:, k, 384:512], xb[:, k, :], k == 0, k == 7, [wfm, xb], [pg])
            c.op("act", lambda: nc.scalar.copy(QT[64:128, :], pg[64:128, :]), [pg], [QT])
            yield
            f6 = f6_p.next(); lf = lf_p.next(); cc = cc_p.next()
            c.act(f6[:], pg[0:6, :], AF.Exp, [pg, nfb], [f6], bias=nfb[0:6, 0:1], scale=-1.0)
            c.act(lf[:], f6[:], AF.Ln, [f6, one_t], [lf], bias=one_t[0:6, 0:1], scale=1.0)
            c.op("dve", lambda: nc.vector.tensor_tensor_scan(cc[:], ones6[:], lf[:], ccar[:, 0:1], ALU.mult, ALU.subtract), [ones6, lf, ccar], [cc])
            c.op("dve", lambda: nc.vector.tensor_copy(ccar[:], cc[:, 511:512]), [cc], [ccar])
            yield
            hi = hi_p.next(); mid = mid_p.next(); lo = lo_p.next(); r1 = r1_p.next(); r2 = r2_p.next()
            c.op("act", lambda: nc.scalar.copy(hi[:], cc[:]), [cc], [hi])
            c.op("dve", lambda: nc.vector.tensor_tensor(r1[:], cc[:], hi[:], ALU.subtract), [cc, hi], [r1])
            c.op("act", lambda: nc.scalar.copy(mid[:], r1[:]), [r1], [mid])
            c.op("dve", lambda: nc.vector.tensor_tensor(r2[:], r1[:], mid[:], ALU.subtract), [r1, mid], [r2])
            c.op("act", lambda: nc.scalar.copy(lo[:], r2[:]), [r2], [lo])
            yield
            aq = aq_p.next(); ak = ak_p.next()
            for (dst, co, final) in ((aq, 0, QT[0:6, :]), (ak, 4, KT[0:6, t0:t0 + 512])):
                c.op("dve", lambda dst=dst, co=co: nc.vector.tensor_scalar(dst[:], hi[:], sel[:, co:co + 1], sel[:, (3 if co == 0 else 7):(4 if co == 0 else 8)], ALU.mult, ALU.add), [hi, sel], [dst])
                c.op("dve", lambda dst=dst, co=co: nc.vector.scalar_tensor_tensor(dst[:], mid[:], sel[:, co + 1:co + 2], dst[:], ALU.mult, ALU.add), [mid, sel, dst], [dst])
                tgt = QT if co == 0 else KT
                c.op("dve", lambda dst=dst, co=co, final=final: nc.vector.scalar_tensor_tensor(final, lo[:], sel[:, co + 2:co + 3], dst[:], ALU.mult, ALU.add), [lo, sel, dst], [tgt])
        if DO_K:
            yield
            pg = pfm.next()
            for k in range(8):
                c.mm(pg[:], wfm[:, k, 512:640], xb[:, k, :], k == 0, k == 7, [wfm, xb], [pg])
            c.act(KT[64:128, t0:t0 + 512], pg[64:128, :], AF.Identity, [pg], [KT], scale=0.125)
        if DO_PW:
            yield
            c.op("pool", lambda: nc.gpsimd.tensor_copy(PU[:, 0:16], PU[:, 512:528]), [PU], [PU])
            pg = pfm.next()
            for k in range(8):
                c.mm(pg[0:64, :], wfm[:, k, 640:704], xb[:, k, :], k == 0, k == 7, [wfm, xb], [pg])
            c.op("act", lambda: nc.scalar.copy(PU[:, 16:528], pg[0:64, :]), [pg], [PU])
            yield
            srcs = [PU] + ps2
            for lv in range(4):
                sh = 1 << lv
                lo_i = 2 * sh - 1
                src = srcs[lv]; dstt = ps2[lv]
                c.op("pool", lambda src=src, dstt=dstt, sh=sh, lo_i=lo_i: nc.gpsimd.tensor_tensor(dstt[:, lo_i:528], src[:, lo_i:528], src[:, lo_i - sh:528 - sh], ALU.add), [src], [dstt])
            yield
            c.op("dve", lambda: nc.vector.tensor_scalar(pmean[:], ps2[0][:, 16:528], pcoef[:, 0:1], None, ALU.mult), [ps2[0], pcoef], [pmean])
            for lv in range(1, 4):
                c.op("dve", lambda lv=lv: nc.vector.scalar_tensor_tensor(pmean[:], ps2[lv][:, 16:528], pcoef[:, lv:lv + 1], pmean[:], ALU.mult, ALU.add), [ps2[lv], pcoef, pmean], [pmean])
            yield
            if blk == 0:
                c.op("pool", lambda: nc.gpsimd.tensor_tensor(pmean[:, 0:16], pmean[:, 0:16], pfix[:], ALU.mult), [pmean, pfix], [pmean])
            paug = paug_p.next()
            c.op("pool", lambda: nc.gpsimd.tensor_tensor(paug[0:64, :], pmean[:], PU[:, 16:528], ALU.subtract), [pmean, PU], [paug])

        ztmb = ztmb_p.next(); dtrb = dtrb_p.next(); dtb = dtb_p.next()
        for tt in range(4):
            ti = blk * 4 + tt
            cs = slice(tt * 128, (tt + 1) * 128)
            ptm = pfm.next()
            for k in range(8):
                c.mm(ptm[:, 0:NTM], xb[:, k, cs], wtm[:, k, :], k == 0, k == 7, [xb, wtm], [ptm])
            c.op("act", lambda: nc.scalar.copy(ztmb[:, tt, :], ptm[:, 0:128]), [ptm], [ztmb])
            if TM >= 2: c.op("act", lambda: nc.scalar.copy(VA[:, ti, 0:64], ptm[:, 128:192]), [ptm], [VA])
            if TM >= 3: c.op("act", lambda: nc.scalar.copy(dtrb[:, tt, :], ptm[:, 192:194]), [ptm], [dtrb])
            yield
        yield
        fts = []
        for g in range(3):
            ft = fmT[g].next()
            c.act(ft[:], cacc3[g][:], AF.Silu, [cacc3[g]], [ft])
            fts.append(ft)
        xsT, BT, CT = fts
        zsb = zsb_p.next()
        c.act(zsb[:], ztmb[:], AF.Silu, [ztmb], [zsb])
        yield
        if TM >= 3: c.op("dve", lambda: nc.vector.tensor_tensor(dtrb[:], dtrb[:], dtbias4[:], ALU.add), [dtrb, dtbias4], [dtrb])
        if TM >= 4: c.act(dtrb[:], dtrb[:], AF.Exp, [dtrb], [dtrb])
        if TM >= 5: c.act(dtb[:], dtrb[:], AF.Ln, [dtrb, one_t], [dtb], bias=one_t[:, 0:1], scale=1.0)
        Ab = Ab_p.next(); A4b = A4b_p.next()
        c.op("dve", lambda: nc.vector.tensor_tensor(Ab[:], dtb[:], abc4[:], ALU.mult), [dtb, abc4], [Ab])
        for ck in range(2):
            c.op("dve", lambda ck=ck: nc.vector.tensor_scalar(A4b[:, :, ck * 2:ck * 2 + 2], Ab[:], CMK[:, ck:ck + 1], None, ALU.mult), [Ab, cm], [A4b])
        yield
        smb = smb_p.next(); exb = exb_p.next(); dtdb = dtdb_p.next()
        c.mm(ppro[:, 0:8], TRI, Ab[:].rearrange("p a b -> p (a b)"), True, True, [cm, Ab], [ppro])
        c.mm(ppro[:, 8:16], BLK, Ab[:].rearrange("p a b -> p (a b)"), True, True, [cm, Ab], [ppro])
        c.mm(ppro[:, 16:32], ONES, A4b[:].rearrange("p a b -> p (a b)"), True, True, [cm, A4b], [ppro])
        c.op("act", lambda: nc.scalar.copy(smb[:], ppro[:, 0:32]), [ppro], [smb])
        yield
        c.op("dve", lambda: nc.vector.tensor_tensor(smb[:, 8:16], smb[:, 8:16], smb[:, 0:8], ALU.subtract), [smb], [smb])
        yield
        c.act(exb[:], smb[:], AF.Exp, [smb], [exb])
        yield
        c.op("dve", lambda: nc.vector.tensor_tensor(dtdb[:].rearrange("p a b -> p (a b)"), dtb[:].rearrange("p a b -> p (a b)"), exb[:, 8:16], ALU.mult), [dtb, exb], [dtdb])
        BC[blk] = (xsT, BT, CT, QT, paug, ztmb, dtb, Ab, A4b, exb, dtdb, zsb)
        yield

    def tiles_fox(blk):
        t0 = blk * 512
        xsT, BT, CT, QT, paug, ztmb, dtb, Ab, A4b, exb, dtdb, zsb = BC.pop(blk)
        nkt = 4 * blk + 4
        def fox_steps():
            prev = None
            for j in range(nkt + 1):
                cur = None
                if j < nkt:
                    ps_ = pst.next()
                    diag = j >= 4 * blk
                    c.mm(ps_[:], KT[:, j * 128:(j + 1) * 128], QT[:], True, not diag, [KT, QT], [ps_])
                    if diag:
                        c.mm(ps_[:], ident_bf[:], dmask[:, j - 4 * blk, :], False, True, [ident_bf, dmask], [ps_])
                    cur = (j, ps_)
                if prev is not None:
                    pj, pps = prev
                    PT = PT_p.next()
                    c.act(PT[:], pps[:], AF.Exp, [pps], [PT])
                    c.mm(pacc[0:65, :], VA[:, pj, 0:65], PT[:], pj == 0, pj == nkt - 1, [VA, PT], [pacc])
                prev = cur
                yield
        fsteps = fox_steps()
        per_tile = (nkt + 1 + 3) // 4

        yos = []

        def ssd_tile(tt):
            nonlocal sbf_i
            u = tt % 2
            bA = tA[u]; bB = tB[u]
            cs = slice(tt * 128, (tt + 1) * 128)
            ztm = ztmb[:, tt, :]; dt_ = dtb[:, tt, :]
            yo = yo_p.next()
            yos.append(yo)
            A_ = Ab[:, tt, :]
            dtd = dtdb[:, tt, :]
            UAs = []
            for h in range(2):
                UA = UA_p.next()
                c.act(UA[:], UMASK, AF.Identity, [cm, Ab], [UA], scale=A_[:, h:h + 1])
                UAs.append(UA)
            ptb = bA[:].bitcast(BF16)
            c.tr(ptb[:, 0:128], xsT[:, cs], ident_bf[:], [xsT, ident_bf], [bA])
            c.tr(ptb[:, 128:256], BT[:, cs], ident_bf[:], [BT, ident_bf], [bA])
            c.mm(bB[:, 0:128], BT[:, cs], CT[:, cs], True, True, [BT, CT], [bB])
            yield
            xstm = xstm_p.next(); btm = btm_p.next()
            c.op("act", lambda: nc.scalar.copy(xstm[:], ptb[:, 0:128]), [bA], [xstm])
            c.op("act", lambda: nc.scalar.copy(btm[:], ptb[:, 128:256]), [bA], [btm])
            X = X_p.next(); Xd = Xd_p.next()
            for h in range(2):
                hs = slice(h * 64, (h + 1) * 64)
                c.act(X[:, hs], ptb[:, hs], AF.Identity, [bA, dtb], [X], scale=dt_[:, h:h + 1])
            for h in range(2):
                hs = slice(h * 64, (h + 1) * 64)
                c.act(Xd[:, hs], ptb[:, hs], AF.Identity, [bA, dtdb], [Xd], scale=dtd[:, h:h + 1])
            for h in range(2):
                c.mm(bB[:, 128 * (h + 1):128 * (h + 2)], UAs[h][:], TRI, True, False, [UAs[h], cm], [bB])
                c.mm(bB[:, 128 * (h + 1):128 * (h + 2)], ident_f[:], NEGM, False, True, [ident_f, cm], [bB])
            yield
            Ls = []
            for h in range(2):
                L = L_p.next()
                c.act(L[:], bB[:, 128 * (h + 1):128 * (h + 2)], AF.Exp, [bB], [L])
                Ls.append(L)
            yield
            MTs = []
            for h in range(2):
                MT = MT_p.next()
                c.op("dve", lambda h=h, MT=MT: nc.vector.tensor_tensor(MT[:], Ls[h][:], bB[:, 0:128], ALU.mult), [Ls[h], bB], [MT])
                MTs.append(MT)
            yield
            for h in range(2):
                hs = slice(h * 64, (h + 1) * 64)
                c.mm(bA[:, hs], MTs[h][:], X[:, hs], True, False, [MTs[h], X], [bA])
                c.mm(bA[:, hs], DI[h][:], xstm[:, hs], False, True, [DI[h], xstm], [bA])
            yield
            for ck in range(2):
                rs_ = slice(ck * 64, (ck + 1) * 64)
                lcs = slice(tt * 128 + ck * 64, tt * 128 + (ck + 1) * 64)
                sb_cur = state_bf[sbf_i]
                c.mm(bA[:, 256 + ck * 128:256 + (ck + 1) * 128], btm[rs_, :], Xd[rs_, :], True, True, [btm, Xd], [bA])
                c.mm(bA[rs_, 128:256], CT[:, lcs], sb_cur[:], True, True, [CT, sb_cur], [bA])
                for h in range(2):
                    hs = slice(h * 64, (h + 1) * 64)
                    c.op("dve", lambda h=h, hs=hs, ck=ck: nc.vector.scalar_tensor_tensor(state[:, hs], state[:, hs], exb[:, 16 + tt * 4 + ck * 2 + h:17 + tt * 4 + ck * 2 + h],
                                                                                         bA[:, 256 + ck * 128 + h * 64:256 + ck * 128 + (h + 1) * 64], ALU.mult, ALU.add),
                         [state, exb, bA], [state])
                sbf_i = 1 - sbf_i
                nb_ = state_bf[sbf_i]
                c.op("act", lambda nb_=nb_: nc.scalar.copy(nb_[:], state[:]), [state], [nb_])
            yield
            pysb = pysb_p.next(); t1 = t1_p.next()
            c.op("act", lambda: nc.scalar.copy(pysb[:], bA[:, 0:128]), [bA], [pysb])
            c.mm(bB[:, 392:456], paug[0:65, cs], pw_bf[:], True, True, [paug, pw_bf], [bB])
            yield
            for h in range(2):
                hs = slice(h * 64, (h + 1) * 64)
                c.op("dve", lambda h=h, hs=hs: nc.vector.scalar_tensor_tensor(t1[:, hs], bA[:, 128 + h * 64:128 + (h + 1) * 64], exb[:, tt * 2 + h:tt * 2 + h + 1], pysb[:, hs], ALU.mult, ALU.add),
                     [bA, exb, pysb], [t1])
            c.op("act", lambda: nc.scalar.copy(yo[:, 192:256], bB[:, 392:456]), [bB], [yo])
            yield
            c.op("pool", lambda: nc.gpsimd.tensor_tensor(yo[:, 0:128], t1[:], zsb[:, tt, :], ALU.mult), [t1, zsb], [yo])
            yield

        NROUND = 10
        fox_per_round = (nkt + 1 + 2 * NROUND - 1) // (2 * NROUND)
        for pair in range(2):
            gens = [ssd_tile(2 * pair), ssd_tile(2 * pair + 1)]
            while gens:
                for g in list(gens):
                    try:
                        next(g)
                    except StopIteration:
                        gens.remove(g)
                for _ in range(fox_per_round):
                    next(fsteps, None)
                yield
        for _ in fsteps:
            yield
        if DO_FOX:
            osb = osb_p.next(); rd = rd_p.next()
            c.op("act", lambda: nc.scalar.copy(osb[:], pacc[0:65, :]), [pacc], [osb])
            pf = tB[0]
            for tt in range(4):
                c.tr(pf[:, tt * 65:(tt + 1) * 65], osb[:, tt * 128:(tt + 1) * 128], ident_f[0:65, 0:65], [osb, ident_f], [pf])
            pfs = pfs_p.next()
            c.op("act", lambda: nc.scalar.copy(pfs[:], pf[:, 0:260]), [pf], [pfs])
            for tt in range(4):
                c.op("dve", lambda tt=tt: nc.vector.reciprocal(rd[:, tt:tt + 1], pfs[:, tt * 65 + 64:tt * 65 + 65]), [pfs], [rd])
        for tt in range(4):
            yo = yos[tt]
            if DO_FOX: c.op("dve", lambda tt=tt, yo=yo: nc.vector.tensor_scalar(yo[:, 128:192], pfs[:, tt * 65:tt * 65 + 64], rd[:, tt:tt + 1], None, ALU.mult), [pfs, rd], [yo])
            r0 = t0 + tt * 128
            c.dma("sp", y[r0:r0 + 128, :], yo[:], reads=[yo])
        yield

    xb_next = None
    run_interleaved([prologue(0)])
    for blk in range(NBLK):
        gens = [tiles_fox(blk)]
        if blk + 1 < NBLK:
            gens.append(prologue(blk + 1))
        run_interleaved(gens)
    barrier(c)
    c.close()
    print("A: instructions", c.ninst, "waits", c.nwait)
    return nc


def prep_A(w_in, conv_w, conv_b, dt_bias, a_log, d_skip, f_bias, pool_w, pool_b, pool_scale, j):
    g = j // 2
    cz = slice(128 * j, 128 * j + 128)
    cxs = slice(512 + 128 * j, 512 + 128 * j + 128)
    cB = slice(1024 + 128 * g, 1024 + 128 * g + 128)
    cC = slice(1280 + 128 * g, 1280 + 128 * g + 128)
    cdt = slice(1536 + 2 * j, 1536 + 2 * j + 2)
    cq = slice(1544 + 64 * j, 1544 + 64 * j + 64)
    ck = slice(1800 + 64 * j, 1800 + 64 * j + 64)
    cv = slice(2056 + 64 * j, 2056 + 64 * j + 64)
    cf = 2312 + j
    cp = slice(2316 + 64 * j, 2316 + 64 * j + 64)
    w_fm = np.zeros((D, NFM), np.float32)
    w_fm[:, 0:128] = w_in[:, cxs]; w_fm[:, 128:256] = w_in[:, cB]; w_fm[:, 256:384] = w_in[:, cC]
    for r in range(6):
        w_fm[:, 384 + r] = w_in[:, cf]
    w_fm[:, 384 + 64:384 + 128] = w_in[:, cq]
    w_fm[:, 512 + 64:512 + 128] = w_in[:, ck]
    w_fm[:, 640:704] = w_in[:, cp]
    w_tm = np.concatenate([w_in[:, cz], w_in[:, cv], w_in[:, cdt], w_in[:, cf:cf + 1], np.zeros((D, 1), np.float32)], 1).astype(np.float32)
    chans = [np.arange(128 * j, 128 * j + 128), np.arange(512 + 128 * g, 512 + 128 * g + 128), np.arange(768 + 128 * g, 768 + 128 * g + 128)]
    cw = np.stack([conv_w[:, ch].T for ch in chans], 1).astype(np.float32)
    cbb = np.stack([conv_b[ch] for ch in chans], 1).astype(np.float32)
    pp = np.zeros(8, np.float32)
    pp[0:2] = dt_bias[2 * j:2 * j + 2]; pp[2:4] = a_log[2 * j:2 * j + 2]; pp[4:6] = d_skip[2 * j:2 * j + 2]; pp[6] = f_bias[j]
    wb = np.concatenate([pool_w[j], pool_b[j][None, :]], 0).astype(np.float32)
    win = (2, 4, 8, 16)[j]
    coef = np.zeros((64, 4), np.float32); coef[:, j] = 1.0 / win
    fix = np.ones((64, 16), np.float32)
    tpos = np.arange(16)
    fix[:, :] = (win / np.minimum(tpos + 1, win))[None, :]
    return {"w_fm": w_fm, "w_tm": w_tm, "conv_w": np.ascontiguousarray(cw), "conv_b": np.ascontiguousarray(cbb), "pp": pp,
            "pool_wb": wb, "pool_scale": np.ascontiguousarray(pool_scale[64 * j:64 * j + 64]).astype(np.float32),
            "pool_coef": coef, "pool_fix": fix}


def build_P(NT):
    nc = bass.Bass("TRN2", target_bir_lowering=False)
    x = nc.dram_tensor("x", [NT, D], F32, kind="ExternalInput").ap()
    xT = nc.dram_tensor("xT", [D, NT], BF16, kind="ExternalOutput").ap()
    c = Ctx(nc)
    ident_bf = make_ident(c, "ident_bf", BF16)
    xt_p = Pool(c, "xt", [128, D], F32, 3); xb_p = Pool(c, "xb", [128, D], BF16, 2)
    ptp = Pool(c, "ptp", [128, 8, 128], BF16, 2, ps=True)
    xTs_p = Pool(c, "xTs", [128, 8, 512], BF16, 2)
    for blk in range(NT // 512):
        xTs = xTs_p.next()
        for tt in range(4):
            r0 = blk * 512 + tt * 128
            xt = xt_p.next(); xb = xb_p.next()
            c.dma("sp", xt[:], x[r0:r0 + 128, :], writes=[xt])
            c.op("dve", lambda: nc.vector.tensor_copy(xb[:], xt[:]), [xt], [xb])
            tp = ptp.next()
            for k in range(8):
                c.tr(tp[:, k, :], xb[:, k * 128:(k + 1) * 128], ident_bf[:], [xb, ident_bf], [tp])
            c.op("act", lambda: nc.scalar.copy(xTs[:, :, tt * 128:(tt + 1) * 128], tp[:]), [tp], [xTs])
        c.dma("sp", xT.rearrange("(k p) t -> p k t", p=128)[:, :, blk * 512:(blk + 1) * 512], xTs[:], reads=[xTs])
    barrier(c)
    c.close()
    return nc


from concourse.bass_utils import run_bass_kernel_spmd

NCORES = 8
SEQ = 16384
BATCH = 2
NTOK = BATCH * SEQ // NCORES
DEPTH = 4
_CACHE = {}


def _prog(key, fn):
    if key not in _CACHE:
        _CACHE[key] = fn()
    return _CACHE[key]


def _run(nc, in_maps):
    res = run_bass_kernel_spmd(nc, in_maps, core_ids=list(range(NCORES)))
    return res.results


def kernel(**inp):
    f32 = lambda a: np.ascontiguousarray(np.asarray(a, dtype=np.float32))
    x = f32(inp["x"]); mem = f32(inp["mem"])
    xs = x.reshape(NCORES, NTOK, D)
    ncP = _prog("P", lambda: build_P(NTOK))
    resP = _run(ncP, [{"x": xs[c]} for c in range(NCORES)])
    xT_parts = [r["xT"] for r in resP]
    x_cur = [xs[c] for c in range(NCORES)]
    ncA = _prog("A", lambda: build_A(SEQ))
    consts = consts_A()
    for layer in range(DEPTH):
        moe = layer % 2 == 1
        jj = layer // 2
        xT_full = [np.ascontiguousarray(np.concatenate(xT_parts[b * 4:(b + 1) * 4], axis=1)) for b in range(BATCH)]
        in_maps = []
        for c in range(NCORES):
            b, j = divmod(c, 4)
            m = prep_A(f32(inp["w_in"][layer]), f32(inp["ssm_conv_w"][layer]), f32(inp["ssm_conv_b"][layer]),
                       f32(inp["ssm_dt_bias"][layer]), f32(inp["ssm_a_log"][layer]), f32(inp["ssm_d"][layer]),
                       f32(inp["fox_f_bias"][layer]), f32(inp["pool_w"][layer]), f32(inp["pool_b"][layer]),
                       f32(inp["pool_scale"][layer]), j)
            m.update(consts)
            m["xT"] = xT_full[b]
            in_maps.append(m)
        resA = _run(ncA, in_maps)
        mix = np.empty((BATCH, SEQ, D), np.float32)
        for c in range(NCORES):
            b, j = divmod(c, 4)
            y = resA[c]["y"]
            mix[b, :, 128 * j:128 * j + 128] = y[:, 0:128]
            mix[b, :, 512 + 64 * j:512 + 64 * j + 64] = y[:, 128:192]
            mix[b, :, 768 + 64 * j:768 + 64 * j + 64] = y[:, 192:256]
        mixs = mix.reshape(NCORES, NTOK, D)
        ncB = _prog("B%d" % moe, lambda: build_B(NTOK, moe))
        wts = {"normw": f32(inp["ssm_norm_w"][layer]), "w_out": f32(inp["w_out"][layer]),
               "ln1_g": f32(inp["ln1_g"][layer]), "ln1_b": f32(inp["ln1_b"][layer]),
               "ln2_g": f32(inp["ln2_g"][layer]), "ln2_b": f32(inp["ln2_b"][layer]),
               "ln3_g": f32(inp["ln3_g"][layer]), "ln3_b": f32(inp["ln3_b"][layer]),
               "wq": f32(inp["xa_wq"][layer]), "wk": f32(inp["xa_wk"][layer]),
               "wv": f32(inp["xa_wv"][layer]), "wo": f32(inp["xa_wo"][layer])}
        if moe:
            wts.update({"router": f32(inp["router_w"][jj]), "w1": f32(inp["moe_w1"][jj]),
                        "w3": f32(inp["moe_w3"][jj]), "w2": f32(inp["moe_w2"][jj])})
        else:
            wts.update({"w1": f32(inp["ffn_w1"][jj:jj + 1]), "w3": f32(inp["ffn_w3"][jj:jj + 1]),
                        "w2": f32(inp["ffn_w2"][jj:jj + 1])})
        in_maps = []
        for c in range(NCORES):
            m = dict(wts)
            m["mix"] = np.ascontiguousarray(mixs[c]); m["x"] = np.ascontiguousarray(x_cur[c])
            m["mem"] = mem[c // 4]
            in_maps.append(m)
        resB = _run(ncB, in_maps)
        x_cur = [r["xo"] for r in resB]
        xT_parts = [r["xoT"] for r in resB]
    return np.stack(x_cur).reshape(BATCH, SEQ, D).astype(np.float32)
```
